# Optimizing a Trainium2 kernel written in Bass

```python
import math
import jax, jax.numpy as jnp
from jax import lax
import numpy as np

D_MODEL = 2048
BATCH = 4
SEQ = 8192
DEPTH = 4

N_BRANCH = 4
BRANCH_WIDTH = 512
EPS = 1e-6
N_MOD = 6
DA_HEADS = 4
DA_QK_DIM = 64
DA_V_DIM = 128
Q_BLOCK = 128
N_BUCKETS = 32
MAX_DISTANCE = 128
ML_HEADS = 4
ML_DIM = 128
ML_CONV = 4
GLA_HEADS = 4
GLA_DK = 64
GLA_DV = 128
GLA_RANK = 16
GLA_TAU = 16.0
CHUNK = 64
S5_CH = 16
S5_GROUPS = BRANCH_WIDTH // S5_CH
S5_STATE = 64
FFN_HIDDEN = -(-(8 * D_MODEL) // (3 * 256)) * 256

DA_QK_W = DA_HEADS * 2 * DA_QK_DIM
DA_V_W = DA_HEADS * DA_V_DIM
ML_W = ML_HEADS * ML_DIM
GLA_K_W = GLA_HEADS * GLA_DK
GLA_V_W = GLA_HEADS * GLA_DV
IN_SPLITS = (DA_QK_W, DA_QK_W, DA_V_W,
             ML_W, ML_W, ML_W, ML_W, ML_HEADS, ML_HEADS,
             GLA_K_W, GLA_K_W, GLA_V_W, GLA_V_W, GLA_RANK,
             BRANCH_WIDTH)
IN_WIDTH = sum(IN_SPLITS)

kernel_name = 'hybrid_gated_parallel_mixer_trunk'


def _rms(x, gain=None):
    xf = x.astype(jnp.float32)
    y = xf * lax.rsqrt(jnp.mean(xf * xf, axis=-1, keepdims=True) + EPS)
    if gain is not None:
        y = y * gain.astype(jnp.float32)
    return y.astype(x.dtype)


def _t5_bucket(rel):
    n = jnp.maximum(-rel, 0)
    exact = N_BUCKETS // 2
    nf = jnp.maximum(n, 1).astype(jnp.float32)
    large = exact + (jnp.log(nf / exact) / math.log(MAX_DISTANCE / exact)
                     * (N_BUCKETS - exact)).astype(jnp.int32)
    return jnp.where(n < exact, n, jnp.minimum(large, N_BUCKETS - 1))


def _causal_conv(x, w):
    k, s = w.shape[0], x.shape[1]
    xp = jnp.pad(x, ((0, 0), (k - 1, 0), (0, 0)))
    y = xp[:, 0:s] * w[0]
    for j in range(1, k):
        y = y + xp[:, j:j + s] * w[j]
    return y


def _to_chunks(t, nc):
    t = t.reshape((t.shape[0], nc, CHUNK) + t.shape[2:])
    return jnp.moveaxis(jnp.swapaxes(t, 2, 3), 1, 0)


def _from_chunks(t):
    t = jnp.swapaxes(jnp.moveaxis(t, 0, 1), 2, 3)
    return t.reshape((t.shape[0], t.shape[1] * t.shape[2], -1))


def _diff_attention(q, k, v, lam, lam_init, rel_bias):
    B, S = q.shape[:2]
    q = q.reshape(B, S, DA_HEADS, 2, DA_QK_DIM).transpose(3, 0, 2, 1, 4)
    k = k.reshape(B, S, DA_HEADS, 2, DA_QK_DIM).transpose(3, 0, 2, 1, 4)
    v = v.reshape(B, S, DA_HEADS, DA_V_DIM).transpose(0, 2, 1, 3)
    kpos = jnp.arange(S)
    scale = DA_QK_DIM ** -0.5

    def block(i):
        start = i * Q_BLOCK
        qb = lax.dynamic_slice_in_dim(q, start, Q_BLOCK, axis=3)
        rel = kpos[None, :] - (start + jnp.arange(Q_BLOCK))[:, None]
        bias = jnp.transpose(rel_bias[_t5_bucket(rel)], (2, 0, 1))
        logits = jnp.einsum('mbhqd,mbhkd->mbhqk', qb, k) * scale + bias
        logits = jnp.where(rel <= 0, logits, -jnp.inf)
        p = jax.nn.softmax(logits, axis=-1)
        return jnp.einsum('bhqk,bhkd->bhqd', p[0] - lam * p[1], v)

    o = lax.map(block, jnp.arange(S // Q_BLOCK))
    o = o.transpose(1, 0, 3, 2, 4).reshape(B, S, DA_HEADS, DA_V_DIM)
    return (_rms(o) * (1.0 - lam_init)).reshape(B, S, DA_V_W)


def _mlstm(q, k, v, ig, lf):
    B, S = q.shape[:2]
    nc = S // CHUNK
    causal = jnp.tril(jnp.ones((CHUNK, CHUNK), bool))

    def step(carry, xs):
        C, n, m = carry
        qc, kc, vc, ic, lfc = xs
        b = jnp.cumsum(lfc, axis=-1)
        Dm = jnp.where(causal, b[..., :, None] - b[..., None, :] + ic[..., None, :], -jnp.inf)
        inter = b + m[..., None]
        m_t = jnp.maximum(inter, jnp.max(Dm, axis=-1))
        s = jnp.einsum('bhtd,bhjd->bhtj', qc, kc) * jnp.exp(Dm - m_t[..., None])
        a = jnp.exp(inter - m_t)
        num = a[..., None] * jnp.einsum('bhtd,bhde->bhte', qc, C) + jnp.einsum('bhtj,bhje->bhte', s, vc)
        den = a * jnp.einsum('bhtd,bhd->bht', qc, n) + jnp.sum(s, axis=-1)
        h = num / jnp.maximum(jnp.abs(den), jnp.exp(-m_t))[..., None]
        m_new = m_t[..., -1]
        a_state = jnp.exp(b[..., -1] + m - m_new)
        wj = jnp.exp(b[..., -1:] - b + ic - m_new[..., None])
        C = a_state[..., None, None] * C + jnp.einsum('bhj,bhjd,bhje->bhde', wj, kc, vc)
        n = a_state[..., None] * n + jnp.einsum('bhj,bhjd->bhd', wj, kc)
        return (C, n, m_new), h

    init = (jnp.zeros((B, ML_HEADS, ML_DIM, ML_DIM), jnp.float32),
            jnp.zeros((B, ML_HEADS, ML_DIM), jnp.float32),
            jnp.zeros((B, ML_HEADS), jnp.float32))
    xs = (_to_chunks(q, nc), _to_chunks(k, nc), _to_chunks(v, nc), _to_chunks(ig, nc), _to_chunks(lf, nc))
    _, h = lax.scan(step, init, xs)
    return _from_chunks(h)


def _gla(q, k, v, la):
    B, S = q.shape[:2]
    nc = S // CHUNK
    causal = jnp.tril(jnp.ones((CHUNK, CHUNK), bool))[:, :, None]

    def step(St, xs):
        qc, kc, vc, lac = xs
        bc = jnp.cumsum(lac, axis=2)
        inter = jnp.einsum('bhtd,bhde->bhte', qc * jnp.exp(bc), St)
        decay = jnp.exp(jnp.where(causal, bc[:, :, :, None, :] - bc[:, :, None, :, :], -jnp.inf))
        att = jnp.einsum('bhtd,bhjd,bhtjd->bhtj', qc, kc, decay)
        o = inter + jnp.einsum('bhtj,bhje->bhte', att, vc)
        last = bc[:, :, -1:, :]
        St = jnp.exp(last[:, :, 0])[..., None] * St + jnp.einsum('bhjd,bhje->bhde', kc * jnp.exp(last - bc), vc)
        return St, o

    init = jnp.zeros((B, GLA_HEADS, GLA_DK, GLA_DV), jnp.float32)
    xs = (_to_chunks(q, nc), _to_chunks(k, nc), _to_chunks(v, nc), _to_chunks(la, nc))
    _, o = lax.scan(step, init, xs)
    return _from_chunks(o).reshape(B, S, GLA_HEADS, GLA_DV)


def _s5(u, a_re, a_im, log_dt, b_re, b_im, c_re, c_im, d_skip):
    f32 = jnp.float32
    a_re, a_im = a_re.astype(f32), a_im.astype(f32)
    dt = jnp.exp(log_dt.astype(f32))[:, None]
    mag = jnp.exp(dt * a_re)
    ab_re, ab_im = mag * jnp.cos(dt * a_im), mag * jnp.sin(dt * a_im)
    nr, ni = ab_re - 1.0, ab_im
    den = a_re * a_re + a_im * a_im
    f_re = (nr * a_re + ni * a_im) / den
    f_im = (ni * a_re - nr * a_im) / den
    b_re, b_im = b_re.astype(f32), b_im.astype(f32)
    bb_re = f_re[..., None] * b_re - f_im[..., None] * b_im
    bb_im = f_re[..., None] * b_im + f_im[..., None] * b_re
    c_re, c_im, d_skip = c_re.astype(f32), c_im.astype(f32), d_skip.astype(f32)

    def combine(e1, e2):
        a1r, a1i, b1r, b1i = e1
        a2r, a2i, b2r, b2i = e2
        return (a2r * a1r - a2i * a1i, a2r * a1i + a2i * a1r,
                a2r * b1r - a2i * b1i + b2r, a2r * b1i + a2i * b1r + b2i)

    def one_seq(us):
        bu_re = jnp.einsum('sgc,gpc->sgp', us, bb_re)
        bu_im = jnp.einsum('sgc,gpc->sgp', us, bb_im)
        ar = jnp.broadcast_to(ab_re, bu_re.shape)
        ai = jnp.broadcast_to(ab_im, bu_re.shape)
        _, _, xr, xi = lax.associative_scan(combine, (ar, ai, bu_re, bu_im), axis=0)
        return (jnp.einsum('sgp,gcp->sgc', xr, c_re) - jnp.einsum('sgp,gcp->sgc', xi, c_im)
                + d_skip * us)

    return lax.map(one_seq, u)


def setup_inputs(seed: int = 0) -> dict:
    key = jax.random.key(seed)
    ks = jax.random.split(key, 32)
    f32 = jnp.float32

    def nrm(k, shape, scale):
        return jax.random.normal(k, shape, f32) * scale

    L, D, W, G, P, C = DEPTH, D_MODEL, BRANCH_WIDTH, S5_GROUPS, S5_STATE, S5_CH
    return {
        'x': nrm(ks[0], (BATCH, SEQ, D), 1.0),
        'c': nrm(ks[1], (BATCH, D), 1.0),
        'ada_w': nrm(ks[2], (L, D, N_MOD * D), 0.5 * D ** -0.5),
        'ada_b': nrm(ks[3], (L, N_MOD * D), 0.01),
        'norm_g': 1.0 + nrm(ks[4], (L, 4, D), 0.01),
        'w_in': nrm(ks[5], (L, D, IN_WIDTH), D ** -0.5),
        'rel_bias': nrm(ks[6], (N_BUCKETS, DA_HEADS), 0.5),
        'diff_lambda': nrm(ks[7], (L, 4, DA_QK_DIM), 0.1),
        'ml_conv': nrm(ks[8], (L, ML_CONV, 2 * ML_W), ML_CONV ** -0.5),
        'ml_gate_b': jnp.stack([nrm(ks[9], (L, ML_HEADS), 0.1),
                                jnp.linspace(3.0, 6.0, ML_HEADS, dtype=f32)[None, :]
                                + nrm(ks[10], (L, ML_HEADS), 0.1)], axis=1),
        'gla_wa2': nrm(ks[11], (L, GLA_RANK, GLA_K_W), GLA_RANK ** -0.5),
        'gla_ba': nrm(ks[12], (L, GLA_K_W), 0.1),
        's5_a_re': -0.5 + nrm(ks[13], (L, G, P), 0.01),
        's5_a_im': math.pi * jnp.arange(P, dtype=f32) + nrm(ks[14], (L, G, P), 0.01),
        's5_log_dt': jax.random.uniform(ks[15], (L, G), f32, math.log(1e-3), math.log(1e-1)),
        's5_b_re': nrm(ks[16], (L, G, P, C), (2 * C) ** -0.5),
        's5_b_im': nrm(ks[17], (L, G, P, C), (2 * C) ** -0.5),
        's5_c_re': nrm(ks[18], (L, G, C, P), (2 * P) ** -0.5),
        's5_c_im': nrm(ks[19], (L, G, C, P), (2 * P) ** -0.5),
        's5_d': nrm(ks[20], (L, G, C), 1.0),
        's5_glu_w': nrm(ks[21], (L, W, W), W ** -0.5),
        's5_glu_b': nrm(ks[22], (L, W), 0.01),
        'w_branch': nrm(ks[23], (L, N_BRANCH, W, D), W ** -0.5),
        'w_gate': nrm(ks[24], (L, N_BRANCH, D, D), D ** -0.5),
        'b_gate': nrm(ks[25], (L, N_BRANCH, D), 0.01),
        'w_out': nrm(ks[26], (L, D, D), D ** -0.5),
        'ffn_w_in': nrm(ks[27], (L, D, 2 * FFN_HIDDEN), D ** -0.5),
        'ffn_w_out': nrm(ks[28], (L, FFN_HIDDEN, D), FFN_HIDDEN ** -0.5),
    }


def reference(x, c, ada_w, ada_b, norm_g, w_in, rel_bias, diff_lambda, ml_conv, ml_gate_b,
              gla_wa2, gla_ba, s5_a_re, s5_a_im, s5_log_dt, s5_b_re, s5_b_im, s5_c_re, s5_c_im,
              s5_d, s5_glu_w, s5_glu_b, w_branch, w_gate, b_gate, w_out, ffn_w_in, ffn_w_out):
    B, S, _ = x.shape
    f32 = jnp.float32
    split_points = [int(p) for p in np.cumsum(IN_SPLITS)[:-1]]
    cs = jax.nn.silu(c)
    rel_bias32 = rel_bias.astype(f32)
    for l in range(DEPTH):
        mod = (cs @ ada_w[l] + ada_b[l]).reshape(B, N_MOD, D_MODEL)[:, :, None, :]
        shift_m, scale_m, gate_m, shift_f, scale_f, gate_f = (mod[:, i] for i in range(N_MOD))

        h = _rms(x, norm_g[l, 0]) * (1.0 + scale_m) + shift_m
        proj = (h @ w_in[l]).astype(f32)
        (da_q, da_k, da_v, ml_q, ml_k, ml_v, ml_o, ml_i, ml_f,
         gl_q, gl_k, gl_v, gl_r, gl_a, s5_u) = jnp.split(proj, split_points, axis=-1)

        lam_init = 0.8 - 0.6 * math.exp(-0.3 * l)
        lp = diff_lambda[l].astype(f32)
        lam = jnp.exp(jnp.sum(lp[0] * lp[1])) - jnp.exp(jnp.sum(lp[2] * lp[3])) + lam_init
        o_a = _diff_attention(da_q, da_k, da_v, lam, lam_init, rel_bias32)

        qk = jax.nn.silu(_causal_conv(jnp.concatenate([ml_q, ml_k], axis=-1), ml_conv[l].astype(f32)))
        mq, mk = jnp.split(qk, 2, axis=-1)
        gb = ml_gate_b[l].astype(f32)
        hm = _mlstm(mq.reshape(B, S, ML_HEADS, ML_DIM),
                    mk.reshape(B, S, ML_HEADS, ML_DIM) * ML_DIM ** -0.5,
                    ml_v.reshape(B, S, ML_HEADS, ML_DIM),
                    ml_i + gb[0], jax.nn.log_sigmoid(ml_f + gb[1]))
        o_b = jax.nn.sigmoid(ml_o) * hm

        la = jax.nn.log_sigmoid(gl_a @ gla_wa2[l].astype(f32) + gla_ba[l].astype(f32)) / GLA_TAU
        og = _gla(gl_q.reshape(B, S, GLA_HEADS, GLA_DK) * GLA_DK ** -0.5,
                  gl_k.reshape(B, S, GLA_HEADS, GLA_DK),
                  gl_v.reshape(B, S, GLA_HEADS, GLA_DV),
                  la.reshape(B, S, GLA_HEADS, GLA_DK))
        o_c = _rms(og).reshape(B, S, GLA_V_W) * jax.nn.silu(gl_r)

        y = _s5(s5_u.reshape(B, S, S5_GROUPS, S5_CH), s5_a_re[l], s5_a_im[l], s5_log_dt[l],
                s5_b_re[l], s5_b_im[l], s5_c_re[l], s5_c_im[l], s5_d[l]).reshape(B, S, BRANCH_WIDTH)
        z = jax.nn.gelu(y)
        o_d = z * jax.nn.sigmoid(z @ s5_glu_w[l].astype(f32) + s5_glu_b[l].astype(f32))

        branches = (o_a, o_b, o_c, o_d)
        merged = [jax.nn.sigmoid(h @ w_gate[l, i] + b_gate[l, i]) * (branches[i].astype(h.dtype) @ w_branch[l, i])
                  for i in range(N_BRANCH)]
        mix = (merged[0] + merged[1] + merged[2] + merged[3]) @ w_out[l]
        x = x + (gate_m * _rms(mix, norm_g[l, 1])).astype(x.dtype)

        h = _rms(x, norm_g[l, 2]) * (1.0 + scale_f) + shift_f
        a, g = jnp.split(h @ ffn_w_in[l], 2, axis=-1)
        x = x + (gate_f * _rms((jax.nn.silu(a) * g) @ ffn_w_out[l], norm_g[l, 3])).astype(x.dtype)
    return x
```

```python
import math
import contextlib
import numpy as np
import concourse.bass as bass
import concourse.mybir as mybir
from concourse.bass_utils import run_bass_kernel_spmd

F32 = mybir.dt.float32
BF16 = mybir.dt.bfloat16
AF = mybir.ActivationFunctionType
ALU = mybir.AluOpType
AX = mybir.AxisListType

D = 2048
NDT = 16
DEPTH = 4
FFH = 5632
INW = 5656
EPS = 1e-6
N_BUCKETS = 32
MAX_DISTANCE = 128


class Res:
    __slots__ = ("w", "r")

    def __init__(self):
        self.w = None
        self.r = {}


class KB:
    def __init__(self, nc):
        self.nc = nc
        self.eng = {"pe": nc.tensor, "act": nc.scalar, "dve": nc.vector, "pool": nc.gpsimd, "sp": nc.sync}
        self.sems = []
        self.own = {}
        self.cnt = {}
        self.known = {e: {} for e in self.eng}
        for e in ("pe", "act", "dve", "pool"):
            self.own[e] = self._newsem("c_" + e)
            self.cnt[e] = 0
        self.lanes = {}
        self.uid = 0
        self.stack = None

    def _newsem(self, name):
        self.sems.append(self.nc.alloc_semaphore(name))
        return len(self.sems) - 1

    def add_lane(self, name, eng, k):
        self.lanes[name] = {"eng": eng, "sems": [self._newsem(f"l_{name}{i}") for i in range(k)], "n": 0}

    def _deps(self, reads, writes):
        toks = []
        for r in reads:
            if r.w is not None:
                toks.append(r.w)
        for w in writes:
            if w.w is not None:
                toks.append(w.w)
            toks.extend(w.r.items())
        return toks

    def _wait(self, e, toks):
        kn = self.known[e]
        eng = self.eng[e]
        for si, v in toks:
            if e == "pe" and si == self.own.get("pe"):
                continue
            if kn.get(si, 0) < v:
                eng.wait_ge(self.sems[si], v)
                kn[si] = v

    def _mark(self, tok, reads, writes):
        si, v = tok
        for r in reads:
            if r.r.get(si, 0) < v:
                r.r[si] = v
        for w in writes:
            w.w = tok
            w.r = {}

    def op(self, e, reads, writes, fn, inc=True):
        self._wait(e, self._deps(reads, writes))
        ins = fn(self.eng[e])
        if inc:
            self.cnt[e] += 1
            ins.then_inc(self.sems[self.own[e]], 1)
            tok = (self.own[e], self.cnt[e])
        else:
            tok = (self.own[e], self.cnt[e] + 1)
        self._mark(tok, reads, writes)

    def dma(self, lane, out, in_, reads, writes, **kw):
        L = self.lanes[lane]
        e = L["eng"]
        n = L["n"]
        k = len(L["sems"])
        toks = self._deps(reads, writes)
        si = L["sems"][n % k]
        if n >= k:
            toks.append((si, 16 * (n // k)))
        self._wait(e, toks)
        self.eng[e].dma_start(out=out, in_=in_, **kw).then_inc(self.sems[si], 16)
        L["n"] = n + 1
        self._mark((si, 16 * (n // k + 1)), reads, writes)

    def final_wait(self, e, ress):
        toks = []
        for r in ress:
            if r.w is not None:
                toks.append(r.w)
        self._wait(e, toks)

    def sb(self, shape, dt, name=None):
        self.uid += 1
        nm = f"{name or 't'}_{self.uid}"
        if self.stack is not None:
            return self.stack.enter_context(self.nc.sbuf_tensor(nm, list(shape), dt))
        return self.nc.alloc_sbuf_tensor(nm, list(shape), dt)

    def scope(self):
        return _Scope(self)

    def dram(self, name, shape, dt, kind="Internal"):
        return self.nc.dram_tensor(name, list(shape), dt, kind=kind).ap()


class _Scope:
    def __init__(self, kb):
        self.kb = kb

    def __enter__(self):
        self.prev = self.kb.stack
        self.kb.stack = contextlib.ExitStack()
        return self

    def __exit__(self, *a):
        self.kb.stack.close()
        self.kb.stack = self.prev
        return False


class T:
    def __init__(self, t):
        self.t = t
        self.res = Res()

    def __getitem__(self, k):
        return self.t[k]


def barrier(kb):
    toks = []
    for e in ("pe", "act", "dve", "pool"):
        if kb.cnt[e] > 0:
            toks.append((kb.own[e], kb.cnt[e]))
    for L in kb.lanes.values():
        n = L["n"]
        k = len(L["sems"])
        for j in range(min(n, k)):
            m = n - 1 - j
            toks.append((L["sems"][m % k], 16 * (m // k + 1)))
    for e in kb.eng:
        kb._wait(e, toks)


class G:
    pass


def alloc_psum(kb, g):
    g.ps = []
    for i in range(8):
        g.ps.append(T(kb.nc.alloc_psum_tensor(f"psb{i}", [128, 512], F32)))
    g.psi = 0
    g.psn = 8


def nps(g):
    p = g.ps[g.psi % g.psn]
    g.psi += 1
    return p


def tsl(i, n=128):
    return slice(i * n, (i + 1) * n)


def emit_consts(kb, g, dr):
    nc = kb.nc
    g.ident = T(kb.sb([128, 128], F32, "ident"))
    g.tri = T(kb.sb([128, 128], F32, "tri"))
    g.ones = T(kb.sb([128, 128], F32, "ones"))
    g.tri_b = T(kb.sb([128, 128], BF16, "trib"))
    kb.dma("sp", g.ident[:], dr["c_ident"], [], [g.ident.res])
    kb.dma("sp", g.tri[:], dr["c_tri"], [], [g.tri.res])
    kb.op("dve", [], [g.ones.res], lambda e: e.memset(g.ones[:], 1.0))
    kb.op("dve", [g.tri.res], [g.tri_b.res], lambda e: e.tensor_copy(out=g.tri_b[:], in_=g.tri[:]))


def emit_mods(kb, g, dr, NL):
    nc = kb.nc
    with kb.scope():
        cs = T(kb.sb([128, NDT], F32, "cs"))
        sg = T(kb.sb([128, NDT], F32, "sg"))
        kb.dma("sp", cs[:], dr["c"].rearrange("o (kt p) -> p (o kt)", p=128), [], [cs.res],
               allow_slow_non_contiguous=True)
        kb.op("act", [cs.res], [sg.res], lambda e: e.activation(out=sg[:], in_=cs[:], func=AF.Sigmoid))
        kb.op("dve", [cs.res, sg.res], [cs.res],
              lambda e: e.tensor_tensor(out=cs[:], in0=cs[:], in1=sg[:], op=ALU.mult))
        wt = [T(kb.sb([128, NDT, 512], F32, "adaw")) for _ in range(2)]
        bt = [T(kb.sb([1, 512], F32, "adab")) for _ in range(2)]
        ot = [T(kb.sb([1, 512], F32, "adao")) for _ in range(2)]
        it = 0
        for l in range(NL):
            wv = dr["ada_w"][l].rearrange("(kt p) n -> p kt n", p=128)
            for cg in range(6 * D // 512):
                w = wt[it % 2]
                b = bt[it % 2]
                o = ot[it % 2]
                kb.dma("sp", w[:], wv[:, :, tsl(cg, 512)], [], [w.res])
                kb.dma("sp", b[:], dr["ada_b"][l:l + 1, tsl(cg, 512)], [], [b.res])
                p = nps(g)
                for kt in range(NDT):
                    kb.op("pe", [cs.res, w.res], [p.res],
                          lambda e, kt=kt: e.matmul(p[0:1, :], lhsT=cs[:, kt:kt + 1], rhs=w[:, kt, :],
                                                    start=(kt == 0), stop=(kt == NDT - 1)),
                          inc=(kt == NDT - 1))
                kb.op("dve", [p.res, b.res], [o.res],
                      lambda e: e.tensor_tensor(out=o[:], in0=p[0:1, :], in1=b[:], op=ALU.add))
                kb.dma("sp", dr["mod_scr"][l:l + 1, tsl(cg, 512)], o[:], [o.res], [g.mod_res])
                it += 1
    barrier(kb)


def emit_layer_consts(kb, g, dr, l):
    L = G()
    modrow = T(kb.sb([96, 128], F32, "modrow"))
    grow = T(kb.sb([64, 128], F32, "grow"))
    L.modT = T(kb.sb([128, 96], F32, "modT"))
    L.gT = T(kb.sb([128, 64], F32, "gT"))
    kb.dma("sp", modrow[:], dr["mod_scr"][l].rearrange("(j p) -> j p", p=128), [g.mod_res], [modrow.res])
    kb.dma("sp", grow[:], dr["norm_g"][l].rearrange("i (j p) -> (i j) p", p=128), [], [grow.res])
    p = nps(g)
    kb.op("pe", [modrow.res, g.ident.res], [p.res],
          lambda e: e.transpose(p[:, 0:96], modrow[:], g.ident[0:96, 0:96]))
    kb.op("dve", [p.res], [L.modT.res], lambda e: e.tensor_copy(out=L.modT[:], in_=p[:, 0:96]))
    p2 = nps(g)
    kb.op("pe", [grow.res, g.ident.res], [p2.res],
          lambda e: e.transpose(p2[:, 0:64], grow[:], g.ident[0:64, 0:64]))
    kb.op("dve", [p2.res], [L.gT.res], lambda e: e.tensor_copy(out=L.gT[:], in_=p2[:, 0:64]))
    L.gs_m = T(kb.sb([128, NDT], F32, "gsm"))
    L.gs_f = T(kb.sb([128, NDT], F32, "gsf"))
    for (dst, gi, sc) in ((L.gs_m, 0, 16), (L.gs_f, 2, 64)):
        kb.op("dve", [L.modT.res, L.gT.res], [dst.res],
              lambda e, dst=dst, gi=gi, sc=sc: e.scalar_tensor_tensor(
                  out=dst[:], in0=L.modT[:, sc:sc + 16], scalar=1.0, in1=L.gT[:, gi * 16:gi * 16 + 16],
                  op0=ALU.add, op1=ALU.mult))
    L.sh_m = T(kb.sb([128, NDT], F32, "shm"))
    L.sh_f = T(kb.sb([128, NDT], F32, "shf"))
    kb.op("dve", [L.modT.res], [L.sh_m.res], lambda e: e.tensor_copy(out=L.sh_m[:], in_=L.modT[:, 0:16]))
    kb.op("dve", [L.modT.res], [L.sh_f.res], lambda e: e.tensor_copy(out=L.sh_f[:], in_=L.modT[:, 48:64]))
    return L


def rstd_from_ss(kb, ss, n, tmp):
    kb.op("dve", [ss.res], [ss.res],
          lambda e: e.tensor_scalar(out=ss[:], in0=ss[:], scalar1=1.0 / n, scalar2=EPS, op0=ALU.mult, op1=ALU.add))
    kb.op("act", [ss.res], [ss.res], lambda e: e.activation(out=ss[:], in_=ss[:], func=AF.Sqrt))
    kb.op("dve", [ss.res], [ss.res], lambda e: e.reciprocal(out=ss[:], in_=ss[:]))


def norm_transpose_block(kb, g, xts, junk, gs, sh, hT, hres, stat):
    for tt in range(4):
        x = xts[tt]
        ss = stat[tt]
        kb.op("act", [x.res], [junk.res, ss.res],
              lambda e: e.activation(out=junk[:], in_=x[:], func=AF.Square, accum_out=ss[:]))
        rstd_from_ss(kb, ss, D, None)
        kb.op("dve", [x.res, ss.res], [x.res],
              lambda e: e.tensor_scalar(out=x[:], in0=x[:], scalar1=ss[:, 0:1], scalar2=None, op0=ALU.mult))
    for dt in range(NDT):
        p = nps(g)
        for tt in range(4):
            kb.op("pe", [xts[tt].res, g.ident.res], [p.res],
                  lambda e: e.transpose(p[:, tsl(tt)], xts[tt][:, tsl(dt)], g.ident[:]), inc=(tt == 3))
        kb.op("act", [p.res, gs.res, sh.res], [hres[dt]],
              lambda e: e.activation(out=hT[:, dt, :], in_=p[:], func=AF.Identity,
                                     scale=gs[:, dt:dt + 1], bias=sh[:, dt:dt + 1]))


PROJ_F = [
    ("qk", 0, 1024), ("mlqk", 1536, 1024), ("glqk", 3592, 512), ("gla", 5128, 16), ("s5u", 5144, 512)]
PROJ_T = [
    ("dav", 1024, 512), ("mlv", 2560, 512), ("mlo", 3072, 512), ("mlif", 3584, 8), ("glv", 4104, 512),
    ("glr", 4616, 512)]


def emit_phaseA(kb, g, dr, L, l, S):
    nc = kb.nc
    NB = S // 512
    xv = dr["xres"]
    with kb.scope():
        xt = [[T(kb.sb([128, D], F32, "xt")) for _ in range(4)] for _ in range(2)]
        junk = T(kb.sb([128, D], F32, "junk"))
        stat = [T(kb.sb([128, 1], F32, "stat")) for _ in range(4)]
        hT = T(kb.sb([128, NDT, 512], BF16, "hT"))
        hres = [Res() for _ in range(NDT)]
        wT = [T(kb.sb([128, NDT, 512], BF16, "wT")) for _ in range(3)]
        stf = [T(kb.sb([128, 512], F32, "stf")) for _ in range(4)]
        stb = [T(kb.sb([128, 512], BF16, "stb")) for _ in range(3)]
        cnt = {"wf": 0, "wt": 0, "sf": 0, "sb": 0, "ev": 0}
        win = dr["w_in"][l].rearrange("(kt p) n -> p kt n", p=128)

        def load_x(b):
            for tt in range(4):
                t = xt[b % 2][tt]
                kb.dma("sp", t[:], xv[b * 512 + tt * 128: b * 512 + (tt + 1) * 128, :], [g.x_res], [t.res])

        def evac(out_ap, in_ap, reads, writes, scale=None):
            cnt["ev"] += 1
            if cnt["ev"] % 2 == 0:
                kb.op("act", reads, writes,
                      lambda e: e.activation(out=out_ap, in_=in_ap, func=AF.Copy,
                                             scale=(1.0 if scale is None else scale)))
            else:
                if scale is None:
                    kb.op("dve", reads, writes, lambda e: e.tensor_copy(out=out_ap, in_=in_ap))
                else:
                    kb.op("dve", reads, writes,
                          lambda e: e.tensor_scalar(out=out_ap, in0=in_ap, scalar1=scale, scalar2=None,
                                                    op0=ALU.mult))

        load_x(0)
        for b in range(NB):
            if b + 1 < NB:
                load_x(b + 1)
            xts = xt[b % 2]
            norm_transpose_block(kb, g, xts, junk, L.gs_m, L.sh_m, hT, hres, stat)
            kb.dma("sp", dr["hT_scr"].rearrange("dt p s -> p dt s")[:, :, tsl(b, 512)], hT[:], hres,
                   [g.scr_res["hT"]])
            ts0 = b * 512
            for (name, c0, ncols) in PROJ_F:
                for gi in range((ncols + 511) // 512):
                    gcols = min(512, ncols - gi * 512)
                    w = wT[cnt["wt"] % 3]
                    cnt["wt"] += 1
                    kb.dma("w", w[:, :, 0:gcols], win[:, :, c0 + gi * 512: c0 + gi * 512 + gcols], [], [w.res])
                    for tq in range((gcols + 127) // 128):
                        ti = gi * 4 + tq
                        nc_ = min(128, gcols - tq * 128)
                        p = nps(g)
                        for kt in range(NDT):
                            kb.op("pe", [w.res, hres[kt]], [p.res],
                                  lambda e: e.matmul(p[0:nc_, :], lhsT=w[:, kt, tq * 128:tq * 128 + nc_], rhs=hT[:, kt, :],
                                                     start=(kt == 0), stop=(kt == NDT - 1)), inc=(kt == NDT - 1))
                        if name == "qk":
                            s = stb[cnt["sb"] % 3]
                            cnt["sb"] += 1
                            sc = 0.125 if ti < 4 else None
                            evac(s[:], p[:], [p.res], [s.res], scale=sc)
                            kb.dma("sp", dr["qk_scr"][ti, :, ts0:ts0 + 512], s[:], [s.res], [g.scr_res["qk"]])
                        else:
                            s = stf[cnt["sf"] % 4]
                            cnt["sf"] += 1
                            sc = 0.125 if (name == "glqk" and ti < 2) else None
                            evac(s[0:nc_, :], p[0:nc_, :], [p.res], [s.res], scale=sc)
                            dst = {"mlqk": dr["mlqk_scr"], "glqk": dr["glqk_scr"], "s5u": dr["s5u_scr"]}.get(name)
                            if name == "gla":
                                kb.dma("sp", dr["gla_scr"][:, ts0:ts0 + 512], s[0:16, :], [s.res], [g.scr_res["gla"]])
                            else:
                                kb.dma("sp", dst[ti, :, ts0:ts0 + 512], s[:], [s.res], [g.scr_res[name]])
            for (name, c0, ncols) in PROJ_T:
                w = wT[cnt["wt"] % 3]
                cnt["wt"] += 1
                kb.dma("w", w[:, :, 0:ncols], win[:, :, c0:c0 + ncols], [], [w.res])
                for tt in range(4):
                    p = nps(g)
                    for kt in range(NDT):
                        kb.op("pe", [w.res, hres[kt]], [p.res],
                              lambda e: e.matmul(p[:, 0:ncols], lhsT=hT[:, kt, tsl(tt)], rhs=w[:, kt, 0:ncols],
                                                 start=(kt == 0), stop=(kt == NDT - 1)), inc=(kt == NDT - 1))
                    r0 = ts0 + tt * 128
                    if name == "dav":
                        s = stb[cnt["sb"] % 3]
                        cnt["sb"] += 1
                    else:
                        s = stf[cnt["sf"] % 4]
                        cnt["sf"] += 1
                    evac(s[:, 0:ncols], p[:, 0:ncols], [p.res], [s.res])
                    kb.dma("sp", dr[name + "_scr"][r0:r0 + 128, :], s[:, 0:ncols], [s.res], [g.scr_res[name]])
    barrier(kb)


def make_gg(kb, g, dr, l, gi, mi, dst, tmp):
    kb.dma("sp", dst[:], dr["norm_g"][l, gi:gi + 1, :].partition_broadcast(128), [], [dst.res])
    kb.dma("sp", tmp[:], dr["mod_scr"][l:l + 1, mi * D:(mi + 1) * D].partition_broadcast(128),
           [g.mod_res], [tmp.res])
    kb.op("dve", [tmp.res], [dst.res],
          lambda e: e.tensor_tensor(out=dst[:], in0=dst[:], in1=tmp[:], op=ALU.mult))


def epilogue(kb, g, dr, yT, yres, gg, b, bufs):
    xe, junk, ss4, ss, tmp = bufs
    for tt in range(4):
        r0 = b * 512 + tt * 128
        x = xe[tt % 2]
        kb.dma("sp", x[:], dr["xres"][r0:r0 + 128, :], [g.x_res], [x.res])
        banks = [nps(g) for _ in range(4)]
        for cg in range(4):
            for dd in range(4):
                kb.op("pe", [yres[cg * 4 + dd], g.ident.res], [banks[cg].res],
                      lambda e: e.transpose(banks[cg][:, tsl(dd)], yT[:, cg * 4 + dd, tsl(tt)], g.ident[:]),
                      inc=(dd == 3))
            kb.op("act", [banks[cg].res], [junk.res, ss4.res],
                  lambda e: e.activation(out=junk[:], in_=banks[cg][:], func=AF.Square,
                                         accum_out=ss4[:, cg:cg + 1]))
        kb.op("dve", [ss4.res], [ss.res], lambda e: e.reduce_sum(out=ss[:], in_=ss4[:], axis=AX.X))
        rstd_from_ss(kb, ss, D, None)
        for cg in range(4):
            t = tmp[cg % 2]
            kb.op("dve", [banks[cg].res, ss.res, gg.res], [t.res],
                  lambda e: e.scalar_tensor_tensor(out=t[:], in0=banks[cg][:], scalar=ss[:, 0:1],
                                                   in1=gg[:, tsl(cg, 512)], op0=ALU.mult, op1=ALU.mult))
            kb.op("pool", [t.res], [x.res],
                  lambda e: e.tensor_tensor(out=x[:, tsl(cg, 512)], in0=x[:, tsl(cg, 512)], in1=t[:], op=ALU.add))
        kb.dma("sp", dr["xres"][r0:r0 + 128, :], x[:], [x.res], [g.x_res])


def emit_phaseC1(kb, g, dr, L, l, S):
    nc = kb.nc
    NB = S // 512
    with kb.scope():
        hT = [T(kb.sb([128, NDT, 512], BF16, "hT"))] * 2
        oT = [T(kb.sb([128, NDT, 512], BF16, "oT"))] * 2
        mg = T(kb.sb([128, NDT, 512], BF16, "mg"))
        mres = [Res() for _ in range(NDT)]
        wg = [T(kb.sb([128, NDT, 512], BF16, "wg")) for _ in range(2)]
        wb = [T(kb.sb([128, 4, 512], BF16, "wb")) for _ in range(2)]
        yT = T(kb.sb([128, NDT, 512], F32, "yT"))
        yres = [Res() for _ in range(NDT)]
        acc = [T(kb.sb([128, 512], F32, "acc")) for _ in range(4)]
        sg = [T(kb.sb([128, 512], F32, "sg")) for _ in range(2)]
        tm = [T(kb.sb([128, 512], F32, "tm")) for _ in range(2)]
        bgrow = T(kb.sb([64, 128], F32, "bgrow"))
        bgT = T(kb.sb([128, 64], F32, "bgT"))
        xe = [T(kb.sb([128, D], F32, "xe")) for _ in range(2)]
        junk = T(kb.sb([128, 512], F32, "junk"))
        ss4 = T(kb.sb([128, 4], F32, "ss4"))
        ss = T(kb.sb([128, 1], F32, "ss"))
        ebufs = (xe, junk, ss4, ss, tm)
        gg_m = T(kb.sb([128, D], F32, "ggm"))
        make_gg(kb, g, dr, l, 1, 2, gg_m, xe[0])
        kb.dma("sp", bgrow[:], dr["b_gate"][l].rearrange("i (j p) -> (i j) p", p=128), [], [bgrow.res])
        p = nps(g)
        kb.op("pe", [bgrow.res, g.ident.res], [p.res],
              lambda e: e.transpose(p[:, 0:64], bgrow[:], g.ident[0:64, 0:64]))
        kb.op("dve", [p.res], [bgT.res], lambda e: e.tensor_copy(out=bgT[:], in_=p[:, 0:64]))
        nw = 0

        def load_blk(b):
            kb.dma("sp", hT[b % 2][:], dr["hT_scr"].rearrange("dt p s -> p dt s")[:, :, tsl(b, 512)],
                   [g.scr_res["hT"]], [hT[b % 2].res])
            kb.dma("sp", oT[b % 2][:], dr["o_scr"].rearrange("dt p s -> p dt s")[:, :, tsl(b, 512)],
                   [g.scr_res["o"]], [oT[b % 2].res])

        for b in range(NB):
            load_blk(b)
            h = hT[b % 2]
            o = oT[b % 2]
            for cg in range(4):
                for i in range(4):
                    w = wg[nw % 2]
                    w2 = wb[nw % 2]
                    nw += 1
                    kb.dma("w", w[:], dr["w_gate"][l, i].rearrange("(kt p) n -> p kt n", p=128)[:, :, tsl(cg, 512)],
                           [], [w.res])
                    kb.dma("w", w2[:], dr["w_branch"][l, i].rearrange("(kt p) n -> p kt n", p=128)[:, :, tsl(cg, 512)],
                           [], [w2.res])
                    for dd in range(4):
                        dt = cg * 4 + dd
                        pg = nps(g)
                        pb = nps(g)
                        for kt in range(NDT):
                            kb.op("pe", [w.res, h.res], [pg.res],
                                  lambda e: e.matmul(pg[:], lhsT=w[:, kt, tsl(dd)], rhs=h[:, kt, :],
                                                     start=(kt == 0), stop=(kt == NDT - 1)), inc=(kt == NDT - 1))
                        for kk in range(4):
                            kb.op("pe", [w2.res, o.res], [pb.res],
                                  lambda e: e.matmul(pb[:], lhsT=w2[:, kk, tsl(dd)], rhs=o[:, i * 4 + kk, :],
                                                     start=(kk == 0), stop=(kk == 3)), inc=(kk == 3))
                        s_ = sg[(i * 4 + dd) % 2]
                        kb.op("act", [pg.res, bgT.res], [s_.res],
                              lambda e: e.activation(out=s_[:], in_=pg[:], func=AF.Sigmoid,
                                                     bias=bgT[:, i * 16 + dt:i * 16 + dt + 1], scale=1.0))
                        a = acc[dd]
                        if i == 0:
                            kb.op("dve", [s_.res, pb.res], [a.res],
                                  lambda e: e.tensor_tensor(out=a[:], in0=s_[:], in1=pb[:], op=ALU.mult))
                        else:
                            kb.op("dve", [s_.res, pb.res], [s_.res],
                                  lambda e: e.tensor_tensor(out=s_[:], in0=s_[:], in1=pb[:], op=ALU.mult))
                            if i < 3:
                                kb.op("pool", [s_.res, a.res], [a.res],
                                      lambda e: e.tensor_tensor(out=a[:], in0=a[:], in1=s_[:], op=ALU.add))
                            else:
                                kb.op("pool", [s_.res, a.res], [mres[dt]],
                                      lambda e: e.tensor_tensor(out=mg[:, dt, :], in0=a[:], in1=s_[:], op=ALU.add))
            for cg in range(4):
                w = wg[nw % 2]
                nw += 1
                kb.dma("w", w[:], dr["w_out"][l].rearrange("(kt p) n -> p kt n", p=128)[:, :, tsl(cg, 512)],
                       [], [w.res])
                for dd in range(4):
                    dt = cg * 4 + dd
                    pm = nps(g)
                    for kt in range(NDT):
                        kb.op("pe", [w.res, mres[kt]], [pm.res],
                              lambda e: e.matmul(pm[:], lhsT=w[:, kt, tsl(dd)], rhs=mg[:, kt, :],
                                                 start=(kt == 0), stop=(kt == NDT - 1)), inc=(kt == NDT - 1))
                    kb.op("act", [pm.res], [yres[dt]],
                          lambda e: e.activation(out=yT[:, dt, :], in_=pm[:], func=AF.Copy))
            epilogue(kb, g, dr, yT, yres, gg_m, b, ebufs)
    barrier(kb)


def emit_phaseC2(kb, g, dr, L, l, S):
    nc = kb.nc
    NB = S // 512
    NH = FFH // 128
    with kb.scope():
        xt = [T(kb.sb([128, D], F32, "xt")) for _ in range(4)]
        junkb = T(kb.sb([128, D], BF16, "junkb"))
        stat = [T(kb.sb([128, 1], F32, "stat")) for _ in range(4)]
        hT = T(kb.sb([128, NDT, 512], BF16, "h2T"))
        hres = [Res() for _ in range(NDT)]
        uT = T(kb.sb([128, NH, 512], BF16, "uT"))
        ures = [Res() for _ in range(NH)]
        wa = [T(kb.sb([128, NDT, 256], BF16, "wa")) for _ in range(2)]
        wgt = [T(kb.sb([128, NDT, 256], BF16, "wgt")) for _ in range(2)]
        wo = [T(kb.sb([128, NH, 128], BF16, "wo")) for _ in range(2)]
        yT = T(kb.sb([128, NDT, 512], F32, "yT"))
        yres = [Res() for _ in range(NDT)]
        sa = [T(kb.sb([128, 512], F32, "sa")) for _ in range(2)]
        tm = [T(kb.sb([128, 512], F32, "tm")) for _ in range(2)]
        junk = T(kb.sb([128, 512], F32, "junk"))
        ss4 = T(kb.sb([128, 4], F32, "ss4"))
        ss = T(kb.sb([128, 1], F32, "ss"))
        ebufs = ([xt[0], xt[1]], junk, ss4, ss, tm)
        gg_f = T(kb.sb([128, D], F32, "ggf"))
        make_gg(kb, g, dr, l, 3, 5, gg_f, xt[0])
        wi = dr["ffn_w_in"][l].rearrange("(kt p) n -> p kt n", p=128)
        wov = dr["ffn_w_out_r"][l]
        nw = 0
        nwo = 0
        for b in range(NB):
            for tt in range(4):
                kb.dma("sp", xt[tt][:], dr["xres"][b * 512 + tt * 128: b * 512 + (tt + 1) * 128, :],
                       [g.x_res], [xt[tt].res])
            norm_transpose_block(kb, g, xt, junkb, L.gs_f, L.sh_f, hT, hres, stat)
            for j in range(NH):
                if j % 2 == 0:
                    w1 = wa[nw % 2]
                    w2 = wgt[nw % 2]
                    nw += 1
                    kb.dma("w", w1[:], wi[:, :, tsl(j // 2, 256)], [], [w1.res])
                    kb.dma("w", w2[:], wi[:, :, FFH + (j // 2) * 256: FFH + (j // 2 + 1) * 256], [], [w2.res])
                jj = tsl(j % 2)
                pa = nps(g)
                pg = nps(g)
                for kt in range(NDT):
                    kb.op("pe", [w1.res, hres[kt]], [pa.res],
                          lambda e: e.matmul(pa[:], lhsT=w1[:, kt, jj], rhs=hT[:, kt, :],
                                             start=(kt == 0), stop=(kt == NDT - 1)), inc=(kt == NDT - 1))
                for kt in range(NDT):
                    kb.op("pe", [w2.res, hres[kt]], [pg.res],
                          lambda e: e.matmul(pg[:], lhsT=w2[:, kt, jj], rhs=hT[:, kt, :],
                                             start=(kt == 0), stop=(kt == NDT - 1)), inc=(kt == NDT - 1))
                s_ = sa[j % 2]
                kb.op("act", [pa.res], [s_.res], lambda e: e.activation(out=s_[:], in_=pa[:], func=AF.Silu))
                kb.op("dve", [s_.res, pg.res], [ures[j]],
                      lambda e: e.tensor_tensor(out=uT[:, j, :], in0=s_[:], in1=pg[:], op=ALU.mult))
            for dt in range(NDT):
                w = wo[nwo % 2]
                nwo += 1
                kb.dma("w", w[:], wov[dt], [], [w.res])
                py = nps(g)
                for kt in range(NH):
                    kb.op("pe", [w.res, ures[kt]], [py.res],
                          lambda e: e.matmul(py[:], lhsT=w[:, kt, :], rhs=uT[:, kt, :],
                                             start=(kt == 0), stop=(kt == NH - 1)), inc=(kt == NH - 1))
                kb.op("act", [py.res], [yres[dt]],
                      lambda e: e.activation(out=yT[:, dt, :], in_=py[:], func=AF.Copy))
            epilogue(kb, g, dr, yT, yres, gg_f, b, ebufs)
    barrier(kb)


def emit_attn(kb, g, dr, l, S):
    nc = kb.nc
    NQB = S // 512
    NKT = S // 128
    lam_init = 0.8 - 0.6 * math.exp(-0.3 * l)
    g.psn = 4
    with kb.scope():
        QT = T(kb.sb([128, S], BF16, "QT"))
        KT = T(kb.sb([128, S], BF16, "KT"))
        V = T(kb.sb([128, NKT, 129], BF16, "V"))
        biasT = T(kb.sb([128, 5, 512], F32, "biasT"))
        cb = T(kb.sb([128, 4], F32, "cb"))
        lp = T(kb.sb([128, 256], F32, "lp"))
        lam = T(kb.sb([128, 4], F32, "lam"))
        pT = [T(kb.sb([128, 512], BF16, "pT")) for _ in range(3)]
        tmpf = [T(kb.sb([128, 512], F32, "tmpf")) for _ in range(2)]
        o0 = [T(kb.sb([128, 128], F32, "o0")) for _ in range(4)]
        o1 = [T(kb.sb([128, 128], F32, "o1")) for _ in range(2)]
        rec = [T(kb.sb([128, 2], F32, "rec")) for _ in range(2)]
        ssq = [T(kb.sb([128, 1], F32, "ssq")) for _ in range(2)]
        junk = T(kb.sb([128, 128], F32, "junk"))
        ost = [T(kb.sb([128, 512], BF16, "ost")) for _ in range(2)]
        kb.dma("sp", lp[:], dr["diff_lambda"][l:l + 1].rearrange("o a d -> o (a d)").partition_broadcast(128),
               [], [lp.res])
        kb.dma("sp", cb[:], dr["c_bias_far"].partition_broadcast(128), [], [cb.res])
        kb.op("dve", [lp.res], [lp.res],
              lambda e: e.tensor_tensor(out=lp[:, 0:64], in0=lp[:, 0:64], in1=lp[:, 64:128], op=ALU.mult))
        kb.op("dve", [lp.res], [lp.res],
              lambda e: e.tensor_tensor(out=lp[:, 128:192], in0=lp[:, 128:192], in1=lp[:, 192:256], op=ALU.mult))
        kb.op("dve", [lp.res], [lam.res], lambda e: e.reduce_sum(out=lam[:, 0:1], in_=lp[:, 0:64], axis=AX.X))
        kb.op("dve", [lp.res], [lam.res], lambda e: e.reduce_sum(out=lam[:, 1:2], in_=lp[:, 128:192], axis=AX.X))
        kb.op("act", [lam.res], [lam.res], lambda e: e.activation(out=lam[:, 0:2], in_=lam[:, 0:2], func=AF.Exp))
        kb.op("dve", [lam.res], [lam.res],
              lambda e: e.tensor_tensor(out=lam[:, 2:3], in0=lam[:, 1:2], in1=lam[:, 0:1], op=ALU.subtract))
        kb.op("dve", [lam.res], [lam.res],
              lambda e: e.tensor_scalar(out=lam[:, 2:3], in0=lam[:, 2:3], scalar1=-lam_init, scalar2=None,
                                        op0=ALU.add))
        kb.op("dve", [], [V.res], lambda e: e.memset(V[:, :, 128:129], 1.0))
        npt = 0
        nev = 0
        for h in range(4):
            kb.dma("sp", QT[:], dr["qk_scr"][h], [g.scr_res["qk"]], [QT.res])
            kb.dma("sp", KT[:], dr["qk_scr"][4 + h], [g.scr_res["qk"]], [KT.res])
            kb.dma("sp", V[:, :, 0:128], dr["dav_scr"][:, tsl(h)].rearrange("(kt p) c -> p kt c", p=128),
                   [g.scr_res["dav"]], [V.res])
            kb.dma("sp", biasT[:], dr["c_biasT"][h].rearrange("a k q -> k a q"), [], [biasT.res])
            for qb in range(NQB):
                for m in range(2):
                    ms = slice(m * 64, (m + 1) * 64)
                    O = [g.ps[4 + qs] for qs in range(4)]
                    nk = 4 * (qb + 1)

                    def qk(kt):
                        sT = nps(g)
                        kb.op("pe", [KT.res, QT.res], [sT.res],
                              lambda e: e.matmul(sT[:], lhsT=KT[ms, tsl(kt)], rhs=QT[ms, tsl(qb, 512)],
                                                 start=True, stop=True))
                        return sT

                    sT_next = qk(0)
                    for kt in range(nk):
                        sT = sT_next
                        if kt + 1 < nk:
                            sT_next = qk(kt + 1)
                        dmin = qb * 4 - kt
                        p_ = pT[npt % 3]
                        npt += 1
                        if dmin >= 2:
                            kb.op("act", [sT.res, cb.res], [p_.res],
                                  lambda e: e.activation(out=p_[:], in_=sT[:], func=AF.Exp,
                                                         bias=cb[:, h:h + 1], scale=1.0))
                        else:
                            q0 = max(0, -dmin) * 128
                            tf = tmpf[kt % 2]
                            kb.op("dve", [sT.res, biasT.res], [tf.res],
                                  lambda e: e.tensor_tensor(out=tf[:, q0:512], in0=sT[:, q0:512],
                                                            in1=biasT[:, dmin + 3, q0:512], op=ALU.add))
                            kb.op("act", [tf.res], [p_.res],
                                  lambda e: e.activation(out=p_[:, q0:512], in_=tf[:, q0:512], func=AF.Exp))
                        for qs in range(4):
                            dl = dmin + qs
                            if dl < 0:
                                continue
                            kb.op("pe", [p_.res, V.res], [O[qs].res],
                                  lambda e: e.matmul(O[qs][:, 0:129], lhsT=p_[:, tsl(qs)], rhs=V[:, kt, :],
                                                     start=(kt == 0), stop=(dl == 0)), inc=(dl == 0))
                    for qs in range(4):
                        r = rec[qs % 2]
                        kb.op("dve", [O[qs].res], [r.res],
                              lambda e: e.reciprocal(out=r[:, 0:1], in_=O[qs][:, 128:129]))
                        if m == 0:
                            kb.op("dve", [O[qs].res, r.res], [o0[qs].res],
                                  lambda e: e.tensor_scalar(out=o0[qs][:], in0=O[qs][:, 0:128], scalar1=r[:, 0:1],
                                                            scalar2=None, op0=ALU.mult))
                        else:
                            oo = o1[qs % 2]
                            sq = ssq[qs % 2]
                            kb.op("dve", [r.res, lam.res], [r.res],
                                  lambda e: e.tensor_tensor(out=r[:, 1:2], in0=r[:, 0:1], in1=lam[:, 2:3], op=ALU.mult))
                            kb.op("dve", [O[qs].res, r.res, o0[qs].res], [oo.res],
                                  lambda e: e.scalar_tensor_tensor(out=oo[:], in0=O[qs][:, 0:128], scalar=r[:, 1:2],
                                                                   in1=o0[qs][:], op0=ALU.mult, op1=ALU.add))
                            kb.op("act", [oo.res], [junk.res, sq.res],
                                  lambda e: e.activation(out=junk[:], in_=oo[:], func=AF.Square, accum_out=sq[:]))
                            rstd_from_ss(kb, sq, 128, None)
                            kb.op("dve", [oo.res, sq.res], [oo.res],
                                  lambda e: e.tensor_scalar(out=oo[:], in0=oo[:], scalar1=sq[:, 0:1],
                                                            scalar2=(1.0 - lam_init), op0=ALU.mult, op1=ALU.mult))
                            pt_ = nps(g)
                            kb.op("pe", [oo.res, g.ident.res], [pt_.res],
                                  lambda e: e.transpose(pt_[:, 0:128], oo[:], g.ident[:]))
                            os_ = ost[qb % 2]
                            kb.op("dve", [pt_.res], [os_.res],
                                  lambda e: e.tensor_copy(out=os_[:, tsl(qs)], in_=pt_[:, 0:128]))
                    if m == 1:
                        os_ = ost[qb % 2]
                        kb.dma("sp", dr["o_scr"][h, :, tsl(qb, 512)], os_[:], [os_.res], [g.scr_res["o"]])
    g.psn = 8
    barrier(kb)


def emit_mlstm(kb, g, dr, l, S):
    nc = kb.nc
    NCH = S // 128
    SEG = min(2048, S)
    KSC = 128 ** -0.5
    with kb.scope():
        qT = T(kb.sb([128, S], BF16, "qT"))
        kT = T(kb.sb([128, S], BF16, "kT"))
        ktok = T(kb.sb([128, NCH, 128], BF16, "ktok"))
        vpp = T(kb.sb([128, NCH, 129], BF16, "vpp"))
        raw = T(kb.sb([128, SEG + 3], F32, "raw"))
        yc = T(kb.sb([128, SEG], F32, "yc"))
        cw = T(kb.sb([128, 4, 8], F32, "cw"))
        gif = T(kb.sb([128, NCH, 8], F32, "gif"))
        gbb = T(kb.sb([128, 8], F32, "gbb"))
        spt = T(kb.sb([128, NCH, 4], F32, "spt"))
        tot = T(kb.sb([128, NCH, 4], F32, "tot"))
        A = T(kb.sb([128, NCH, 4], F32, "A"))
        R = T(kb.sb([128, NCH, 4], F32, "R"))
        EL = T(kb.sb([128, NCH, 4], F32, "EL"))
        vst = [T(kb.sb([128, 8, 128], F32, "vst")) for _ in range(2)]
        ost_ = [T(kb.sb([128, 8, 128], F32, "osg")) for _ in range(2)]
        sTm = [T(kb.sb([128, 128], BF16, "sTm")) for _ in range(2)]
        Cf = T(kb.sb([128, 129], F32, "Cf"))
        Cb = [T(kb.sb([128, 129], BF16, "Cb")) for _ in range(2)]
        dd = [T(kb.sb([128, 4], F32, "dd")) for _ in range(2)]
        ho = [T(kb.sb([128, 128], F32, "ho")) for _ in range(2)]
        ost = [T(kb.sb([128, 512], BF16, "ost")) for _ in range(2)]
        for j in range(4):
            kb.dma("sp", cw[:, j, :], dr["ml_conv"][l, j].rearrange("(t p) -> p t", p=128), [], [cw.res],
                   allow_slow_non_contiguous=True)
        kb.dma("sp", gbb[:], dr["ml_gate_b"][l:l + 1].rearrange("o a h -> o (a h)").partition_broadcast(128),
               [], [gbb.res])
        kb.dma("sp", gif[:], dr["mlif_scr"].rearrange("(c p) j -> p c j", p=128), [g.scr_res["mlif"]], [gif.res])
        for j in range(8):
            kb.op("dve", [gif.res, gbb.res], [gif.res],
                  lambda e: e.tensor_scalar(out=gif[:, :, j], in0=gif[:, :, j], scalar1=gbb[:, j:j + 1],
                                            scalar2=None, op0=ALU.add))
        kb.op("act", [gif.res], [spt.res],
              lambda e: e.activation(out=spt[:], in_=gif[:, :, 4:8], func=AF.Exp, scale=-1.0))
        kb.op("act", [spt.res], [spt.res],
              lambda e: e.activation(out=spt[:], in_=spt[:], func=AF.Ln, bias=1.0, scale=1.0))
        pc = nps(g)
        ptot = nps(g)
        spf = spt[:].rearrange("p c h -> p (c h)")
        kb.op("pe", [spt.res, g.tri.res], [pc.res],
              lambda e: e.matmul(pc[:, 0:NCH * 4], lhsT=g.tri[:], rhs=spf, start=True, stop=True))
        kb.op("pe", [spt.res, g.ones.res], [ptot.res],
              lambda e: e.matmul(ptot[:, 0:NCH * 4], lhsT=g.ones[:], rhs=spf, start=True, stop=True))
        totf = tot[:].rearrange("p c h -> p (c h)")
        Af = A[:].rearrange("p c h -> p (c h)")
        Rf = R[:].rearrange("p c h -> p (c h)")
        ELf = EL[:].rearrange("p c h -> p (c h)")
        kb.op("dve", [ptot.res], [tot.res], lambda e: e.tensor_copy(out=totf, in_=ptot[:, 0:NCH * 4]))
        kb.op("dve", [pc.res, tot.res], [R.res],
              lambda e: e.tensor_tensor(out=Rf, in0=pc[:, 0:NCH * 4], in1=totf, op=ALU.subtract))
        kb.op("dve", [R.res, gif.res], [A.res],
              lambda e: e.tensor_tensor(out=A[:], in0=R[:], in1=gif[:, :, 0:4], op=ALU.add))
        kb.op("act", [A.res], [A.res], lambda e: e.activation(out=Af, in_=Af, func=AF.Exp))
        kb.op("act", [R.res], [R.res], lambda e: e.activation(out=Rf, in_=Rf, func=AF.Exp, scale=-1.0))
        kb.op("act", [tot.res], [EL.res], lambda e: e.activation(out=ELf, in_=totf, func=AF.Exp, scale=-1.0))

        def conv_seg(tile_idx, s0, is_k):
            n = min(SEG, S - s0)
            src = dr["mlqk_scr"][tile_idx]
            if s0 == 0:
                kb.op("dve", [], [raw.res], lambda e: e.memset(raw[:, 0:3], 0.0))
                kb.dma("sp", raw[:, 3:3 + n], src[:, 0:n], [g.scr_res["mlqk"]], [raw.res])
            else:
                kb.dma("sp", raw[:, 0:3 + n], src[:, s0 - 3:s0 + n], [g.scr_res["mlqk"]], [raw.res])
            kb.op("dve", [raw.res, cw.res], [yc.res],
                  lambda e: e.tensor_scalar(out=yc[:, 0:n], in0=raw[:, 3:3 + n], scalar1=cw[:, 3, tile_idx:tile_idx + 1],
                                            scalar2=None, op0=ALU.mult))
            for j in (2, 1, 0):
                kb.op("dve", [raw.res, cw.res, yc.res], [yc.res],
                      lambda e: e.scalar_tensor_tensor(out=yc[:, 0:n], in0=raw[:, j:j + n],
                                                       scalar=cw[:, j, tile_idx:tile_idx + 1], in1=yc[:, 0:n],
                                                       op0=ALU.mult, op1=ALU.add))
            if not is_k:
                kb.op("act", [yc.res], [qT.res],
                      lambda e: e.activation(out=qT[:, s0:s0 + n], in_=yc[:, 0:n], func=AF.Silu))
            else:
                kb.op("act", [yc.res], [yc.res],
                      lambda e: e.activation(out=yc[:, 0:n], in_=yc[:, 0:n], func=AF.Silu))
                kb.op("dve", [yc.res], [kT.res],
                      lambda e: e.tensor_scalar(out=kT[:, s0:s0 + n], in0=yc[:, 0:n], scalar1=KSC, scalar2=None,
                                                op0=ALU.mult))
                for c4 in range(n // 512):
                    p = nps(g)
                    for cc in range(4):
                        kb.op("pe", [yc.res, g.ident.res], [p.res],
                              lambda e: e.transpose(p[:, tsl(cc)], yc[:, c4 * 512 + cc * 128: c4 * 512 + (cc + 1) * 128],
                                                    g.ident[:]), inc=(cc == 3))
                    c0 = s0 // 128 + c4 * 4
                    kb.op("act", [p.res], [ktok.res],
                          lambda e: e.activation(out=ktok[:, c0:c0 + 4, :].rearrange("p c d -> p (c d)"), in_=p[:],
                                                 func=AF.Copy, scale=KSC))

        for h in range(4):
            for s0 in range(0, S, SEG):
                conv_seg(h, s0, False)
            for s0 in range(0, S, SEG):
                conv_seg(4 + h, s0, True)
            for c8 in range(NCH // 8 if NCH >= 8 else 1):
                ncg = min(8, NCH)
                vs = vst[c8 % 2]
                kb.dma("sp", vs[:, 0:ncg, :],
                       dr["mlv_scr"][c8 * 1024: c8 * 1024 + ncg * 128, tsl(h)].rearrange("(c p) d -> p c d", p=128),
                       [g.scr_res["mlv"]], [vs.res])
                for cc in range(ncg):
                    c = c8 * 8 + cc
                    kb.op("dve", [vs.res, A.res], [vpp.res],
                          lambda e: e.tensor_scalar(out=vpp[:, c, 0:128], in0=vs[:, cc, :], scalar1=A[:, c, h:h + 1],
                                                    scalar2=None, op0=ALU.mult))
            kb.op("dve", [A.res], [vpp.res], lambda e: e.tensor_copy(out=vpp[:, :, 128], in_=A[:, :, h]))
            for c in range(NCH):
                cs_ = tsl(c)
                if c % 8 == 0:
                    ncg = min(8, NCH)
                    og = ost_[(c // 8) % 2]
                    kb.dma("sp", og[:, 0:ncg, :],
                           dr["mlo_scr"][c * 128: (c + ncg) * 128, tsl(h)].rearrange("(c p) d -> p c d", p=128),
                           [g.scr_res["mlo"]], [og.res])
                    kb.op("act", [og.res], [og.res],
                          lambda e: e.activation(out=og[:, 0:ncg, :], in_=og[:, 0:ncg, :], func=AF.Sigmoid))
                og = ost_[(c // 8) % 2]
                ps_s = nps(g)
                kb.op("pe", [kT.res, qT.res], [ps_s.res],
                      lambda e: e.matmul(ps_s[:, 0:128], lhsT=kT[:, cs_], rhs=qT[:, cs_], start=True, stop=True))
                sm = sTm[c % 2]
                kb.op("dve", [ps_s.res, g.tri.res], [sm.res],
                      lambda e: e.tensor_tensor(out=sm[:], in0=ps_s[:, 0:128], in1=g.tri[:], op=ALU.mult))
                cb_ = Cb[c % 2]
                if c > 0:
                    kb.op("dve", [Cf.res, EL.res], [cb_.res],
                          lambda e: e.tensor_scalar(out=cb_[:], in0=Cf[:], scalar1=EL[:, c, h:h + 1], scalar2=None,
                                                    op0=ALU.mult))
                pn = nps(g)
                kb.op("pe", [sm.res, vpp.res], [pn.res],
                      lambda e: e.matmul(pn[:, 0:129], lhsT=sm[:], rhs=vpp[:, c, :], start=True, stop=(c == 0)),
                      inc=(c == 0))
                if c > 0:
                    kb.op("pe", [qT.res, cb_.res], [pn.res],
                          lambda e: e.matmul(pn[:, 0:129], lhsT=qT[:, cs_], rhs=cb_[:], start=False, stop=True))
                pd = nps(g)
                kb.op("pe", [ktok.res, vpp.res], [pd.res],
                      lambda e: e.matmul(pd[:, 0:129], lhsT=ktok[:, c, :], rhs=vpp[:, c, :], start=True, stop=True))
                if c == 0:
                    kb.op("dve", [pd.res], [Cf.res], lambda e: e.tensor_copy(out=Cf[:], in_=pd[:, 0:129]))
                else:
                    kb.op("dve", [pd.res, Cf.res, EL.res], [Cf.res],
                          lambda e: e.scalar_tensor_tensor(out=Cf[:], in0=Cf[:], scalar=EL[:, c, h:h + 1],
                                                           in1=pd[:, 0:129], op0=ALU.mult, op1=ALU.add))
                d_ = dd[c % 2]
                kb.op("dve", [pn.res, R.res], [d_.res],
                      lambda e: e.tensor_tensor(out=d_[:, 0:1], in0=pn[:, 128:129], in1=R[:, c, h:h + 1], op=ALU.mult))
                kb.op("dve", [d_.res], [d_.res],
                      lambda e: e.scalar_tensor_tensor(out=d_[:, 1:2], in0=d_[:, 0:1], scalar=-1.0, in1=d_[:, 0:1],
                                                       op0=ALU.mult, op1=ALU.max))
                kb.op("dve", [d_.res], [d_.res],
                      lambda e: e.tensor_scalar(out=d_[:, 1:2], in0=d_[:, 1:2], scalar1=1.0, scalar2=None,
                                                op0=ALU.max))
                kb.op("dve", [d_.res], [d_.res], lambda e: e.reciprocal(out=d_[:, 2:3], in_=d_[:, 1:2]))
                kb.op("dve", [d_.res, R.res], [d_.res],
                      lambda e: e.tensor_tensor(out=d_[:, 3:4], in0=d_[:, 2:3], in1=R[:, c, h:h + 1], op=ALU.mult))
                ho_ = ho[c % 2]
                kb.op("dve", [pn.res, d_.res, og.res], [ho_.res],
                      lambda e: e.scalar_tensor_tensor(out=ho_[:], in0=pn[:, 0:128], scalar=d_[:, 3:4],
                                                       in1=og[:, c % 8, :], op0=ALU.mult, op1=ALU.mult))
                pt_ = nps(g)
                kb.op("pe", [ho_.res, g.ident.res], [pt_.res],
                      lambda e: e.transpose(pt_[:, 0:128], ho_[:], g.ident[:]))
                os_ = ost[(c // 4) % 2]
                kb.op("act", [pt_.res], [os_.res],
                      lambda e: e.activation(out=os_[:, tsl(c % 4)], in_=pt_[:, 0:128], func=AF.Copy))
                if c % 4 == 3:
                    kb.dma("sp", dr["o_scr"][4 + h, :, tsl(c // 4, 512)], os_[:], [os_.res], [g.scr_res["o"]])
    barrier(kb)


def emit_gla(kb, g, dr, l, S):
    nc = kb.nc
    NCH = S // 128
    SEG = min(2048, S)
    with kb.scope():
        qtT = T(kb.sb([128, S], BF16, "qtT"))
        ktT = T(kb.sb([128, S], BF16, "ktT"))
        ktok = T(kb.sb([128, NCH, 128], BF16, "gktok"))
        vb = [T(kb.sb([128, NCH, 128], BF16, "gvb")) for _ in range(2)]
        ELt = T(kb.sb([128, NCH], F32, "ELt"))
        wa2 = T(kb.sb([16, 256], F32, "wa2"))
        nba = T(kb.sb([128, 2], F32, "nba"))
        glaT = T(kb.sb([16, SEG], F32, "glaT"))
        spg = T(kb.sb([128, SEG], F32, "spg"))
        csg = T(kb.sb([128, SEG], F32, "csg"))
        rm = T(kb.sb([128, SEG], F32, "rm"))
        Ee = T(kb.sb([128, SEG], F32, "Ee"))
        rawq = T(kb.sb([128, SEG], F32, "rawq"))
        ktf = T(kb.sb([128, SEG], F32, "ktf"))
        srg = [T(kb.sb([128, 8, 128], F32, "srg")) for _ in range(2)]
        sTm = [T(kb.sb([128, 128], BF16, "gsTm")) for _ in range(2)]
        U = T(kb.sb([128, 128], F32, "U"))
        Sb = [T(kb.sb([128, 128], BF16, "Sb")) for _ in range(2)]
        ssq = [T(kb.sb([128, 1], F32, "gss")) for _ in range(2)]
        junk = T(kb.sb([128, 128], F32, "gjunk"))
        ho = [T(kb.sb([128, 128], F32, "gho")) for _ in range(2)]
        ost = [[T(kb.sb([128, 512], BF16, "gost")) for _ in range(2)] for _ in range(2)]
        kb.dma("sp", wa2[:], dr["gla_wa2"][l], [], [wa2.res])
        kb.dma("sp", nba[:], dr["gla_ba"][l].rearrange("(t p) -> p t", p=128), [], [nba.res],
               allow_slow_non_contiguous=True)
        kb.op("dve", [nba.res], [nba.res],
              lambda e: e.tensor_scalar(out=nba[:], in0=nba[:], scalar1=-1.0, scalar2=None, op0=ALU.mult))
        kb.op("dve", [], [rm.res], lambda e: e.memset(rm[:], 1.0))
        kb.op("dve", [rm.res], [rm.res],
              lambda e: e.memset(rm[:].rearrange("p (c t) -> p c t", t=128)[:, :, 0:1], 0.0))
        for hp in range(2):
            for s0 in range(0, S, SEG):
                n = min(SEG, S - s0)
                kb.dma("sp", glaT[:, 0:n], dr["gla_scr"][:, s0:s0 + n], [g.scr_res["gla"]], [glaT.res])
                kb.dma("sp", rawq[:, 0:n], dr["glqk_scr"][hp, :, s0:s0 + n], [g.scr_res["glqk"]], [rawq.res])
                kb.dma("sp", ktf[:, 0:n], dr["glqk_scr"][2 + hp, :, s0:s0 + n], [g.scr_res["glqk"]], [ktf.res])
                for b5 in range(n // 512):
                    pz = nps(g)
                    kb.op("pe", [wa2.res, glaT.res], [pz.res],
                          lambda e: e.matmul(pz[:], lhsT=wa2[:, tsl(hp)], rhs=glaT[:, tsl(b5, 512)],
                                             start=True, stop=True))
                    kb.op("act", [pz.res, nba.res], [spg.res],
                          lambda e: e.activation(out=spg[:, tsl(b5, 512)], in_=pz[:], func=AF.Exp,
                                                 bias=nba[:, hp:hp + 1], scale=-1.0))
                kb.op("act", [spg.res], [spg.res],
                      lambda e: e.activation(out=spg[:, 0:n], in_=spg[:, 0:n], func=AF.Ln, bias=1.0, scale=1.0))
                kb.op("dve", [spg.res, rm.res], [csg.res],
                      lambda e: e.tensor_tensor_scan(out=csg[:, 0:n], data0=rm[:, 0:n], data1=spg[:, 0:n],
                                                     initial=0.0, op0=ALU.mult, op1=ALU.add))
                kb.op("act", [csg.res], [Ee.res],
                      lambda e: e.activation(out=Ee[:, 0:n], in_=csg[:, 0:n], func=AF.Exp, scale=-1.0 / 16.0))
                kb.op("dve", [rawq.res, Ee.res], [qtT.res],
                      lambda e: e.tensor_tensor(out=qtT[:, s0:s0 + n], in0=rawq[:, 0:n], in1=Ee[:, 0:n], op=ALU.mult))
                c0 = s0 // 128
                kb.op("dve", [Ee.res], [ELt.res],
                      lambda e: e.tensor_copy(out=ELt[:, c0:c0 + n // 128],
                                              in_=Ee[:, 0:n].rearrange("p (c t) -> p c t", t=128)[:, :, 127]))
                kb.op("act", [csg.res], [Ee.res],
                      lambda e: e.activation(out=Ee[:, 0:n], in_=csg[:, 0:n], func=AF.Exp, scale=1.0 / 16.0))
                kb.op("dve", [ktf.res, Ee.res], [ktf.res],
                      lambda e: e.tensor_tensor(out=ktf[:, 0:n], in0=ktf[:, 0:n], in1=Ee[:, 0:n], op=ALU.mult))
                kb.op("act", [ktf.res], [ktT.res],
                      lambda e: e.activation(out=ktT[:, s0:s0 + n], in_=ktf[:, 0:n], func=AF.Copy))
                for c4 in range(n // 512):
                    p = nps(g)
                    for cc in range(4):
                        kb.op("pe", [ktf.res, g.ident.res], [p.res],
                              lambda e: e.transpose(p[:, tsl(cc)], ktf[:, c4 * 512 + cc * 128: c4 * 512 + (cc + 1) * 128],
                                                    g.ident[:]), inc=(cc == 3))
                    cc0 = c0 + c4 * 4
                    kb.op("dve", [p.res], [ktok.res],
                          lambda e: e.tensor_copy(out=ktok[:, cc0:cc0 + 4, :].rearrange("p c d -> p (c d)"), in_=p[:]))
            for hh in range(2):
                hd = hp * 2 + hh
                kb.dma("pool", vb[hh][:], dr["glv_scr"][:, tsl(hd)].rearrange("(c p) d -> p c d", p=128),
                       [g.scr_res["glv"]], [vb[hh].res])
            for c in range(NCH):
                cs_ = tsl(c)
                for hh in range(2):
                    hd = hp * 2 + hh
                    hs = slice(hh * 64, (hh + 1) * 64)
                    if c % 8 == 0:
                        ncg = min(8, NCH)
                        sr = srg[hh]
                        kb.dma("sp", sr[:, 0:ncg, :],
                               dr["glr_scr"][c * 128:(c + ncg) * 128, tsl(hd)].rearrange("(c p) d -> p c d", p=128),
                               [g.scr_res["glr"]], [sr.res])
                        kb.op("act", [sr.res], [sr.res],
                              lambda e: e.activation(out=sr[:, 0:ncg, :], in_=sr[:, 0:ncg, :], func=AF.Silu))
                    sr = srg[hh]
                    ps_s = nps(g)
                    kb.op("pe", [ktT.res, qtT.res], [ps_s.res],
                          lambda e: e.matmul(ps_s[:, 0:128], lhsT=ktT[hs, cs_], rhs=qtT[hs, cs_], start=True, stop=True))
                    sm = sTm[hh]
                    kb.op("dve", [ps_s.res, g.tri.res], [sm.res],
                          lambda e: e.tensor_tensor(out=sm[:], in0=ps_s[:, 0:128], in1=g.tri[:], op=ALU.mult))
                    sb_ = Sb[hh]
                    if c > 0:
                        kb.op("dve", [U.res, ELt.res], [sb_.res],
                              lambda e: e.tensor_scalar(out=sb_[hs, :], in0=U[hs, :], scalar1=ELt[hs, c - 1:c],
                                                        scalar2=None, op0=ALU.mult))
                    po = nps(g)
                    kb.op("pe", [sm.res, vb[hh].res], [po.res],
                          lambda e: e.matmul(po[:, 0:128], lhsT=sm[:], rhs=vb[hh][:, c, :], start=True, stop=(c == 0)),
                          inc=(c == 0))
                    if c > 0:
                        kb.op("pe", [qtT.res, sb_.res], [po.res],
                              lambda e: e.matmul(po[:, 0:128], lhsT=qtT[hs, cs_], rhs=sb_[hs, :], start=False, stop=True))
                    pd = nps(g)
                    kb.op("pe", [ktok.res, vb[hh].res], [pd.res],
                          lambda e: e.matmul(pd[:, 0:128], lhsT=ktok[:, c, :], rhs=vb[hh][:, c, :], start=True, stop=True))
                    if c == 0:
                        kb.op("dve", [pd.res], [U.res], lambda e: e.tensor_copy(out=U[hs, :], in_=pd[hs, 0:128]))
                    else:
                        kb.op("dve", [pd.res, U.res, ELt.res], [U.res],
                              lambda e: e.scalar_tensor_tensor(out=U[hs, :], in0=U[hs, :], scalar=ELt[hs, c - 1:c],
                                                               in1=pd[hs, 0:128], op0=ALU.mult, op1=ALU.add))
                    sq = ssq[hh]
                    kb.op("act", [po.res], [junk.res, sq.res],
                          lambda e: e.activation(out=junk[:], in_=po[:, 0:128], func=AF.Square, accum_out=sq[:]))
                    rstd_from_ss(kb, sq, 128, None)
                    ho_ = ho[hh]
                    kb.op("dve", [po.res, sq.res, sr.res], [ho_.res],
                          lambda e: e.scalar_tensor_tensor(out=ho_[:], in0=po[:, 0:128], scalar=sq[:, 0:1],
                                                           in1=sr[:, c % 8, :], op0=ALU.mult, op1=ALU.mult))
                    pt_ = nps(g)
                    kb.op("pe", [ho_.res, g.ident.res], [pt_.res],
                          lambda e: e.transpose(pt_[:, 0:128], ho_[:], g.ident[:]))
                    os_ = ost[hh][(c // 4) % 2]
                    kb.op("act", [pt_.res], [os_.res],
                          lambda e: e.activation(out=os_[:, tsl(c % 4)], in_=pt_[:, 0:128], func=AF.Copy))
                    if c % 4 == 3:
                        kb.dma("sp", dr["o_scr"][8 + hd, :, tsl(c // 4, 512)], os_[:], [os_.res], [g.scr_res["o"]])
    barrier(kb)


def emit_s5(kb, g, dr, l, S):
    nc = kb.nc
    TB = 256
    NBK = S // TB
    g.psn = 4
    with kb.scope():
        def small(name, shape=(128, 16)):
            return T(kb.sb(list(shape), F32, name))

        rows = [T(kb.sb([16, 128], F32, "s5row")) for _ in range(3)]
        ldt2 = T(kb.sb([16, 2], F32, "ldt2"))
        are, aim, dtt = small("are"), small("aim"), small("dtt")
        mu, Lc, Ls, t1, t2, t3 = small("mu"), small("Lc"), small("Ls"), small("t1"), small("t2"), small("t3")
        Rc, Rs, nRs = small("Rc"), small("Rs"), small("nRs")
        fre, fim, nfim = small("fre"), small("fim"), small("nfim")
        Tc = T(kb.sb([128, 16, TB], F32, "Tc"))
        Ts = T(kb.sb([128, 16, TB], F32, "Ts"))
        Tm = T(kb.sb([128, 16, TB], F32, "Tm"))
        half = T(kb.sb([128, 2], F32, "half"))
        pm = T(kb.sb([128, 128], F32, "pm"))
        bre = T(kb.sb([128, 16, 16], F32, "bre"))
        bim = T(kb.sb([128, 16, 16], F32, "bim"))
        bbr = T(kb.sb([128, 16, 16], F32, "bbr"))
        bbi = T(kb.sb([128, 16, 16], F32, "bbi"))
        btmp = T(kb.sb([128, 16, 16], F32, "btmp"))
        BR = T(kb.sb([16, 16, 2, 128], BF16, "BR"))
        BI = T(kb.sb([16, 16, 2, 128], BF16, "BI"))
        cin = T(kb.sb([128, 128], F32, "cin"))
        CRE = T(kb.sb([128, 16, 128], BF16, "CRE"))
        NCRE = T(kb.sb([128, 16, 128], BF16, "NCRE"))
        NCIM = T(kb.sb([128, 16, 128], BF16, "NCIM"))
        drow = T(kb.sb([8, 128], F32, "drow"))
        dcol = T(kb.sb([128, 8], F32, "dcol"))
        gw = T(kb.sb([128, 4, 512], BF16, "gw"))
        kb.dma("sp", half[:], dr["c_half"], [], [half.res])
        kb.dma("sp", pm[:], dr["c_pm"], [], [pm.res])
        kb.dma("sp", rows[0][:], dr["s5_a_re"][l].rearrange("(pr j) p -> pr (j p)", j=2), [], [rows[0].res])
        kb.dma("sp", rows[1][:], dr["s5_a_im"][l].rearrange("(pr j) p -> pr (j p)", j=2), [], [rows[1].res])
        kb.dma("sp", ldt2[:], dr["s5_log_dt"][l].rearrange("(pr j) -> pr j", j=2), [], [ldt2.res])
        for j in range(2):
            kb.op("dve", [ldt2.res], [rows[2].res],
                  lambda e: e.tensor_copy(out=rows[2][:, j * 64:(j + 1) * 64], in_=ldt2[:, j:j + 1].to_broadcast([16, 64])))
        for src, dst in ((rows[0], are), (rows[1], aim), (rows[2], dtt)):
            p = nps(g)
            kb.op("pe", [src.res, g.ident.res], [p.res],
                  lambda e: e.transpose(p[:, 0:16], src[:], g.ident[0:16, 0:16]))
            kb.op("dve", [p.res], [dst.res], lambda e: e.tensor_copy(out=dst[:], in_=p[:, 0:16]))
        kb.op("act", [dtt.res], [dtt.res], lambda e: e.activation(out=dtt[:], in_=dtt[:], func=AF.Exp))

        def tt(out, a, b, op):
            kb.op("dve", [a.res, b.res], [out.res], lambda e: e.tensor_tensor(out=out[:], in0=a[:], in1=b[:], op=op))

        tt(mu, dtt, are, ALU.mult)
        kb.op("act", [mu.res], [mu.res], lambda e: e.activation(out=mu[:], in_=mu[:], func=AF.Exp))
        tt(t1, dtt, aim, ALU.mult)
        kb.op("act", [t1.res], [Ls.res], lambda e: e.activation(out=Ls[:], in_=t1[:], func=AF.Sin, scale=1.0 / 16.0))
        hpi = small("hpi", (128, 1))
        kb.op("dve", [], [hpi.res], lambda e: e.memset(hpi[:], math.pi / 2))
        kb.op("act", [t1.res, hpi.res], [Lc.res],
              lambda e: e.activation(out=Lc[:], in_=t1[:], func=AF.Sin, scale=1.0 / 16.0, bias=hpi[:, 0:1]))

        def csq(c, s):
            tt(t2, c, s, ALU.mult)
            tt(c, c, c, ALU.mult)
            tt(t3, s, s, ALU.mult)
            tt(c, c, t3, ALU.subtract)
            kb.op("dve", [t2.res], [s.res],
                  lambda e: e.tensor_scalar(out=s[:], in0=t2[:], scalar1=2.0, scalar2=None, op0=ALU.mult))

        for _ in range(4):
            csq(Lc, Ls)
        nr, ni, den = small("nr"), small("ni"), small("den")
        tt(nr, mu, Lc, ALU.mult)
        kb.op("dve", [nr.res], [nr.res],
              lambda e: e.tensor_scalar(out=nr[:], in0=nr[:], scalar1=-1.0, scalar2=None, op0=ALU.add))
        tt(ni, mu, Ls, ALU.mult)
        tt(den, are, are, ALU.mult)
        tt(t2, aim, aim, ALU.mult)
        tt(den, den, t2, ALU.add)
        kb.op("dve", [den.res], [den.res], lambda e: e.reciprocal(out=den[:], in_=den[:]))
        tt(fre, nr, are, ALU.mult)
        tt(t2, ni, aim, ALU.mult)
        tt(fre, fre, t2, ALU.add)
        tt(fre, fre, den, ALU.mult)
        tt(fim, ni, are, ALU.mult)
        tt(t2, nr, aim, ALU.mult)
        tt(fim, fim, t2, ALU.subtract)
        tt(fim, fim, den, ALU.mult)
        kb.op("dve", [], [Tc.res], lambda e: e.memset(Tc[:, :, 0:1], 1.0))
        kb.op("dve", [], [Ts.res], lambda e: e.memset(Ts[:, :, 0:1], 0.0))
        kb.op("dve", [mu.res], [Tm.res],
              lambda e: e.tensor_copy(out=Tm[:], in_=mu[:].unsqueeze(2).to_broadcast([128, 16, TB])))
        tmpa = T(kb.sb([128, 16, TB // 2], F32, "tmpa"))
        n = 1
        while n < TB:
            lcb = Lc[:].unsqueeze(2).to_broadcast([128, 16, n])
            lsb = Ls[:].unsqueeze(2).to_broadcast([128, 16, n])
            kb.op("dve", [Tc.res, Lc.res], [Tc.res],
                  lambda e: e.tensor_tensor(out=Tc[:, :, n:2 * n], in0=Tc[:, :, 0:n], in1=lcb, op=ALU.mult))
            kb.op("dve", [Ts.res, Ls.res], [tmpa.res],
                  lambda e: e.tensor_tensor(out=tmpa[:, :, 0:n], in0=Ts[:, :, 0:n], in1=lsb, op=ALU.mult))
            kb.op("dve", [Tc.res, tmpa.res], [Tc.res],
                  lambda e: e.tensor_tensor(out=Tc[:, :, n:2 * n], in0=Tc[:, :, n:2 * n], in1=tmpa[:, :, 0:n],
                                            op=ALU.subtract))
            kb.op("dve", [Ts.res, Lc.res], [Ts.res],
                  lambda e: e.tensor_tensor(out=Ts[:, :, n:2 * n], in0=Ts[:, :, 0:n], in1=lcb, op=ALU.mult))
            kb.op("dve", [Tc.res, Ls.res], [tmpa.res],
                  lambda e: e.tensor_tensor(out=tmpa[:, :, 0:n], in0=Tc[:, :, 0:n], in1=lsb, op=ALU.mult))
            kb.op("dve", [Ts.res, tmpa.res], [Ts.res],
                  lambda e: e.tensor_tensor(out=Ts[:, :, n:2 * n], in0=Ts[:, :, n:2 * n], in1=tmpa[:, :, 0:n],
                                            op=ALU.add))
            csq(Lc, Ls)
            n *= 2
        kb.op("dve", [Lc.res], [Rc.res], lambda e: e.tensor_copy(out=Rc[:], in_=Lc[:]))
        kb.op("dve", [Ls.res], [Rs.res], lambda e: e.tensor_copy(out=Rs[:], in_=Ls[:]))
        kb.op("dve", [Ls.res], [nRs.res],
              lambda e: e.tensor_scalar(out=nRs[:], in0=Ls[:], scalar1=-1.0, scalar2=None, op0=ALU.mult))
        kb.dma("sp", bre[:], dr["s5_b_re"][l].rearrange("(pr j) p c -> (j p) pr c", j=2), [], [bre.res])
        kb.dma("sp", bim[:], dr["s5_b_im"][l].rearrange("(pr j) p c -> (j p) pr c", j=2), [], [bim.res])
        frb = fre[:].unsqueeze(2).to_broadcast([128, 16, 16])
        fib = fim[:].unsqueeze(2).to_broadcast([128, 16, 16])

        def t3op(out, a, b_ap, breads, op):
            kb.op("dve", [a.res] + breads, [out.res], lambda e: e.tensor_tensor(out=out[:], in0=a[:], in1=b_ap, op=op))

        t3op(bbr, bre, frb, [fre.res], ALU.mult)
        t3op(btmp, bim, fib, [fim.res], ALU.mult)
        t3op(bbr, bbr, btmp[:], [btmp.res], ALU.subtract)
        t3op(bbi, bim, frb, [fre.res], ALU.mult)
        t3op(btmp, bre, fib, [fim.res], ALU.mult)
        t3op(bbi, bbi, btmp[:], [btmp.res], ALU.add)
        for (srcb, dstB) in ((bbr, BR), (bbi, BI)):
            for j in range(2):
                kb.op("dve", [srcb.res, half.res], [btmp.res],
                      lambda e: e.tensor_scalar(out=btmp[:], in0=srcb[:], scalar1=half[:, j:j + 1], scalar2=None,
                                                op0=ALU.mult))
                for p4 in range(4):
                    p = nps(g)
                    for q in range(4):
                        kb.op("pe", [btmp.res, g.ident.res], [p.res],
                              lambda e: e.transpose(p[0:16, tsl(q)], btmp[:, p4 * 4 + q, :], g.ident[:]), inc=(q == 3))
                    kb.op("act", [p.res], [dstB.res],
                          lambda e: e.activation(out=dstB[:, p4 * 4:p4 * 4 + 4, j, :],
                                                 in_=p[0:16, :].rearrange("c (q m) -> c q m", q=4), func=AF.Copy))
        for (t_, z_) in ((CRE, 0), (NCRE, 0), (NCIM, 0)):
            kb.op("pool", [], [t_.res], lambda e: e.memset(t_[:], 0.0))
        for (srcname, outs) in (("s5_c_re", ((CRE, 1.0), (NCRE, -1.0))), ("s5_c_im", ((NCIM, -1.0),))):
            cv = dr[srcname][l].rearrange("g c p -> (g c) p")
            for gt in range(4):
                kb.dma("sp", cin[:, 0:64], cv[tsl(gt), :], [], [cin.res])
                kb.dma("sp", cin[:, 64:128], cv[tsl(gt), :], [], [cin.res])
                kb.op("dve", [cin.res, pm.res], [cin.res],
                      lambda e: e.tensor_tensor(out=cin[:], in0=cin[:], in1=pm[:], op=ALU.mult))
                p = nps(g)
                kb.op("pe", [cin.res, g.ident.res], [p.res], lambda e: e.transpose(p[:, 0:128], cin[:], g.ident[:]))
                for (dstC, sgn) in outs:
                    for q in range(4):
                        kb.op("act", [p.res], [dstC.res],
                              lambda e: e.activation(out=dstC[:, gt * 4 + q, q * 32:(q + 1) * 32],
                                                     in_=p[:, q * 32:(q + 1) * 32], func=AF.Copy, scale=sgn))
        kb.dma("sp", drow[0:4, :], dr["s5_d"][l].rearrange("(t g) c -> t (g c)", t=4), [], [drow.res])
        kb.dma("sp", drow[4:8, :], dr["s5_glu_b"][l].rearrange("(t p) -> t p", p=128), [], [drow.res])
        p = nps(g)
        kb.op("pe", [drow.res, g.ident.res], [p.res], lambda e: e.transpose(p[:, 0:8], drow[:], g.ident[0:8, 0:8]))
        kb.op("dve", [p.res], [dcol.res], lambda e: e.tensor_copy(out=dcol[:], in_=p[:, 0:8]))
        kb.dma("pool", gw[:], dr["s5_glu_w"][l].rearrange("(kt p) n -> p kt n", p=128), [], [gw.res])

        umm = [T(kb.sb([16, 32, TB], BF16, "umm")) for _ in range(2)]
        usk = [T(kb.sb([128, 4, TB], F32, "usk")) for _ in range(2)]
        dmb = [[T(kb.sb([128, TB], F32, "dm")) for _ in range(4)] for _ in range(2)]
        winb = [[T(kb.sb([128, TB], F32, "win")) for _ in range(2)] for _ in range(2)]
        wrt = [T(kb.sb([128, TB], F32, "wrt")) for _ in range(2)]
        wit = [T(kb.sb([128, TB], F32, "wit")) for _ in range(2)]
        PP = [[T(kb.sb([128, TB], BF16, "PP")) for _ in range(4)] for _ in range(2)]
        w0r = T(kb.sb([128, 16], F32, "w0r"))
        w0i = T(kb.sb([128, 16], F32, "w0i"))
        cr = [T(kb.sb([128, 2], F32, "cr")) for _ in range(2)]
        ysb = [T(kb.sb([128, TB], F32, "ysb")) for _ in range(2)]
        gt1 = [T(kb.sb([128, TB], F32, "gt1")) for _ in range(2)]
        zf = T(kb.sb([128, 4, TB], F32, "zf"))
        zb = T(kb.sb([128, 4, TB], BF16, "zb"))
        sgl = [T(kb.sb([128, TB], F32, "sgl")) for _ in range(2)]
        ost = [T(kb.sb([128, TB], BF16, "s5ost")) for _ in range(2)]
        w0r_res = [Res() for _ in range(16)]
        w0i_res = [Res() for _ in range(16)]
        kb.op("dve", [], w0r_res, lambda e: e.memset(w0r[:], 0.0))
        kb.op("dve", [], w0i_res, lambda e: e.memset(w0i[:], 0.0))
        uv = dr["s5u_scr"]
        npp = 0
        for bk in range(NBK):
            ts_ = slice(bk * TB, (bk + 1) * TB)
            um = umm[bk % 2]
            us = usk[bk % 2]
            kb.dma("pool", um[:], uv.rearrange("t (g c) s -> c (t g) s", c=16)[:, :, ts_], [g.scr_res["s5u"]], [um.res])
            kb.dma("sp", us[:], uv.rearrange("t p s -> p t s")[:, :, ts_], [g.scr_res["s5u"]], [us.res])
            def stage1(pr):
                pbr = nps(g)
                pbi = nps(g)
                for (pb_, B_) in ((pbr, BR), (pbi, BI)):
                    for j in range(2):
                        kb.op("pe", [B_.res, um.res], [pb_.res],
                              lambda e: e.matmul(pb_[:, 0:TB], lhsT=B_[:, pr, j, :], rhs=um[:, 2 * pr + j, :],
                                                 start=(j == 0), stop=(j == 1)), inc=(j == 1))
                d0, d1, d2, d3 = dmb[pr % 2]
                for (o_, tab, src) in ((d0, Tc, pbr), (d1, Ts, pbi), (d2, Tc, pbi), (d3, Ts, pbr)):
                    kb.op("dve", [tab.res, src.res], [o_.res],
                          lambda e: e.tensor_tensor(out=o_[:], in0=tab[:, pr, :], in1=src[:, 0:TB], op=ALU.mult))
                wr_in, wi_in = winb[pr % 2]
                kb.op("pool", [d0.res, d1.res], [wr_in.res],
                      lambda e: e.tensor_tensor(out=wr_in[:], in0=d0[:], in1=d1[:], op=ALU.add))
                kb.op("pool", [d2.res, d3.res], [wi_in.res],
                      lambda e: e.tensor_tensor(out=wi_in[:], in0=d2[:], in1=d3[:], op=ALU.subtract))
                wr = wrt[pr % 2]
                wi = wit[pr % 2]
                kb.op("dve", [Tm.res, wr_in.res, w0r_res[pr]], [wr.res],
                      lambda e: e.tensor_tensor_scan(out=wr[:], data0=Tm[:, pr, :], data1=wr_in[:],
                                                     initial=w0r[:, pr:pr + 1], op0=ALU.mult, op1=ALU.add))
                kb.op("dve", [Tm.res, wi_in.res, w0i_res[pr]], [wi.res],
                      lambda e: e.tensor_tensor_scan(out=wi[:], data0=Tm[:, pr, :], data1=wi_in[:],
                                                     initial=w0i[:, pr:pr + 1], op0=ALU.mult, op1=ALU.add))
                c_ = cr[pr % 2]
                kb.op("act", [wr.res, Rc.res], [c_.res],
                      lambda e: e.activation(out=c_[:, 0:1], in_=wr[:, TB - 1:TB], func=AF.Copy, scale=Rc[:, pr:pr + 1]))
                kb.op("act", [wr.res, Rs.res], [c_.res],
                      lambda e: e.activation(out=c_[:, 1:2], in_=wr[:, TB - 1:TB], func=AF.Copy, scale=Rs[:, pr:pr + 1]))
                kb.op("act", [wi.res, nRs.res, c_.res], [w0r_res[pr]],
                      lambda e: e.activation(out=w0r[:, pr:pr + 1], in_=wi[:, TB - 1:TB], func=AF.Identity,
                                             scale=nRs[:, pr:pr + 1], bias=c_[:, 0:1]))
                kb.op("act", [wi.res, Rc.res, c_.res], [w0i_res[pr]],
                      lambda e: e.activation(out=w0i[:, pr:pr + 1], in_=wi[:, TB - 1:TB], func=AF.Identity,
                                             scale=Rc[:, pr:pr + 1], bias=c_[:, 1:2]))
                P = PP[pr % 2]
                for k_, (o_, tab, src) in enumerate(((P[0], Tc, wr), (P[1], Ts, wi), (P[2], Ts, wr), (P[3], Tc, wi))):
                    en = "dve" if k_ == 3 else "pool"
                    kb.op(en, [tab.res, src.res], [o_.res],
                          lambda e: e.tensor_tensor(out=o_[:], in0=tab[:, pr, :], in1=src[:], op=ALU.mult))

            def stage2(pr):
                P = PP[pr % 2]
                t = pr // 4
                py = g.ps[4 + t]
                for k_, (Cm, Pk) in enumerate(((CRE, P[0]), (NCRE, P[1]), (NCIM, P[2]), (NCIM, P[3]))):
                    first = (pr % 4 == 0 and k_ == 0)
                    last = (pr % 4 == 3 and k_ == 3)
                    kb.op("pe", [Cm.res, Pk.res], [py.res],
                          lambda e: e.matmul(py[:, 0:TB], lhsT=Cm[:, pr, :], rhs=Pk[:], start=first, stop=last),
                          inc=(k_ == 3))
                if pr % 4 == 3:
                    y = ysb[t % 2]
                    g1 = gt1[t % 2]
                    kb.op("dve", [us.res, dcol.res, py.res], [y.res],
                          lambda e: e.scalar_tensor_tensor(out=y[:], in0=us[:, t, :], scalar=dcol[:, t:t + 1],
                                                           in1=py[:, 0:TB], op0=ALU.mult, op1=ALU.add))
                    kb.op("act", [y.res], [g1.res], lambda e: e.activation(out=g1[:], in_=y[:], func=AF.Square))
                    kb.op("dve", [g1.res], [g1.res],
                          lambda e: e.tensor_scalar(out=g1[:], in0=g1[:], scalar1=0.0713548162726, scalar2=1.5957691216057308,
                                                    op0=ALU.mult, op1=ALU.add))
                    kb.op("dve", [g1.res, y.res], [g1.res],
                          lambda e: e.tensor_tensor(out=g1[:], in0=g1[:], in1=y[:], op=ALU.mult))
                    kb.op("act", [g1.res], [g1.res], lambda e: e.activation(out=g1[:], in_=g1[:], func=AF.Sigmoid))
                    kb.op("dve", [g1.res, y.res], [zf.res],
                          lambda e: e.tensor_tensor(out=zf[:, t, :], in0=g1[:], in1=y[:], op=ALU.mult))
                    kb.op("act", [zf.res], [zb.res], lambda e: e.activation(out=zb[:, t, :], in_=zf[:, t, :], func=AF.Copy))

            stage1(0)
            for pr in range(16):
                if pr + 1 < 16:
                    stage1(pr + 1)
                stage2(pr)
            for ct in range(4):
                pg = nps(g)
                for kt in range(4):
                    kb.op("pe", [gw.res, zb.res], [pg.res],
                          lambda e: e.matmul(pg[:, 0:TB], lhsT=gw[:, kt, tsl(ct)], rhs=zb[:, kt, :],
                                             start=(kt == 0), stop=(kt == 3)), inc=(kt == 3))
                s_ = sgl[ct % 2]
                kb.op("act", [pg.res, dcol.res], [s_.res],
                      lambda e: e.activation(out=s_[:], in_=pg[:, 0:TB], func=AF.Sigmoid, bias=dcol[:, 4 + ct:5 + ct],
                                             scale=1.0))
                os_ = ost[ct % 2]
                kb.op("dve", [s_.res, zf.res], [os_.res],
                      lambda e: e.tensor_tensor(out=os_[:], in0=zf[:, ct, :], in1=s_[:], op=ALU.mult))
                kb.dma("sp", dr["o_scr"][12 + ct, :, ts_], os_[:], [os_.res], [g.scr_res["o"]])
    g.psn = 8
    barrier(kb)


WEIGHT_SPECS = {
    "ada_w": (D, 6 * D), "ada_b": (6 * D,), "norm_g": (4, D), "w_in": (D, INW), "diff_lambda": (4, 64),
    "ml_conv": (4, 1024), "ml_gate_b": (2, 4), "gla_wa2": (16, 256), "gla_ba": (256,),
    "s5_a_re": (32, 64), "s5_a_im": (32, 64), "s5_log_dt": (32,), "s5_b_re": (32, 64, 16), "s5_b_im": (32, 64, 16),
    "s5_c_re": (32, 16, 64), "s5_c_im": (32, 16, 64), "s5_d": (32, 16), "s5_glu_w": (512, 512), "s5_glu_b": (512,),
    "w_branch": (4, 512, D), "w_gate": (4, D, D), "b_gate": (4, D), "w_out": (D, D),
    "ffn_w_in": (D, 2 * FFH), "ffn_w_out": (FFH, D),
}
CONST_SPECS = {"c_ident": (128, 128), "c_tri": (128, 128), "c_biasT": (4, 5, 128, 512), "c_bias_far": (1, 4),
               "c_half": (128, 2), "c_pm": (128, 128)}


SWAP_LANES = True
BF_WEIGHTS = ("w_in", "w_gate", "w_branch", "w_out", "ffn_w_in")


def emit_weight_cast(kb, g, dr, nc, NL):
    for name in BF_WEIGHTS:
        src = dr[name]
        shp = list(src.shape)
        dst = nc.dram_tensor(name + "_bf", shp, BF16, kind="Internal").ap()
        if len(shp) == 4:
            s2 = src.rearrange("l i r c -> (l i r) c")
            d2 = dst.rearrange("l i r c -> (l i r) c")
        else:
            s2 = src.rearrange("l r c -> (l r) c")
            d2 = dst.rearrange("l r c -> (l r) c")
        rows = s2.shape[0]
        step = 512
        for r0 in range(0, rows, step):
            r1 = min(rows, r0 + step)
            kb.dma("pool", d2[r0:r1, :], s2[r0:r1, :], [], [g.w_res])
        dr[name] = dst
    NH = FFH // 128
    dst = nc.dram_tensor("ffn_w_out_r", [NL, NDT, 128, NH, 128], BF16, kind="Internal").ap()
    for l in range(NL):
        srcv = dr["ffn_w_out"][l].rearrange("(kt p) n -> p kt n", p=128)
        for dt in range(NDT):
            kb.dma("pool", dst[l, dt], srcv[:, :, tsl(dt)], [], [g.w_res])
    dr["ffn_w_out_r"] = dst
    barrier(kb)


def build_program(S, NL, debug=(), phases="ABCD"):
    nc = bass.Bass("TRN2", target_bir_lowering=False)
    kb = KB(nc)
    if SWAP_LANES:
        kb.add_lane("sp", "pool", 4)
        kb.add_lane("w", "sp", 4)
    else:
        kb.add_lane("sp", "sp", 4)
        kb.add_lane("w", "pool", 4)
    kb.add_lane("pool", "pool", 4)
    g = G()
    dr = {}
    dr["x"] = nc.dram_tensor("x", [S, D], F32, kind="ExternalInput").ap()
    dr["c"] = nc.dram_tensor("c", [1, D], F32, kind="ExternalInput").ap()
    for k, shp in WEIGHT_SPECS.items():
        dr[k] = nc.dram_tensor(k, [NL] + list(shp), F32, kind="ExternalInput").ap()
    for k, shp in CONST_SPECS.items():
        dr[k] = nc.dram_tensor(k, list(shp), F32, kind="ExternalInput").ap()
    dr["xres"] = nc.dram_tensor("out", [S, D], F32, kind="ExternalOutput").ap()
    scr = {"mod": ([NL, 6 * D], F32), "hT": ([16, 128, S], BF16), "qk": ([8, 128, S], BF16), "dav": ([S, 512], BF16),
           "mlqk": ([8, 128, S], F32), "mlv": ([S, 512], F32), "mlo": ([S, 512], F32), "mlif": ([S, 8], F32),
           "glqk": ([4, 128, S], F32), "gla": ([16, S], F32), "glv": ([S, 512], F32), "glr": ([S, 512], F32),
           "s5u": ([4, 128, S], F32), "o": ([16, 128, S], BF16)}
    g.scr_res = {}
    for k, (shp, dt) in scr.items():
        kind = "ExternalOutput" if k in debug else "Internal"
        dr[k + "_scr"] = nc.dram_tensor(k + "_scr", shp, dt, kind=kind).ap()
        g.scr_res[k] = Res()
    g.mod_res = g.scr_res["mod"]
    g.x_res = Res()
    alloc_psum(kb, g)
    emit_consts(kb, g, dr)
    nchunk = max(1, S // 1024)
    rows = S // nchunk
    for i in range(nchunk):
        kb.dma("sp", dr["xres"][i * rows:(i + 1) * rows, :], dr["x"][i * rows:(i + 1) * rows, :], [], [g.x_res])
    emit_mods(kb, g, dr, NL)
    g.w_res = Res()
    emit_weight_cast(kb, g, dr, nc, NL)
    for l in range(NL):
        with kb.scope():
            L = emit_layer_consts(kb, g, dr, l)
            if "A" in phases:
                emit_phaseA(kb, g, dr, L, l, S)
            if "B" in phases or "a" in phases:
                emit_attn(kb, g, dr, l, S)
            if "B" in phases or "m" in phases:
                emit_mlstm(kb, g, dr, l, S)
            if "B" in phases or "g" in phases:
                emit_gla(kb, g, dr, l, S)
            if "B" in phases or "s" in phases:
                emit_s5(kb, g, dr, l, S)
            if "C" in phases:
                emit_phaseC1(kb, g, dr, L, l, S)
            if "D" in phases:
                emit_phaseC2(kb, g, dr, L, l, S)
        barrier(kb)
    barrier(kb)
    return nc


def t5_bucket(rel):
    n = np.maximum(-rel, 0)
    exact = N_BUCKETS // 2
    nf = np.maximum(n, 1).astype(np.float32)
    large = exact + (np.log(nf / exact) / math.log(MAX_DISTANCE / exact) * (N_BUCKETS - exact)).astype(np.int32)
    return np.where(n < exact, n, np.minimum(large, N_BUCKETS - 1))


def host_consts(rel_bias):
    rb = np.asarray(rel_bias, np.float32)
    k = np.arange(128)[:, None]
    q = np.arange(128)[None, :]
    biasT = np.empty((4, 5, 128, 512), np.float32)
    qq = np.arange(512)[None, :]
    for di, dmin in enumerate(range(-3, 2)):
        rel = k - (dmin * 128 + qq)
        idx = t5_bucket(rel)
        for h in range(4):
            tab = rb[:, h][idx]
            biasT[h, di] = np.where(rel <= 0, tab, np.float32(-30000.0))
    c = {
        "c_ident": np.eye(128, dtype=np.float32),
        "c_tri": np.triu(np.ones((128, 128), np.float32)),
        "c_biasT": biasT,
        "c_bias_far": np.ascontiguousarray(rb[N_BUCKETS - 1:N_BUCKETS, :]),
        "c_half": np.stack([(np.arange(128) < 64), (np.arange(128) >= 64)], axis=1).astype(np.float32),
    }
    g8 = (np.arange(128) // 16)[:, None]
    j = (np.arange(128) // 64)[None, :]
    c["c_pm"] = ((g8 % 2) == j).astype(np.float32)
    return c


_PROG = {}


def kernel(**inputs):
    x = np.asarray(inputs["x"], np.float32)
    B, S, _ = x.shape
    NL = np.asarray(inputs["ada_w"]).shape[0]
    key = (S, NL)
    if key not in _PROG:
        _PROG[key] = build_program(S, NL)
    nc = _PROG[key]
    consts = host_consts(inputs["rel_bias"])
    shared = {k: np.ascontiguousarray(np.asarray(inputs[k], np.float32)) for k in WEIGHT_SPECS}
    shared.update(consts)
    in_maps = []
    for b in range(B):
        m = dict(shared)
        m["x"] = np.ascontiguousarray(x[b])
        m["c"] = np.ascontiguousarray(np.asarray(inputs["c"], np.float32)[b:b + 1])
        in_maps.append(m)
    res = run_bass_kernel_spmd(nc, in_maps, core_ids=list(range(B)))
    return np.stack([np.asarray(r["out"], np.float32) for r in res.results], axis=0)
```

```python
import math
import contextlib
import numpy as np
import concourse.bass as bass
import concourse.mybir as mybir
from concourse.bass_utils import run_bass_kernel_spmd

F32 = mybir.dt.float32
BF16 = mybir.dt.bfloat16
AF = mybir.ActivationFunctionType
ALU = mybir.AluOpType
AX = mybir.AxisListType

D = 2048
NDT = 16
DEPTH = 4
FFH = 5632
INW = 5656
EPS = 1e-6
N_BUCKETS = 32
MAX_DISTANCE = 128


class Res:
    __slots__ = ("w", "r")

    def __init__(self):
        self.w = None
        self.r = {}


class KB:
    def __init__(self, nc):
        self.nc = nc
        self.eng = {"pe": nc.tensor, "act": nc.scalar, "dve": nc.vector, "pool": nc.gpsimd, "sp": nc.sync}
        self.sems = []
        self.own = {}
        self.cnt = {}
        self.known = {e: {} for e in self.eng}
        for e in ("pe", "act", "dve", "pool"):
            self.own[e] = self._newsem("c_" + e)
            self.cnt[e] = 0
        self.lanes = {}
        self.uid = 0
        self.stack = None

    def _newsem(self, name):
        self.sems.append(self.nc.alloc_semaphore(name))
        return len(self.sems) - 1

    def add_lane(self, name, eng, k):
        self.lanes[name] = {"eng": eng, "sems": [self._newsem(f"l_{name}{i}") for i in range(k)], "n": 0}

    def _deps(self, reads, writes):
        toks = []
        for r in reads:
            if r.w is not None:
                toks.append(r.w)
        for w in writes:
            if w.w is not None:
                toks.append(w.w)
            toks.extend(w.r.items())
        return toks

    def _wait(self, e, toks):
        kn = self.known[e]
        eng = self.eng[e]
        for si, v in toks:
            if e == "pe" and si == self.own.get("pe"):
                continue
            if kn.get(si, 0) < v:
                eng.wait_ge(self.sems[si], v)
                kn[si] = v

    def _mark(self, tok, reads, writes):
        si, v = tok
        for r in reads:
            if r.r.get(si, 0) < v:
                r.r[si] = v
        for w in writes:
            w.w = tok
            w.r = {}

    def op(self, e, reads, writes, fn, inc=True, fuse=None):
        toks = self._deps(reads, writes)
        if fuse is None:
            fuse = (e != "pe")
        last = None
        if fuse:
            kn = self.known[e]
            need = {}
            for si, v in toks:
                if kn.get(si, 0) < v and need.get(si, 0) < v:
                    need[si] = v
            if need:
                items = list(need.items())
                last = items[-1]
                toks = items[:-1]
            else:
                toks = []
        self._wait(e, toks)
        if last is not None:
            n0 = self.nc.n_instructions()
        ins = fn(self.eng[e])
        if last is not None:
            assert self.nc.n_instructions() - n0 == 1, "fused wait on a multi-instruction builder"
            ins._wait_ge(self.sems[last[0]], last[1])
            self.known[e][last[0]] = last[1]
        if inc:
            self.cnt[e] += 1
            ins.then_inc(self.sems[self.own[e]], 1)
            tok = (self.own[e], self.cnt[e])
        else:
            tok = (self.own[e], self.cnt[e] + 1)
        self._mark(tok, reads, writes)

    def dma(self, lane, out, in_, reads, writes, **kw):
        L = self.lanes[lane]
        e = L["eng"]
        n = L["n"]
        k = len(L["sems"])
        toks = self._deps(reads, writes)
        si = L["sems"][n % k]
        if n >= k:
            toks.append((si, 16 * (n // k)))
        self._wait(e, toks)
        self.eng[e].dma_start(out=out, in_=in_, **kw).then_inc(self.sems[si], 16)
        L["n"] = n + 1
        self._mark((si, 16 * (n // k + 1)), reads, writes)

    def final_wait(self, e, ress):
        toks = []
        for r in ress:
            if r.w is not None:
                toks.append(r.w)
        self._wait(e, toks)

    def sb(self, shape, dt, name=None):
        self.uid += 1
        nm = f"{name or 't'}_{self.uid}"
        if self.stack is not None:
            return self.stack.enter_context(self.nc.sbuf_tensor(nm, list(shape), dt))
        return self.nc.alloc_sbuf_tensor(nm, list(shape), dt)

    def scope(self):
        return _Scope(self)

    def dram(self, name, shape, dt, kind="Internal"):
        return self.nc.dram_tensor(name, list(shape), dt, kind=kind).ap()


class _Scope:
    def __init__(self, kb):
        self.kb = kb

    def __enter__(self):
        self.prev = self.kb.stack
        self.kb.stack = contextlib.ExitStack()
        return self

    def __exit__(self, *a):
        self.kb.stack.close()
        self.kb.stack = self.prev
        return False


class T:
    def __init__(self, t):
        self.t = t
        self.res = Res()

    def __getitem__(self, k):
        return self.t[k]


def barrier(kb):
    toks = []
    for e in ("pe", "act", "dve", "pool"):
        if kb.cnt[e] > 0:
            toks.append((kb.own[e], kb.cnt[e]))
    for L in kb.lanes.values():
        n = L["n"]
        k = len(L["sems"])
        for j in range(min(n, k)):
            m = n - 1 - j
            toks.append((L["sems"][m % k], 16 * (m // k + 1)))
    for e in kb.eng:
        kb._wait(e, toks)


class G:
    pass


def alloc_psum(kb, g):
    g.ps = []
    for i in range(8):
        g.ps.append(T(kb.nc.alloc_psum_tensor(f"psb{i}", [128, 512], F32)))
    g.psi = 0
    g.psn = 8


def nps(g):
    p = g.ps[g.psi % g.psn]
    g.psi += 1
    return p


def tsl(i, n=128):
    return slice(i * n, (i + 1) * n)


def emit_consts(kb, g, dr):
    nc = kb.nc
    g.ident = T(kb.sb([128, 128], F32, "ident"))
    g.tri = T(kb.sb([128, 128], F32, "tri"))
    g.ones = T(kb.sb([128, 128], F32, "ones"))
    g.tri_b = T(kb.sb([128, 128], BF16, "trib"))
    kb.dma("sp", g.ident[:], dr["c_ident"], [], [g.ident.res])
    kb.dma("sp", g.tri[:], dr["c_tri"], [], [g.tri.res])
    kb.op("dve", [], [g.ones.res], lambda e: e.memset(g.ones[:], 1.0))
    kb.op("dve", [g.tri.res], [g.tri_b.res], lambda e: e.tensor_copy(out=g.tri_b[:], in_=g.tri[:]))


def emit_mods(kb, g, dr, NL):
    nc = kb.nc
    with kb.scope():
        cs = T(kb.sb([128, NDT], F32, "cs"))
        sg = T(kb.sb([128, NDT], F32, "sg"))
        kb.dma("sp", cs[:], dr["c"].rearrange("o (kt p) -> p (o kt)", p=128), [], [cs.res],
               allow_slow_non_contiguous=True)
        kb.op("act", [cs.res], [sg.res], lambda e: e.activation(out=sg[:], in_=cs[:], func=AF.Sigmoid))
        kb.op("dve", [cs.res, sg.res], [cs.res],
              lambda e: e.tensor_tensor(out=cs[:], in0=cs[:], in1=sg[:], op=ALU.mult))
        wt = [T(kb.sb([128, NDT, 512], F32, "adaw")) for _ in range(2)]
        bt = [T(kb.sb([1, 512], F32, "adab")) for _ in range(2)]
        ot = [T(kb.sb([1, 512], F32, "adao")) for _ in range(2)]
        it = 0
        for l in range(NL):
            wv = dr["ada_w"][l].rearrange("(kt p) n -> p kt n", p=128)
            for cg in range(6 * D // 512):
                w = wt[it % 2]
                b = bt[it % 2]
                o = ot[it % 2]
                kb.dma("sp", w[:], wv[:, :, tsl(cg, 512)], [], [w.res])
                kb.dma("sp", b[:], dr["ada_b"][l:l + 1, tsl(cg, 512)], [], [b.res])
                p = nps(g)
                for kt in range(NDT):
                    kb.op("pe", [cs.res, w.res], [p.res],
                          lambda e, kt=kt: e.matmul(p[0:1, :], lhsT=cs[:, kt:kt + 1], rhs=w[:, kt, :],
                                                    start=(kt == 0), stop=(kt == NDT - 1)),
                          inc=(kt == NDT - 1))
                kb.op("dve", [p.res, b.res], [o.res],
                      lambda e: e.tensor_tensor(out=o[:], in0=p[0:1, :], in1=b[:], op=ALU.add))
                kb.dma("sp", dr["mod_scr"][l:l + 1, tsl(cg, 512)], o[:], [o.res], [g.mod_res])
                it += 1
    barrier(kb)


def emit_layer_consts(kb, g, dr, l):
    L = G()
    modrow = T(kb.sb([96, 128], F32, "modrow"))
    grow = T(kb.sb([64, 128], F32, "grow"))
    L.modT = T(kb.sb([128, 96], F32, "modT"))
    L.gT = T(kb.sb([128, 64], F32, "gT"))
    kb.dma("sp", modrow[:], dr["mod_scr"][l].rearrange("(j p) -> j p", p=128), [g.mod_res], [modrow.res])
    kb.dma("sp", grow[:], dr["norm_g"][l].rearrange("i (j p) -> (i j) p", p=128), [], [grow.res])
    p = nps(g)
    kb.op("pe", [modrow.res, g.ident.res], [p.res],
          lambda e: e.transpose(p[:, 0:96], modrow[:], g.ident[0:96, 0:96]))
    kb.op("dve", [p.res], [L.modT.res], lambda e: e.tensor_copy(out=L.modT[:], in_=p[:, 0:96]))
    p2 = nps(g)
    kb.op("pe", [grow.res, g.ident.res], [p2.res],
          lambda e: e.transpose(p2[:, 0:64], grow[:], g.ident[0:64, 0:64]))
    kb.op("dve", [p2.res], [L.gT.res], lambda e: e.tensor_copy(out=L.gT[:], in_=p2[:, 0:64]))
    L.gs_m = T(kb.sb([128, NDT], F32, "gsm"))
    L.gs_f = T(kb.sb([128, NDT], F32, "gsf"))
    for (dst, gi, sc) in ((L.gs_m, 0, 16), (L.gs_f, 2, 64)):
        kb.op("dve", [L.modT.res, L.gT.res], [dst.res],
              lambda e, dst=dst, gi=gi, sc=sc: e.scalar_tensor_tensor(
                  out=dst[:], in0=L.modT[:, sc:sc + 16], scalar=1.0, in1=L.gT[:, gi * 16:gi * 16 + 16],
                  op0=ALU.add, op1=ALU.mult))
    L.sh_m = T(kb.sb([128, NDT], F32, "shm"))
    L.sh_f = T(kb.sb([128, NDT], F32, "shf"))
    kb.op("dve", [L.modT.res], [L.sh_m.res], lambda e: e.tensor_copy(out=L.sh_m[:], in_=L.modT[:, 0:16]))
    kb.op("dve", [L.modT.res], [L.sh_f.res], lambda e: e.tensor_copy(out=L.sh_f[:], in_=L.modT[:, 48:64]))
    return L


def rstd_from_ss(kb, ss, n, tmp):
    kb.op("dve", [ss.res], [ss.res],
          lambda e: e.tensor_scalar(out=ss[:], in0=ss[:], scalar1=1.0 / n, scalar2=EPS, op0=ALU.mult, op1=ALU.add))
    kb.op("act", [ss.res], [ss.res], lambda e: e.activation(out=ss[:], in_=ss[:], func=AF.Sqrt))
    kb.op("dve", [ss.res], [ss.res], lambda e: e.reciprocal(out=ss[:], in_=ss[:]))


def norm_transpose_block(kb, g, xts, junk, gs, sh, hT, hres, stat):
    for tt in range(4):
        x = xts[tt]
        ss = stat[tt]
        kb.op("act", [x.res], [junk.res, ss.res],
              lambda e: e.activation(out=junk[:], in_=x[:], func=AF.Square, accum_out=ss[:]), fuse=False)
        rstd_from_ss(kb, ss, D, None)
        kb.op("dve", [x.res, ss.res], [x.res],
              lambda e: e.tensor_scalar(out=x[:], in0=x[:], scalar1=ss[:, 0:1], scalar2=None, op0=ALU.mult))
    for dt in range(NDT):
        p = nps(g)
        for tt in range(4):
            kb.op("pe", [xts[tt].res, g.ident.res], [p.res],
                  lambda e: e.transpose(p[:, tsl(tt)], xts[tt][:, tsl(dt)], g.ident[:]), inc=(tt == 3))
        kb.op("act", [p.res, gs.res, sh.res], [hres[dt]],
              lambda e: e.activation(out=hT[:, dt, :], in_=p[:], func=AF.Identity,
                                     scale=gs[:, dt:dt + 1], bias=sh[:, dt:dt + 1]))


PROJ_F = [
    ("qk", 0, 1024), ("mlqk", 1536, 1024), ("glqk", 3592, 512), ("gla", 5128, 16), ("s5u", 5144, 512)]
PROJ_T = [
    ("dav", 1024, 512), ("mlv", 2560, 512), ("mlo", 3072, 512), ("mlif", 3584, 8), ("glv", 4104, 512),
    ("glr", 4616, 512)]


def emit_phaseA(kb, g, dr, L, l, S):
    nc = kb.nc
    NB = S // 512
    xv = dr["xres"]
    with kb.scope():
        xt = [[T(kb.sb([128, D], F32, "xt")) for _ in range(4)] for _ in range(2)]
        junk = T(kb.sb([128, D], F32, "junk"))
        stat = [T(kb.sb([128, 1], F32, "stat")) for _ in range(4)]
        hT = T(kb.sb([128, NDT, 512], BF16, "hT"))
        hres = [Res() for _ in range(NDT)]
        wT = [T(kb.sb([128, NDT, 512], BF16, "wT")) for _ in range(3)]
        stf = [T(kb.sb([128, 512], F32, "stf")) for _ in range(4)]
        stb = [T(kb.sb([128, 512], BF16, "stb")) for _ in range(3)]
        cnt = {"wf": 0, "wt": 0, "sf": 0, "sb": 0, "ev": 0}
        win = dr["w_in"][l].rearrange("(kt p) n -> p kt n", p=128)

        def load_x(b):
            for tt in range(4):
                t = xt[b % 2][tt]
                kb.dma("sp", t[:], xv[b * 512 + tt * 128: b * 512 + (tt + 1) * 128, :], [g.x_res], [t.res])

        def evac(out_ap, in_ap, reads, writes, scale=None):
            cnt["ev"] += 1
            if cnt["ev"] % 2 == 0:
                kb.op("act", reads, writes,
                      lambda e: e.activation(out=out_ap, in_=in_ap, func=AF.Copy,
                                             scale=(1.0 if scale is None else scale)))
            else:
                if scale is None:
                    kb.op("dve", reads, writes, lambda e: e.tensor_copy(out=out_ap, in_=in_ap))
                else:
                    kb.op("dve", reads, writes,
                          lambda e: e.tensor_scalar(out=out_ap, in0=in_ap, scalar1=scale, scalar2=None,
                                                    op0=ALU.mult))

        load_x(0)
        for b in range(NB):
            if b + 1 < NB:
                load_x(b + 1)
            xts = xt[b % 2]
            norm_transpose_block(kb, g, xts, junk, L.gs_m, L.sh_m, hT, hres, stat)
            kb.dma("sp", dr["hT_scr"].rearrange("dt p s -> p dt s")[:, :, tsl(b, 512)], hT[:], hres,
                   [g.scr_res["hT"]])
            ts0 = b * 512
            for (name, c0, ncols) in PROJ_F:
                for gi in range((ncols + 511) // 512):
                    gcols = min(512, ncols - gi * 512)
                    w = wT[cnt["wt"] % 3]
                    cnt["wt"] += 1
                    kb.dma("w", w[:, :, 0:gcols], win[:, :, c0 + gi * 512: c0 + gi * 512 + gcols], [], [w.res])
                    for tq in range((gcols + 127) // 128):
                        ti = gi * 4 + tq
                        nc_ = min(128, gcols - tq * 128)
                        p = nps(g)
                        for kt in range(NDT):
                            kb.op("pe", [w.res, hres[kt]], [p.res],
                                  lambda e: e.matmul(p[0:nc_, :], lhsT=w[:, kt, tq * 128:tq * 128 + nc_], rhs=hT[:, kt, :],
                                                     start=(kt == 0), stop=(kt == NDT - 1)), inc=(kt == NDT - 1))
                        if name == "qk":
                            s = stb[cnt["sb"] % 3]
                            cnt["sb"] += 1
                            sc = 0.125 if ti < 4 else None
                            evac(s[:], p[:], [p.res], [s.res], scale=sc)
                            kb.dma("sp", dr["qk_scr"][ti, :, ts0:ts0 + 512], s[:], [s.res], [g.scr_res["qk"]])
                        else:
                            s = stf[cnt["sf"] % 4]
                            cnt["sf"] += 1
                            sc = 0.125 if (name == "glqk" and ti < 2) else None
                            evac(s[0:nc_, :], p[0:nc_, :], [p.res], [s.res], scale=sc)
                            dst = {"mlqk": dr["mlqk_scr"], "glqk": dr["glqk_scr"], "s5u": dr["s5u_scr"]}.get(name)
                            if name == "gla":
                                kb.dma("sp", dr["gla_scr"][:, ts0:ts0 + 512], s[0:16, :], [s.res], [g.scr_res["gla"]])
                            else:
                                kb.dma("sp", dst[ti, :, ts0:ts0 + 512], s[:], [s.res], [g.scr_res[name]])
            for (name, c0, ncols) in PROJ_T:
                w = wT[cnt["wt"] % 3]
                cnt["wt"] += 1
                kb.dma("w", w[:, :, 0:ncols], win[:, :, c0:c0 + ncols], [], [w.res])
                for tt in range(4):
                    p = nps(g)
                    for kt in range(NDT):
                        kb.op("pe", [w.res, hres[kt]], [p.res],
                              lambda e: e.matmul(p[:, 0:ncols], lhsT=hT[:, kt, tsl(tt)], rhs=w[:, kt, 0:ncols],
                                                 start=(kt == 0), stop=(kt == NDT - 1)), inc=(kt == NDT - 1))
                    r0 = ts0 + tt * 128
                    if name == "dav":
                        s = stb[cnt["sb"] % 3]
                        cnt["sb"] += 1
                    else:
                        s = stf[cnt["sf"] % 4]
                        cnt["sf"] += 1
                    evac(s[:, 0:ncols], p[:, 0:ncols], [p.res], [s.res])
                    kb.dma("sp", dr[name + "_scr"][r0:r0 + 128, :], s[:, 0:ncols], [s.res], [g.scr_res[name]])
    barrier(kb)


def make_gg(kb, g, dr, l, gi, mi, dst, tmp):
    kb.dma("sp", dst[:], dr["norm_g"][l, gi:gi + 1, :].partition_broadcast(128), [], [dst.res])
    kb.dma("sp", tmp[:], dr["mod_scr"][l:l + 1, mi * D:(mi + 1) * D].partition_broadcast(128),
           [g.mod_res], [tmp.res])
    kb.op("dve", [tmp.res], [dst.res],
          lambda e: e.tensor_tensor(out=dst[:], in0=dst[:], in1=tmp[:], op=ALU.mult))


def epilogue(kb, g, dr, yT, yres, gg, b, bufs):
    xe, junk, ss4, ss, tmp = bufs
    for tt in range(4):
        r0 = b * 512 + tt * 128
        x = xe[tt % 2]
        kb.dma("sp", x[:], dr["xres"][r0:r0 + 128, :], [g.x_res], [x.res])
        banks = [nps(g) for _ in range(4)]
        for cg in range(4):
            for dd in range(4):
                kb.op("pe", [yres[cg * 4 + dd], g.ident.res], [banks[cg].res],
                      lambda e: e.transpose(banks[cg][:, tsl(dd)], yT[:, cg * 4 + dd, tsl(tt)], g.ident[:]),
                      inc=(dd == 3))
            kb.op("act", [banks[cg].res], [junk.res, ss4.res],
                  lambda e: e.activation(out=junk[:], in_=banks[cg][:], func=AF.Square,
                                         accum_out=ss4[:, cg:cg + 1]), fuse=False)
        kb.op("dve", [ss4.res], [ss.res], lambda e: e.reduce_sum(out=ss[:], in_=ss4[:], axis=AX.X))
        rstd_from_ss(kb, ss, D, None)
        for cg in range(4):
            t = tmp[cg % 2]
            kb.op("dve", [banks[cg].res, ss.res, gg.res], [t.res],
                  lambda e: e.scalar_tensor_tensor(out=t[:], in0=banks[cg][:], scalar=ss[:, 0:1],
                                                   in1=gg[:, tsl(cg, 512)], op0=ALU.mult, op1=ALU.mult))
            kb.op("pool", [t.res], [x.res],
                  lambda e: e.tensor_tensor(out=x[:, tsl(cg, 512)], in0=x[:, tsl(cg, 512)], in1=t[:], op=ALU.add))
        kb.dma("sp", dr["xres"][r0:r0 + 128, :], x[:], [x.res], [g.x_res])


def emit_phaseC1(kb, g, dr, L, l, S):
    nc = kb.nc
    NB = S // 512
    with kb.scope():
        hT = [T(kb.sb([128, NDT, 512], BF16, "hT"))] * 2
        oT = [T(kb.sb([128, NDT, 512], BF16, "oT"))] * 2
        mg = T(kb.sb([128, NDT, 512], BF16, "mg"))
        mres = [Res() for _ in range(NDT)]
        wg = [T(kb.sb([128, NDT, 512], BF16, "wg")) for _ in range(2)]
        wb = [T(kb.sb([128, 4, 512], BF16, "wb")) for _ in range(2)]
        yT = T(kb.sb([128, NDT, 512], F32, "yT"))
        yres = [Res() for _ in range(NDT)]
        acc = [T(kb.sb([128, 512], F32, "acc")) for _ in range(4)]
        sg = [T(kb.sb([128, 512], F32, "sg")) for _ in range(2)]
        tm = [T(kb.sb([128, 512], F32, "tm")) for _ in range(2)]
        bgrow = T(kb.sb([64, 128], F32, "bgrow"))
        bgT = T(kb.sb([128, 64], F32, "bgT"))
        xe = [T(kb.sb([128, D], F32, "xe")) for _ in range(2)]
        junk = T(kb.sb([128, 512], F32, "junk"))
        ss4 = T(kb.sb([128, 4], F32, "ss4"))
        ss = T(kb.sb([128, 1], F32, "ss"))
        ebufs = (xe, junk, ss4, ss, tm)
        gg_m = T(kb.sb([128, D], F32, "ggm"))
        make_gg(kb, g, dr, l, 1, 2, gg_m, xe[0])
        kb.dma("sp", bgrow[:], dr["b_gate"][l].rearrange("i (j p) -> (i j) p", p=128), [], [bgrow.res])
        p = nps(g)
        kb.op("pe", [bgrow.res, g.ident.res], [p.res],
              lambda e: e.transpose(p[:, 0:64], bgrow[:], g.ident[0:64, 0:64]))
        kb.op("dve", [p.res], [bgT.res], lambda e: e.tensor_copy(out=bgT[:], in_=p[:, 0:64]))
        nw = 0

        def load_blk(b):
            kb.dma("sp", hT[b % 2][:], dr["hT_scr"].rearrange("dt p s -> p dt s")[:, :, tsl(b, 512)],
                   [g.scr_res["hT"]], [hT[b % 2].res])
            kb.dma("sp", oT[b % 2][:], dr["o_scr"].rearrange("dt p s -> p dt s")[:, :, tsl(b, 512)],
                   [g.scr_res["o"]], [oT[b % 2].res])

        for b in range(NB):
            load_blk(b)
            h = hT[b % 2]
            o = oT[b % 2]
            for cg in range(4):
                for i in range(4):
                    w = wg[nw % 2]
                    w2 = wb[nw % 2]
                    nw += 1
                    kb.dma("w", w[:], dr["w_gate"][l, i].rearrange("(kt p) n -> p kt n", p=128)[:, :, tsl(cg, 512)],
                           [], [w.res])
                    kb.dma("w", w2[:], dr["w_branch"][l, i].rearrange("(kt p) n -> p kt n", p=128)[:, :, tsl(cg, 512)],
                           [], [w2.res])
                    for dd in range(4):
                        dt = cg * 4 + dd
                        pg = nps(g)
                        pb = nps(g)
                        for kt in range(NDT):
                            kb.op("pe", [w.res, h.res], [pg.res],
                                  lambda e: e.matmul(pg[:], lhsT=w[:, kt, tsl(dd)], rhs=h[:, kt, :],
                                                     start=(kt == 0), stop=(kt == NDT - 1)), inc=(kt == NDT - 1))
                        for kk in range(4):
                            kb.op("pe", [w2.res, o.res], [pb.res],
                                  lambda e: e.matmul(pb[:], lhsT=w2[:, kk, tsl(dd)], rhs=o[:, i * 4 + kk, :],
                                                     start=(kk == 0), stop=(kk == 3)), inc=(kk == 3))
                        s_ = sg[(i * 4 + dd) % 2]
                        kb.op("act", [pg.res, bgT.res], [s_.res],
                              lambda e: e.activation(out=s_[:], in_=pg[:], func=AF.Sigmoid,
                                                     bias=bgT[:, i * 16 + dt:i * 16 + dt + 1], scale=1.0))
                        a = acc[dd]
                        if i == 0:
                            kb.op("dve", [s_.res, pb.res], [a.res],
                                  lambda e: e.tensor_tensor(out=a[:], in0=s_[:], in1=pb[:], op=ALU.mult))
                        else:
                            kb.op("dve", [s_.res, pb.res], [s_.res],
                                  lambda e: e.tensor_tensor(out=s_[:], in0=s_[:], in1=pb[:], op=ALU.mult))
                            if i < 3:
                                kb.op("pool", [s_.res, a.res], [a.res],
                                      lambda e: e.tensor_tensor(out=a[:], in0=a[:], in1=s_[:], op=ALU.add))
                            else:
                                kb.op("pool", [s_.res, a.res], [mres[dt]],
                                      lambda e: e.tensor_tensor(out=mg[:, dt, :], in0=a[:], in1=s_[:], op=ALU.add))
            for cg in range(4):
                w = wg[nw % 2]
                nw += 1
                kb.dma("w", w[:], dr["w_out"][l].rearrange("(kt p) n -> p kt n", p=128)[:, :, tsl(cg, 512)],
                       [], [w.res])
                for dd in range(4):
                    dt = cg * 4 + dd
                    pm = nps(g)
                    for kt in range(NDT):
                        kb.op("pe", [w.res, mres[kt]], [pm.res],
                              lambda e: e.matmul(pm[:], lhsT=w[:, kt, tsl(dd)], rhs=mg[:, kt, :],
                                                 start=(kt == 0), stop=(kt == NDT - 1)), inc=(kt == NDT - 1))
                    kb.op("act", [pm.res], [yres[dt]],
                          lambda e: e.activation(out=yT[:, dt, :], in_=pm[:], func=AF.Copy))
            epilogue(kb, g, dr, yT, yres, gg_m, b, ebufs)
    barrier(kb)


def emit_phaseC2(kb, g, dr, L, l, S):
    nc = kb.nc
    NB = S // 512
    NH = FFH // 128
    with kb.scope():
        xt = [T(kb.sb([128, D], F32, "xt")) for _ in range(4)]
        junkb = T(kb.sb([128, D], BF16, "junkb"))
        stat = [T(kb.sb([128, 1], F32, "stat")) for _ in range(4)]
        hT = T(kb.sb([128, NDT, 512], BF16, "h2T"))
        hres = [Res() for _ in range(NDT)]
        uT = T(kb.sb([128, NH, 512], BF16, "uT"))
        ures = [Res() for _ in range(NH)]
        wa = [T(kb.sb([128, NDT, 256], BF16, "wa")) for _ in range(2)]
        wgt = [T(kb.sb([128, NDT, 256], BF16, "wgt")) for _ in range(2)]
        wo = [T(kb.sb([128, NH, 128], BF16, "wo")) for _ in range(2)]
        yT = T(kb.sb([128, NDT, 512], F32, "yT"))
        yres = [Res() for _ in range(NDT)]
        sa = [T(kb.sb([128, 512], F32, "sa")) for _ in range(2)]
        tm = [T(kb.sb([128, 512], F32, "tm")) for _ in range(2)]
        junk = T(kb.sb([128, 512], F32, "junk"))
        ss4 = T(kb.sb([128, 4], F32, "ss4"))
        ss = T(kb.sb([128, 1], F32, "ss"))
        ebufs = ([xt[0], xt[1]], junk, ss4, ss, tm)
        gg_f = T(kb.sb([128, D], F32, "ggf"))
        make_gg(kb, g, dr, l, 3, 5, gg_f, xt[0])
        wi = dr["ffn_w_in"][l].rearrange("(kt p) n -> p kt n", p=128)
        wov = dr["ffn_w_out_r"][l]
        nw = 0
        nwo = 0
        for b in range(NB):
            for tt in range(4):
                kb.dma("sp", xt[tt][:], dr["xres"][b * 512 + tt * 128: b * 512 + (tt + 1) * 128, :],
                       [g.x_res], [xt[tt].res])
            norm_transpose_block(kb, g, xt, junkb, L.gs_f, L.sh_f, hT, hres, stat)
            for j in range(NH):
                if j % 2 == 0:
                    w1 = wa[nw % 2]
                    w2 = wgt[nw % 2]
                    nw += 1
                    kb.dma("w", w1[:], wi[:, :, tsl(j // 2, 256)], [], [w1.res])
                    kb.dma("w", w2[:], wi[:, :, FFH + (j // 2) * 256: FFH + (j // 2 + 1) * 256], [], [w2.res])
                jj = tsl(j % 2)
                pa = nps(g)
                pg = nps(g)
                for kt in range(NDT):
                    kb.op("pe", [w1.res, hres[kt]], [pa.res],
                          lambda e: e.matmul(pa[:], lhsT=w1[:, kt, jj], rhs=hT[:, kt, :],
                                             start=(kt == 0), stop=(kt == NDT - 1)), inc=(kt == NDT - 1))
                for kt in range(NDT):
                    kb.op("pe", [w2.res, hres[kt]], [pg.res],
                          lambda e: e.matmul(pg[:], lhsT=w2[:, kt, jj], rhs=hT[:, kt, :],
                                             start=(kt == 0), stop=(kt == NDT - 1)), inc=(kt == NDT - 1))
                s_ = sa[j % 2]
                kb.op("act", [pa.res], [s_.res], lambda e: e.activation(out=s_[:], in_=pa[:], func=AF.Silu))
                kb.op("dve", [s_.res, pg.res], [ures[j]],
                      lambda e: e.tensor_tensor(out=uT[:, j, :], in0=s_[:], in1=pg[:], op=ALU.mult))
            for dt in range(NDT):
                w = wo[nwo % 2]
                nwo += 1
                kb.dma("w", w[:], wov[dt], [], [w.res])
                py = nps(g)
                for kt in range(NH):
                    kb.op("pe", [w.res, ures[kt]], [py.res],
                          lambda e: e.matmul(py[:], lhsT=w[:, kt, :], rhs=uT[:, kt, :],
                                             start=(kt == 0), stop=(kt == NH - 1)), inc=(kt == NH - 1))
                kb.op("act", [py.res], [yres[dt]],
                      lambda e: e.activation(out=yT[:, dt, :], in_=py[:], func=AF.Copy))
            epilogue(kb, g, dr, yT, yres, gg_f, b, ebufs)
    barrier(kb)


def emit_attn(kb, g, dr, l, S):
    nc = kb.nc
    NQB = S // 512
    NKT = S // 128
    lam_init = 0.8 - 0.6 * math.exp(-0.3 * l)
    g.psn = 4
    with kb.scope():
        QT = T(kb.sb([128, S], BF16, "QT"))
        KT = T(kb.sb([128, S], BF16, "KT"))
        V = T(kb.sb([128, NKT, 129], BF16, "V"))
        biasT = T(kb.sb([128, 5, 512], F32, "biasT"))
        cb = T(kb.sb([128, 4], F32, "cb"))
        lp = T(kb.sb([128, 256], F32, "lp"))
        lam = T(kb.sb([128, 4], F32, "lam"))
        pT = [T(kb.sb([128, 512], BF16, "pT")) for _ in range(3)]
        tmpf = [T(kb.sb([128, 512], F32, "tmpf")) for _ in range(2)]
        o0 = [T(kb.sb([128, 128], F32, "o0")) for _ in range(4)]
        o1 = [T(kb.sb([128, 128], F32, "o1")) for _ in range(2)]
        rec = [T(kb.sb([128, 2], F32, "rec")) for _ in range(2)]
        ssq = [T(kb.sb([128, 1], F32, "ssq")) for _ in range(2)]
        junk = T(kb.sb([128, 128], F32, "junk"))
        ost = [T(kb.sb([128, 512], BF16, "ost")) for _ in range(2)]
        kb.dma("sp", lp[:], dr["diff_lambda"][l:l + 1].rearrange("o a d -> o (a d)").partition_broadcast(128),
               [], [lp.res])
        kb.dma("sp", cb[:], dr["c_bias_far"].partition_broadcast(128), [], [cb.res])
        kb.op("dve", [lp.res], [lp.res],
              lambda e: e.tensor_tensor(out=lp[:, 0:64], in0=lp[:, 0:64], in1=lp[:, 64:128], op=ALU.mult))
        kb.op("dve", [lp.res], [lp.res],
              lambda e: e.tensor_tensor(out=lp[:, 128:192], in0=lp[:, 128:192], in1=lp[:, 192:256], op=ALU.mult))
        kb.op("dve", [lp.res], [lam.res], lambda e: e.reduce_sum(out=lam[:, 0:1], in_=lp[:, 0:64], axis=AX.X))
        kb.op("dve", [lp.res], [lam.res], lambda e: e.reduce_sum(out=lam[:, 1:2], in_=lp[:, 128:192], axis=AX.X))
        kb.op("act", [lam.res], [lam.res], lambda e: e.activation(out=lam[:, 0:2], in_=lam[:, 0:2], func=AF.Exp))
        kb.op("dve", [lam.res], [lam.res],
              lambda e: e.tensor_tensor(out=lam[:, 2:3], in0=lam[:, 1:2], in1=lam[:, 0:1], op=ALU.subtract))
        kb.op("dve", [lam.res], [lam.res],
              lambda e: e.tensor_scalar(out=lam[:, 2:3], in0=lam[:, 2:3], scalar1=-lam_init, scalar2=None,
                                        op0=ALU.add))
        kb.op("dve", [], [V.res], lambda e: e.memset(V[:, :, 128:129], 1.0))
        npt = 0
        nev = 0
        for h in range(4):
            kb.dma("sp", QT[:], dr["qk_scr"][h], [g.scr_res["qk"]], [QT.res])
            kb.dma("sp", KT[:], dr["qk_scr"][4 + h], [g.scr_res["qk"]], [KT.res])
            kb.dma("sp", V[:, :, 0:128], dr["dav_scr"][:, tsl(h)].rearrange("(kt p) c -> p kt c", p=128),
                   [g.scr_res["dav"]], [V.res])
            kb.dma("sp", biasT[:], dr["c_biasT"][h].rearrange("a k q -> k a q"), [], [biasT.res])
            for qb in range(NQB):
                for m in range(2):
                    ms = slice(m * 64, (m + 1) * 64)
                    O = [g.ps[4 + qs] for qs in range(4)]
                    nk = 4 * (qb + 1)

                    def qk(kt):
                        sT = nps(g)
                        kb.op("pe", [KT.res, QT.res], [sT.res],
                              lambda e: e.matmul(sT[:], lhsT=KT[ms, tsl(kt)], rhs=QT[ms, tsl(qb, 512)],
                                                 start=True, stop=True))
                        return sT

                    sT_next = qk(0)
                    for kt in range(nk):
                        sT = sT_next
                        if kt + 1 < nk:
                            sT_next = qk(kt + 1)
                        dmin = qb * 4 - kt
                        p_ = pT[npt % 3]
                        npt += 1
                        if dmin >= 2:
                            kb.op("act", [sT.res, cb.res], [p_.res],
                                  lambda e: e.activation(out=p_[:], in_=sT[:], func=AF.Exp,
                                                         bias=cb[:, h:h + 1], scale=1.0))
                        else:
                            q0 = max(0, -dmin) * 128
                            tf = tmpf[kt % 2]
                            kb.op("dve", [sT.res, biasT.res], [tf.res],
                                  lambda e: e.tensor_tensor(out=tf[:, q0:512], in0=sT[:, q0:512],
                                                            in1=biasT[:, dmin + 3, q0:512], op=ALU.add))
                            kb.op("act", [tf.res], [p_.res],
                                  lambda e: e.activation(out=p_[:, q0:512], in_=tf[:, q0:512], func=AF.Exp))
                        for qs in range(4):
                            dl = dmin + qs
                            if dl < 0:
                                continue
                            kb.op("pe", [p_.res, V.res], [O[qs].res],
                                  lambda e: e.matmul(O[qs][:, 0:129], lhsT=p_[:, tsl(qs)], rhs=V[:, kt, :],
                                                     start=(kt == 0), stop=(dl == 0)), inc=(dl == 0))
                    for qs in range(4):
                        r = rec[qs % 2]
                        kb.op("dve", [O[qs].res], [r.res],
                              lambda e: e.reciprocal(out=r[:, 0:1], in_=O[qs][:, 128:129]))
                        if m == 0:
                            kb.op("dve", [O[qs].res, r.res], [o0[qs].res],
                                  lambda e: e.tensor_scalar(out=o0[qs][:], in0=O[qs][:, 0:128], scalar1=r[:, 0:1],
                                                            scalar2=None, op0=ALU.mult))
                        else:
                            oo = o1[qs % 2]
                            sq = ssq[qs % 2]
                            kb.op("dve", [r.res, lam.res], [r.res],
                                  lambda e: e.tensor_tensor(out=r[:, 1:2], in0=r[:, 0:1], in1=lam[:, 2:3], op=ALU.mult))
                            kb.op("dve", [O[qs].res, r.res, o0[qs].res], [oo.res],
                                  lambda e: e.scalar_tensor_tensor(out=oo[:], in0=O[qs][:, 0:128], scalar=r[:, 1:2],
                                                                   in1=o0[qs][:], op0=ALU.mult, op1=ALU.add))
                            kb.op("act", [oo.res], [junk.res, sq.res],
                                  lambda e: e.activation(out=junk[:], in_=oo[:], func=AF.Square, accum_out=sq[:]), fuse=False)
                            rstd_from_ss(kb, sq, 128, None)
                            kb.op("dve", [oo.res, sq.res], [oo.res],
                                  lambda e: e.tensor_scalar(out=oo[:], in0=oo[:], scalar1=sq[:, 0:1],
                                                            scalar2=(1.0 - lam_init), op0=ALU.mult, op1=ALU.mult))
                            pt_ = nps(g)
                            kb.op("pe", [oo.res, g.ident.res], [pt_.res],
                                  lambda e: e.transpose(pt_[:, 0:128], oo[:], g.ident[:]))
                            os_ = ost[qb % 2]
                            kb.op("dve", [pt_.res], [os_.res],
                                  lambda e: e.tensor_copy(out=os_[:, tsl(qs)], in_=pt_[:, 0:128]))
                    if m == 1:
                        os_ = ost[qb % 2]
                        kb.dma("sp", dr["o_scr"][h, :, tsl(qb, 512)], os_[:], [os_.res], [g.scr_res["o"]])
    g.psn = 8
    barrier(kb)


def emit_mlstm(kb, g, dr, l, S):
    nc = kb.nc
    NCH = S // 128
    SEG = min(2048, S)
    KSC = 128 ** -0.5
    with kb.scope():
        qT = T(kb.sb([128, S], BF16, "qT"))
        kT = T(kb.sb([128, S], BF16, "kT"))
        ktok = T(kb.sb([128, NCH, 128], BF16, "ktok"))
        vpp = T(kb.sb([128, NCH, 129], BF16, "vpp"))
        raw = T(kb.sb([128, SEG + 3], F32, "raw"))
        yc = T(kb.sb([128, SEG], F32, "yc"))
        cw = T(kb.sb([128, 4, 8], F32, "cw"))
        gif = T(kb.sb([128, NCH, 8], F32, "gif"))
        gbb = T(kb.sb([128, 8], F32, "gbb"))
        spt = T(kb.sb([128, NCH, 4], F32, "spt"))
        tot = T(kb.sb([128, NCH, 4], F32, "tot"))
        A = T(kb.sb([128, NCH, 4], F32, "A"))
        R = T(kb.sb([128, NCH, 4], F32, "R"))
        EL = T(kb.sb([128, NCH, 4], F32, "EL"))
        vst = [T(kb.sb([128, 8, 128], F32, "vst")) for _ in range(2)]
        ost_ = [T(kb.sb([128, 8, 128], F32, "osg")) for _ in range(2)]
        sTm = [T(kb.sb([128, 128], BF16, "sTm")) for _ in range(2)]
        Cf = T(kb.sb([128, 129], F32, "Cf"))
        Cb = [T(kb.sb([128, 129], BF16, "Cb")) for _ in range(2)]
        dd = [T(kb.sb([128, 4], F32, "dd")) for _ in range(2)]
        ho = [T(kb.sb([128, 128], F32, "ho")) for _ in range(2)]
        ost = [T(kb.sb([128, 512], BF16, "ost")) for _ in range(2)]
        for j in range(4):
            kb.dma("sp", cw[:, j, :], dr["ml_conv"][l, j].rearrange("(t p) -> p t", p=128), [], [cw.res],
                   allow_slow_non_contiguous=True)
        kb.dma("sp", gbb[:], dr["ml_gate_b"][l:l + 1].rearrange("o a h -> o (a h)").partition_broadcast(128),
               [], [gbb.res])
        kb.dma("sp", gif[:], dr["mlif_scr"].rearrange("(c p) j -> p c j", p=128), [g.scr_res["mlif"]], [gif.res])
        for j in range(8):
            kb.op("dve", [gif.res, gbb.res], [gif.res],
                  lambda e: e.tensor_scalar(out=gif[:, :, j], in0=gif[:, :, j], scalar1=gbb[:, j:j + 1],
                                            scalar2=None, op0=ALU.add))
        kb.op("act", [gif.res], [spt.res],
              lambda e: e.activation(out=spt[:], in_=gif[:, :, 4:8], func=AF.Exp, scale=-1.0))
        kb.op("act", [spt.res], [spt.res],
              lambda e: e.activation(out=spt[:], in_=spt[:], func=AF.Ln, bias=1.0, scale=1.0))
        pc = nps(g)
        ptot = nps(g)
        spf = spt[:].rearrange("p c h -> p (c h)")
        kb.op("pe", [spt.res, g.tri.res], [pc.res],
              lambda e: e.matmul(pc[:, 0:NCH * 4], lhsT=g.tri[:], rhs=spf, start=True, stop=True))
        kb.op("pe", [spt.res, g.ones.res], [ptot.res],
              lambda e: e.matmul(ptot[:, 0:NCH * 4], lhsT=g.ones[:], rhs=spf, start=True, stop=True))
        totf = tot[:].rearrange("p c h -> p (c h)")
        Af = A[:].rearrange("p c h -> p (c h)")
        Rf = R[:].rearrange("p c h -> p (c h)")
        ELf = EL[:].rearrange("p c h -> p (c h)")
        kb.op("dve", [ptot.res], [tot.res], lambda e: e.tensor_copy(out=totf, in_=ptot[:, 0:NCH * 4]))
        kb.op("dve", [pc.res, tot.res], [R.res],
              lambda e: e.tensor_tensor(out=Rf, in0=pc[:, 0:NCH * 4], in1=totf, op=ALU.subtract))
        kb.op("dve", [R.res, gif.res], [A.res],
              lambda e: e.tensor_tensor(out=A[:], in0=R[:], in1=gif[:, :, 0:4], op=ALU.add))
        kb.op("act", [A.res], [A.res], lambda e: e.activation(out=Af, in_=Af, func=AF.Exp))
        kb.op("act", [R.res], [R.res], lambda e: e.activation(out=Rf, in_=Rf, func=AF.Exp, scale=-1.0))
        kb.op("act", [tot.res], [EL.res], lambda e: e.activation(out=ELf, in_=totf, func=AF.Exp, scale=-1.0))

        def conv_seg(tile_idx, s0, is_k):
            n = min(SEG, S - s0)
            src = dr["mlqk_scr"][tile_idx]
            if s0 == 0:
                kb.op("dve", [], [raw.res], lambda e: e.memset(raw[:, 0:3], 0.0))
                kb.dma("sp", raw[:, 3:3 + n], src[:, 0:n], [g.scr_res["mlqk"]], [raw.res])
            else:
                kb.dma("sp", raw[:, 0:3 + n], src[:, s0 - 3:s0 + n], [g.scr_res["mlqk"]], [raw.res])
            kb.op("dve", [raw.res, cw.res], [yc.res],
                  lambda e: e.tensor_scalar(out=yc[:, 0:n], in0=raw[:, 3:3 + n], scalar1=cw[:, 3, tile_idx:tile_idx + 1],
                                            scalar2=None, op0=ALU.mult))
            for j in (2, 1, 0):
                kb.op("dve", [raw.res, cw.res, yc.res], [yc.res],
                      lambda e: e.scalar_tensor_tensor(out=yc[:, 0:n], in0=raw[:, j:j + n],
                                                       scalar=cw[:, j, tile_idx:tile_idx + 1], in1=yc[:, 0:n],
                                                       op0=ALU.mult, op1=ALU.add))
            if not is_k:
                kb.op("act", [yc.res], [qT.res],
                      lambda e: e.activation(out=qT[:, s0:s0 + n], in_=yc[:, 0:n], func=AF.Silu))
            else:
                kb.op("act", [yc.res], [yc.res],
                      lambda e: e.activation(out=yc[:, 0:n], in_=yc[:, 0:n], func=AF.Silu))
                kb.op("dve", [yc.res], [kT.res],
                      lambda e: e.tensor_scalar(out=kT[:, s0:s0 + n], in0=yc[:, 0:n], scalar1=KSC, scalar2=None,
                                                op0=ALU.mult))
                for c4 in range(n // 512):
                    p = nps(g)
                    for cc in range(4):
                        kb.op("pe", [yc.res, g.ident.res], [p.res],
                              lambda e: e.transpose(p[:, tsl(cc)], yc[:, c4 * 512 + cc * 128: c4 * 512 + (cc + 1) * 128],
                                                    g.ident[:]), inc=(cc == 3))
                    c0 = s0 // 128 + c4 * 4
                    kb.op("act", [p.res], [ktok.res],
                          lambda e: e.activation(out=ktok[:, c0:c0 + 4, :].rearrange("p c d -> p (c d)"), in_=p[:],
                                                 func=AF.Copy, scale=KSC))

        for h in range(4):
            for s0 in range(0, S, SEG):
                conv_seg(h, s0, False)
            for s0 in range(0, S, SEG):
                conv_seg(4 + h, s0, True)
            for c8 in range(NCH // 8 if NCH >= 8 else 1):
                ncg = min(8, NCH)
                vs = vst[c8 % 2]
                kb.dma("sp", vs[:, 0:ncg, :],
                       dr["mlv_scr"][c8 * 1024: c8 * 1024 + ncg * 128, tsl(h)].rearrange("(c p) d -> p c d", p=128),
                       [g.scr_res["mlv"]], [vs.res])
                for cc in range(ncg):
                    c = c8 * 8 + cc
                    kb.op("dve", [vs.res, A.res], [vpp.res],
                          lambda e: e.tensor_scalar(out=vpp[:, c, 0:128], in0=vs[:, cc, :], scalar1=A[:, c, h:h + 1],
                                                    scalar2=None, op0=ALU.mult))
            kb.op("dve", [A.res], [vpp.res], lambda e: e.tensor_copy(out=vpp[:, :, 128], in_=A[:, :, h]))
            for c in range(NCH):
                cs_ = tsl(c)
                if c % 8 == 0:
                    ncg = min(8, NCH)
                    og = ost_[(c // 8) % 2]
                    kb.dma("sp", og[:, 0:ncg, :],
                           dr["mlo_scr"][c * 128: (c + ncg) * 128, tsl(h)].rearrange("(c p) d -> p c d", p=128),
                           [g.scr_res["mlo"]], [og.res])
                    kb.op("act", [og.res], [og.res],
                          lambda e: e.activation(out=og[:, 0:ncg, :], in_=og[:, 0:ncg, :], func=AF.Sigmoid))
                og = ost_[(c // 8) % 2]
                ps_s = nps(g)
                kb.op("pe", [kT.res, qT.res], [ps_s.res],
                      lambda e: e.matmul(ps_s[:, 0:128], lhsT=kT[:, cs_], rhs=qT[:, cs_], start=True, stop=True))
                sm = sTm[c % 2]
                kb.op("dve", [ps_s.res, g.tri.res], [sm.res],
                      lambda e: e.tensor_tensor(out=sm[:], in0=ps_s[:, 0:128], in1=g.tri[:], op=ALU.mult))
                cb_ = Cb[c % 2]
                if c > 0:
                    kb.op("dve", [Cf.res, EL.res], [cb_.res],
                          lambda e: e.tensor_scalar(out=cb_[:], in0=Cf[:], scalar1=EL[:, c, h:h + 1], scalar2=None,
                                                    op0=ALU.mult))
                pn = nps(g)
                kb.op("pe", [sm.res, vpp.res], [pn.res],
                      lambda e: e.matmul(pn[:, 0:129], lhsT=sm[:], rhs=vpp[:, c, :], start=True, stop=(c == 0)),
                      inc=(c == 0))
                if c > 0:
                    kb.op("pe", [qT.res, cb_.res], [pn.res],
                          lambda e: e.matmul(pn[:, 0:129], lhsT=qT[:, cs_], rhs=cb_[:], start=False, stop=True))
                pd = nps(g)
                kb.op("pe", [ktok.res, vpp.res], [pd.res],
                      lambda e: e.matmul(pd[:, 0:129], lhsT=ktok[:, c, :], rhs=vpp[:, c, :], start=True, stop=True))
                if c == 0:
                    kb.op("dve", [pd.res], [Cf.res], lambda e: e.tensor_copy(out=Cf[:], in_=pd[:, 0:129]))
                else:
                    kb.op("dve", [pd.res, Cf.res, EL.res], [Cf.res],
                          lambda e: e.scalar_tensor_tensor(out=Cf[:], in0=Cf[:], scalar=EL[:, c, h:h + 1],
                                                           in1=pd[:, 0:129], op0=ALU.mult, op1=ALU.add))
                d_ = dd[c % 2]
                kb.op("dve", [pn.res, R.res], [d_.res],
                      lambda e: e.tensor_tensor(out=d_[:, 0:1], in0=pn[:, 128:129], in1=R[:, c, h:h + 1], op=ALU.mult))
                kb.op("dve", [d_.res], [d_.res],
                      lambda e: e.scalar_tensor_tensor(out=d_[:, 1:2], in0=d_[:, 0:1], scalar=-1.0, in1=d_[:, 0:1],
                                                       op0=ALU.mult, op1=ALU.max))
                kb.op("dve", [d_.res], [d_.res],
                      lambda e: e.tensor_scalar(out=d_[:, 1:2], in0=d_[:, 1:2], scalar1=1.0, scalar2=None,
                                                op0=ALU.max))
                kb.op("dve", [d_.res], [d_.res], lambda e: e.reciprocal(out=d_[:, 2:3], in_=d_[:, 1:2]))
                kb.op("dve", [d_.res, R.res], [d_.res],
                      lambda e: e.tensor_tensor(out=d_[:, 3:4], in0=d_[:, 2:3], in1=R[:, c, h:h + 1], op=ALU.mult))
                ho_ = ho[c % 2]
                kb.op("dve", [pn.res, d_.res, og.res], [ho_.res],
                      lambda e: e.scalar_tensor_tensor(out=ho_[:], in0=pn[:, 0:128], scalar=d_[:, 3:4],
                                                       in1=og[:, c % 8, :], op0=ALU.mult, op1=ALU.mult))
                pt_ = nps(g)
                kb.op("pe", [ho_.res, g.ident.res], [pt_.res],
                      lambda e: e.transpose(pt_[:, 0:128], ho_[:], g.ident[:]))
                os_ = ost[(c // 4) % 2]
                kb.op("act", [pt_.res], [os_.res],
                      lambda e: e.activation(out=os_[:, tsl(c % 4)], in_=pt_[:, 0:128], func=AF.Copy))
                if c % 4 == 3:
                    kb.dma("sp", dr["o_scr"][4 + h, :, tsl(c // 4, 512)], os_[:], [os_.res], [g.scr_res["o"]])
    barrier(kb)


def emit_gla(kb, g, dr, l, S):
    nc = kb.nc
    NCH = S // 128
    SEG = min(2048, S)
    with kb.scope():
        qtT = T(kb.sb([128, S], BF16, "qtT"))
        ktT = T(kb.sb([128, S], BF16, "ktT"))
        ktok = T(kb.sb([128, NCH, 128], BF16, "gktok"))
        vb = [T(kb.sb([128, NCH, 128], BF16, "gvb")) for _ in range(2)]
        ELt = T(kb.sb([128, NCH], F32, "ELt"))
        wa2 = T(kb.sb([16, 256], F32, "wa2"))
        nba = T(kb.sb([128, 2], F32, "nba"))
        glaT = T(kb.sb([16, SEG], F32, "glaT"))
        spg = T(kb.sb([128, SEG], F32, "spg"))
        csg = T(kb.sb([128, SEG], F32, "csg"))
        rm = T(kb.sb([128, SEG], F32, "rm"))
        Ee = T(kb.sb([128, SEG], F32, "Ee"))
        rawq = T(kb.sb([128, SEG], F32, "rawq"))
        ktf = T(kb.sb([128, SEG], F32, "ktf"))
        srg = [T(kb.sb([128, 8, 128], F32, "srg")) for _ in range(2)]
        sTm = [T(kb.sb([128, 128], BF16, "gsTm")) for _ in range(2)]
        U = T(kb.sb([128, 128], F32, "U"))
        Sb = [T(kb.sb([128, 128], BF16, "Sb")) for _ in range(2)]
        ssq = [T(kb.sb([128, 1], F32, "gss")) for _ in range(2)]
        junk = T(kb.sb([128, 128], F32, "gjunk"))
        ho = [T(kb.sb([128, 128], F32, "gho")) for _ in range(2)]
        ost = [[T(kb.sb([128, 512], BF16, "gost")) for _ in range(2)] for _ in range(2)]
        kb.dma("sp", wa2[:], dr["gla_wa2"][l], [], [wa2.res])
        kb.dma("sp", nba[:], dr["gla_ba"][l].rearrange("(t p) -> p t", p=128), [], [nba.res],
               allow_slow_non_contiguous=True)
        kb.op("dve", [nba.res], [nba.res],
              lambda e: e.tensor_scalar(out=nba[:], in0=nba[:], scalar1=-1.0, scalar2=None, op0=ALU.mult))
        kb.op("dve", [], [rm.res], lambda e: e.memset(rm[:], 1.0))
        kb.op("dve", [rm.res], [rm.res],
              lambda e: e.memset(rm[:].rearrange("p (c t) -> p c t", t=128)[:, :, 0:1], 0.0))
        for hp in range(2):
            for s0 in range(0, S, SEG):
                n = min(SEG, S - s0)
                kb.dma("sp", glaT[:, 0:n], dr["gla_scr"][:, s0:s0 + n], [g.scr_res["gla"]], [glaT.res])
                kb.dma("sp", rawq[:, 0:n], dr["glqk_scr"][hp, :, s0:s0 + n], [g.scr_res["glqk"]], [rawq.res])
                kb.dma("sp", ktf[:, 0:n], dr["glqk_scr"][2 + hp, :, s0:s0 + n], [g.scr_res["glqk"]], [ktf.res])
                for b5 in range(n // 512):
                    pz = nps(g)
                    kb.op("pe", [wa2.res, glaT.res], [pz.res],
                          lambda e: e.matmul(pz[:], lhsT=wa2[:, tsl(hp)], rhs=glaT[:, tsl(b5, 512)],
                                             start=True, stop=True))
                    kb.op("act", [pz.res, nba.res], [spg.res],
                          lambda e: e.activation(out=spg[:, tsl(b5, 512)], in_=pz[:], func=AF.Exp,
                                                 bias=nba[:, hp:hp + 1], scale=-1.0))
                kb.op("act", [spg.res], [spg.res],
                      lambda e: e.activation(out=spg[:, 0:n], in_=spg[:, 0:n], func=AF.Ln, bias=1.0, scale=1.0))
                kb.op("dve", [spg.res, rm.res], [csg.res],
                      lambda e: e.tensor_tensor_scan(out=csg[:, 0:n], data0=rm[:, 0:n], data1=spg[:, 0:n],
                                                     initial=0.0, op0=ALU.mult, op1=ALU.add))
                kb.op("act", [csg.res], [Ee.res],
                      lambda e: e.activation(out=Ee[:, 0:n], in_=csg[:, 0:n], func=AF.Exp, scale=-1.0 / 16.0))
                kb.op("dve", [rawq.res, Ee.res], [qtT.res],
                      lambda e: e.tensor_tensor(out=qtT[:, s0:s0 + n], in0=rawq[:, 0:n], in1=Ee[:, 0:n], op=ALU.mult))
                c0 = s0 // 128
                kb.op("dve", [Ee.res], [ELt.res],
                      lambda e: e.tensor_copy(out=ELt[:, c0:c0 + n // 128],
                                              in_=Ee[:, 0:n].rearrange("p (c t) -> p c t", t=128)[:, :, 127]))
                kb.op("act", [csg.res], [Ee.res],
                      lambda e: e.activation(out=Ee[:, 0:n], in_=csg[:, 0:n], func=AF.Exp, scale=1.0 / 16.0))
                kb.op("dve", [ktf.res, Ee.res], [ktf.res],
                      lambda e: e.tensor_tensor(out=ktf[:, 0:n], in0=ktf[:, 0:n], in1=Ee[:, 0:n], op=ALU.mult))
                kb.op("act", [ktf.res], [ktT.res],
                      lambda e: e.activation(out=ktT[:, s0:s0 + n], in_=ktf[:, 0:n], func=AF.Copy))
                for c4 in range(n // 512):
                    p = nps(g)
                    for cc in range(4):
                        kb.op("pe", [ktf.res, g.ident.res], [p.res],
                              lambda e: e.transpose(p[:, tsl(cc)], ktf[:, c4 * 512 + cc * 128: c4 * 512 + (cc + 1) * 128],
                                                    g.ident[:]), inc=(cc == 3))
                    cc0 = c0 + c4 * 4
                    kb.op("dve", [p.res], [ktok.res],
                          lambda e: e.tensor_copy(out=ktok[:, cc0:cc0 + 4, :].rearrange("p c d -> p (c d)"), in_=p[:]))
            for hh in range(2):
                hd = hp * 2 + hh
                kb.dma("pool", vb[hh][:], dr["glv_scr"][:, tsl(hd)].rearrange("(c p) d -> p c d", p=128),
                       [g.scr_res["glv"]], [vb[hh].res])
            for c in range(NCH):
                cs_ = tsl(c)
                for hh in range(2):
                    hd = hp * 2 + hh
                    hs = slice(hh * 64, (hh + 1) * 64)
                    if c % 8 == 0:
                        ncg = min(8, NCH)
                        sr = srg[hh]
                        kb.dma("sp", sr[:, 0:ncg, :],
                               dr["glr_scr"][c * 128:(c + ncg) * 128, tsl(hd)].rearrange("(c p) d -> p c d", p=128),
                               [g.scr_res["glr"]], [sr.res])
                        kb.op("act", [sr.res], [sr.res],
                              lambda e: e.activation(out=sr[:, 0:ncg, :], in_=sr[:, 0:ncg, :], func=AF.Silu))
                    sr = srg[hh]
                    ps_s = nps(g)
                    kb.op("pe", [ktT.res, qtT.res], [ps_s.res],
                          lambda e: e.matmul(ps_s[:, 0:128], lhsT=ktT[hs, cs_], rhs=qtT[hs, cs_], start=True, stop=True))
                    sm = sTm[hh]
                    kb.op("dve", [ps_s.res, g.tri.res], [sm.res],
                          lambda e: e.tensor_tensor(out=sm[:], in0=ps_s[:, 0:128], in1=g.tri[:], op=ALU.mult))
                    sb_ = Sb[hh]
                    if c > 0:
                        kb.op("dve", [U.res, ELt.res], [sb_.res],
                              lambda e: e.tensor_scalar(out=sb_[hs, :], in0=U[hs, :], scalar1=ELt[hs, c - 1:c],
                                                        scalar2=None, op0=ALU.mult))
                    po = nps(g)
                    kb.op("pe", [sm.res, vb[hh].res], [po.res],
                          lambda e: e.matmul(po[:, 0:128], lhsT=sm[:], rhs=vb[hh][:, c, :], start=True, stop=(c == 0)),
                          inc=(c == 0))
                    if c > 0:
                        kb.op("pe", [qtT.res, sb_.res], [po.res],
                              lambda e: e.matmul(po[:, 0:128], lhsT=qtT[hs, cs_], rhs=sb_[hs, :], start=False, stop=True))
                    pd = nps(g)
                    kb.op("pe", [ktok.res, vb[hh].res], [pd.res],
                          lambda e: e.matmul(pd[:, 0:128], lhsT=ktok[:, c, :], rhs=vb[hh][:, c, :], start=True, stop=True))
                    if c == 0:
                        kb.op("dve", [pd.res], [U.res], lambda e: e.tensor_copy(out=U[hs, :], in_=pd[hs, 0:128]))
                    else:
                        kb.op("dve", [pd.res, U.res, ELt.res], [U.res],
                              lambda e: e.scalar_tensor_tensor(out=U[hs, :], in0=U[hs, :], scalar=ELt[hs, c - 1:c],
                                                               in1=pd[hs, 0:128], op0=ALU.mult, op1=ALU.add))
                    sq = ssq[hh]
                    kb.op("act", [po.res], [junk.res, sq.res],
                          lambda e: e.activation(out=junk[:], in_=po[:, 0:128], func=AF.Square, accum_out=sq[:]), fuse=False)
                    rstd_from_ss(kb, sq, 128, None)
                    ho_ = ho[hh]
                    kb.op("dve", [po.res, sq.res, sr.res], [ho_.res],
                          lambda e: e.scalar_tensor_tensor(out=ho_[:], in0=po[:, 0:128], scalar=sq[:, 0:1],
                                                           in1=sr[:, c % 8, :], op0=ALU.mult, op1=ALU.mult))
                    pt_ = nps(g)
                    kb.op("pe", [ho_.res, g.ident.res], [pt_.res],
                          lambda e: e.transpose(pt_[:, 0:128], ho_[:], g.ident[:]))
                    os_ = ost[hh][(c // 4) % 2]
                    kb.op("act", [pt_.res], [os_.res],
                          lambda e: e.activation(out=os_[:, tsl(c % 4)], in_=pt_[:, 0:128], func=AF.Copy))
                    if c % 4 == 3:
                        kb.dma("sp", dr["o_scr"][8 + hd, :, tsl(c // 4, 512)], os_[:], [os_.res], [g.scr_res["o"]])
    barrier(kb)


def emit_s5(kb, g, dr, l, S):
    nc = kb.nc
    TB = 256
    NBK = S // TB
    g.psn = 4
    with kb.scope():
        def small(name, shape=(128, 16)):
            return T(kb.sb(list(shape), F32, name))

        rows = [T(kb.sb([16, 128], F32, "s5row")) for _ in range(3)]
        ldt2 = T(kb.sb([16, 2], F32, "ldt2"))
        are, aim, dtt = small("are"), small("aim"), small("dtt")
        mu, Lc, Ls, t1, t2, t3 = small("mu"), small("Lc"), small("Ls"), small("t1"), small("t2"), small("t3")
        Rc, Rs, nRs = small("Rc"), small("Rs"), small("nRs")
        fre, fim, nfim = small("fre"), small("fim"), small("nfim")
        Tc = T(kb.sb([128, 16, TB], F32, "Tc"))
        Ts = T(kb.sb([128, 16, TB], F32, "Ts"))
        Tm = T(kb.sb([128, 16, TB], F32, "Tm"))
        half = T(kb.sb([128, 2], F32, "half"))
        pm = T(kb.sb([128, 128], F32, "pm"))
        bre = T(kb.sb([128, 16, 16], F32, "bre"))
        bim = T(kb.sb([128, 16, 16], F32, "bim"))
        bbr = T(kb.sb([128, 16, 16], F32, "bbr"))
        bbi = T(kb.sb([128, 16, 16], F32, "bbi"))
        btmp = T(kb.sb([128, 16, 16], F32, "btmp"))
        BR = T(kb.sb([16, 16, 2, 128], BF16, "BR"))
        BI = T(kb.sb([16, 16, 2, 128], BF16, "BI"))
        cin = T(kb.sb([128, 128], F32, "cin"))
        CRE = T(kb.sb([128, 16, 128], BF16, "CRE"))
        NCRE = T(kb.sb([128, 16, 128], BF16, "NCRE"))
        NCIM = T(kb.sb([128, 16, 128], BF16, "NCIM"))
        drow = T(kb.sb([8, 128], F32, "drow"))
        dcol = T(kb.sb([128, 8], F32, "dcol"))
        gw = T(kb.sb([128, 4, 512], BF16, "gw"))
        kb.dma("sp", half[:], dr["c_half"], [], [half.res])
        kb.dma("sp", pm[:], dr["c_pm"], [], [pm.res])
        kb.dma("sp", rows[0][:], dr["s5_a_re"][l].rearrange("(pr j) p -> pr (j p)", j=2), [], [rows[0].res])
        kb.dma("sp", rows[1][:], dr["s5_a_im"][l].rearrange("(pr j) p -> pr (j p)", j=2), [], [rows[1].res])
        kb.dma("sp", ldt2[:], dr["s5_log_dt"][l].rearrange("(pr j) -> pr j", j=2), [], [ldt2.res])
        for j in range(2):
            kb.op("dve", [ldt2.res], [rows[2].res],
                  lambda e: e.tensor_copy(out=rows[2][:, j * 64:(j + 1) * 64], in_=ldt2[:, j:j + 1].to_broadcast([16, 64])))
        for src, dst in ((rows[0], are), (rows[1], aim), (rows[2], dtt)):
            p = nps(g)
            kb.op("pe", [src.res, g.ident.res], [p.res],
                  lambda e: e.transpose(p[:, 0:16], src[:], g.ident[0:16, 0:16]))
            kb.op("dve", [p.res], [dst.res], lambda e: e.tensor_copy(out=dst[:], in_=p[:, 0:16]))
        kb.op("act", [dtt.res], [dtt.res], lambda e: e.activation(out=dtt[:], in_=dtt[:], func=AF.Exp))

        def tt(out, a, b, op):
            kb.op("dve", [a.res, b.res], [out.res], lambda e: e.tensor_tensor(out=out[:], in0=a[:], in1=b[:], op=op))

        tt(mu, dtt, are, ALU.mult)
        kb.op("act", [mu.res], [mu.res], lambda e: e.activation(out=mu[:], in_=mu[:], func=AF.Exp))
        tt(t1, dtt, aim, ALU.mult)
        kb.op("act", [t1.res], [Ls.res], lambda e: e.activation(out=Ls[:], in_=t1[:], func=AF.Sin, scale=1.0 / 16.0))
        hpi = small("hpi", (128, 1))
        kb.op("dve", [], [hpi.res], lambda e: e.memset(hpi[:], math.pi / 2))
        kb.op("act", [t1.res, hpi.res], [Lc.res],
              lambda e: e.activation(out=Lc[:], in_=t1[:], func=AF.Sin, scale=1.0 / 16.0, bias=hpi[:, 0:1]))

        def csq(c, s):
            tt(t2, c, s, ALU.mult)
            tt(c, c, c, ALU.mult)
            tt(t3, s, s, ALU.mult)
            tt(c, c, t3, ALU.subtract)
            kb.op("dve", [t2.res], [s.res],
                  lambda e: e.tensor_scalar(out=s[:], in0=t2[:], scalar1=2.0, scalar2=None, op0=ALU.mult))

        for _ in range(4):
            csq(Lc, Ls)
        nr, ni, den = small("nr"), small("ni"), small("den")
        tt(nr, mu, Lc, ALU.mult)
        kb.op("dve", [nr.res], [nr.res],
              lambda e: e.tensor_scalar(out=nr[:], in0=nr[:], scalar1=-1.0, scalar2=None, op0=ALU.add))
        tt(ni, mu, Ls, ALU.mult)
        tt(den, are, are, ALU.mult)
        tt(t2, aim, aim, ALU.mult)
        tt(den, den, t2, ALU.add)
        kb.op("dve", [den.res], [den.res], lambda e: e.reciprocal(out=den[:], in_=den[:]))
        tt(fre, nr, are, ALU.mult)
        tt(t2, ni, aim, ALU.mult)
        tt(fre, fre, t2, ALU.add)
        tt(fre, fre, den, ALU.mult)
        tt(fim, ni, are, ALU.mult)
        tt(t2, nr, aim, ALU.mult)
        tt(fim, fim, t2, ALU.subtract)
        tt(fim, fim, den, ALU.mult)
        kb.op("dve", [], [Tc.res], lambda e: e.memset(Tc[:, :, 0:1], 1.0))
        kb.op("dve", [], [Ts.res], lambda e: e.memset(Ts[:, :, 0:1], 0.0))
        kb.op("dve", [mu.res], [Tm.res],
              lambda e: e.tensor_copy(out=Tm[:], in_=mu[:].unsqueeze(2).to_broadcast([128, 16, TB])))
        tmpa = T(kb.sb([128, 16, TB // 2], F32, "tmpa"))
        n = 1
        while n < TB:
            lcb = Lc[:].unsqueeze(2).to_broadcast([128, 16, n])
            lsb = Ls[:].unsqueeze(2).to_broadcast([128, 16, n])
            kb.op("dve", [Tc.res, Lc.res], [Tc.res],
                  lambda e: e.tensor_tensor(out=Tc[:, :, n:2 * n], in0=Tc[:, :, 0:n], in1=lcb, op=ALU.mult))
            kb.op("dve", [Ts.res, Ls.res], [tmpa.res],
                  lambda e: e.tensor_tensor(out=tmpa[:, :, 0:n], in0=Ts[:, :, 0:n], in1=lsb, op=ALU.mult))
            kb.op("dve", [Tc.res, tmpa.res], [Tc.res],
                  lambda e: e.tensor_tensor(out=Tc[:, :, n:2 * n], in0=Tc[:, :, n:2 * n], in1=tmpa[:, :, 0:n],
                                            op=ALU.subtract))
            kb.op("dve", [Ts.res, Lc.res], [Ts.res],
                  lambda e: e.tensor_tensor(out=Ts[:, :, n:2 * n], in0=Ts[:, :, 0:n], in1=lcb, op=ALU.mult))
            kb.op("dve", [Tc.res, Ls.res], [tmpa.res],
                  lambda e: e.tensor_tensor(out=tmpa[:, :, 0:n], in0=Tc[:, :, 0:n], in1=lsb, op=ALU.mult))
            kb.op("dve", [Ts.res, tmpa.res], [Ts.res],
                  lambda e: e.tensor_tensor(out=Ts[:, :, n:2 * n], in0=Ts[:, :, n:2 * n], in1=tmpa[:, :, 0:n],
                                            op=ALU.add))
            csq(Lc, Ls)
            n *= 2
        kb.op("dve", [Lc.res], [Rc.res], lambda e: e.tensor_copy(out=Rc[:], in_=Lc[:]))
        kb.op("dve", [Ls.res], [Rs.res], lambda e: e.tensor_copy(out=Rs[:], in_=Ls[:]))
        kb.op("dve", [Ls.res], [nRs.res],
              lambda e: e.tensor_scalar(out=nRs[:], in0=Ls[:], scalar1=-1.0, scalar2=None, op0=ALU.mult))
        kb.dma("sp", bre[:], dr["s5_b_re"][l].rearrange("(pr j) p c -> (j p) pr c", j=2), [], [bre.res])
        kb.dma("sp", bim[:], dr["s5_b_im"][l].rearrange("(pr j) p c -> (j p) pr c", j=2), [], [bim.res])
        frb = fre[:].unsqueeze(2).to_broadcast([128, 16, 16])
        fib = fim[:].unsqueeze(2).to_broadcast([128, 16, 16])

        def t3op(out, a, b_ap, breads, op):
            kb.op("dve", [a.res] + breads, [out.res], lambda e: e.tensor_tensor(out=out[:], in0=a[:], in1=b_ap, op=op))

        t3op(bbr, bre, frb, [fre.res], ALU.mult)
        t3op(btmp, bim, fib, [fim.res], ALU.mult)
        t3op(bbr, bbr, btmp[:], [btmp.res], ALU.subtract)
        t3op(bbi, bim, frb, [fre.res], ALU.mult)
        t3op(btmp, bre, fib, [fim.res], ALU.mult)
        t3op(bbi, bbi, btmp[:], [btmp.res], ALU.add)
        for (srcb, dstB) in ((bbr, BR), (bbi, BI)):
            for j in range(2):
                kb.op("dve", [srcb.res, half.res], [btmp.res],
                      lambda e: e.tensor_scalar(out=btmp[:], in0=srcb[:], scalar1=half[:, j:j + 1], scalar2=None,
                                                op0=ALU.mult))
                for p4 in range(4):
                    p = nps(g)
                    for q in range(4):
                        kb.op("pe", [btmp.res, g.ident.res], [p.res],
                              lambda e: e.transpose(p[0:16, tsl(q)], btmp[:, p4 * 4 + q, :], g.ident[:]), inc=(q == 3))
                    kb.op("act", [p.res], [dstB.res],
                          lambda e: e.activation(out=dstB[:, p4 * 4:p4 * 4 + 4, j, :],
                                                 in_=p[0:16, :].rearrange("c (q m) -> c q m", q=4), func=AF.Copy))
        for (t_, z_) in ((CRE, 0), (NCRE, 0), (NCIM, 0)):
            kb.op("pool", [], [t_.res], lambda e: e.memset(t_[:], 0.0))
        for (srcname, outs) in (("s5_c_re", ((CRE, 1.0), (NCRE, -1.0))), ("s5_c_im", ((NCIM, -1.0),))):
            cv = dr[srcname][l].rearrange("g c p -> (g c) p")
            for gt in range(4):
                kb.dma("sp", cin[:, 0:64], cv[tsl(gt), :], [], [cin.res])
                kb.dma("sp", cin[:, 64:128], cv[tsl(gt), :], [], [cin.res])
                kb.op("dve", [cin.res, pm.res], [cin.res],
                      lambda e: e.tensor_tensor(out=cin[:], in0=cin[:], in1=pm[:], op=ALU.mult))
                p = nps(g)
                kb.op("pe", [cin.res, g.ident.res], [p.res], lambda e: e.transpose(p[:, 0:128], cin[:], g.ident[:]))
                for (dstC, sgn) in outs:
                    for q in range(4):
                        kb.op("act", [p.res], [dstC.res],
                              lambda e: e.activation(out=dstC[:, gt * 4 + q, q * 32:(q + 1) * 32],
                                                     in_=p[:, q * 32:(q + 1) * 32], func=AF.Copy, scale=sgn))
        kb.dma("sp", drow[0:4, :], dr["s5_d"][l].rearrange("(t g) c -> t (g c)", t=4), [], [drow.res])
        kb.dma("sp", drow[4:8, :], dr["s5_glu_b"][l].rearrange("(t p) -> t p", p=128), [], [drow.res])
        p = nps(g)
        kb.op("pe", [drow.res, g.ident.res], [p.res], lambda e: e.transpose(p[:, 0:8], drow[:], g.ident[0:8, 0:8]))
        kb.op("dve", [p.res], [dcol.res], lambda e: e.tensor_copy(out=dcol[:], in_=p[:, 0:8]))
        kb.dma("pool", gw[:], dr["s5_glu_w"][l].rearrange("(kt p) n -> p kt n", p=128), [], [gw.res])

        umm = [T(kb.sb([16, 32, TB], BF16, "umm")) for _ in range(2)]
        usk = [T(kb.sb([128, 4, TB], F32, "usk")) for _ in range(2)]
        dmb = [[T(kb.sb([128, TB], F32, "dm")) for _ in range(4)] for _ in range(2)]
        winb = [[T(kb.sb([128, TB], F32, "win")) for _ in range(2)] for _ in range(2)]
        wrt = [T(kb.sb([128, TB], F32, "wrt")) for _ in range(2)]
        wit = [T(kb.sb([128, TB], F32, "wit")) for _ in range(2)]
        PP = [[T(kb.sb([128, TB], BF16, "PP")) for _ in range(4)] for _ in range(2)]
        w0r = T(kb.sb([128, 16], F32, "w0r"))
        w0i = T(kb.sb([128, 16], F32, "w0i"))
        cr = [T(kb.sb([128, 2], F32, "cr")) for _ in range(2)]
        ysb = [T(kb.sb([128, TB], F32, "ysb")) for _ in range(2)]
        gt1 = [T(kb.sb([128, TB], F32, "gt1")) for _ in range(2)]
        zf = T(kb.sb([128, 4, TB], F32, "zf"))
        zb = T(kb.sb([128, 4, TB], BF16, "zb"))
        sgl = [T(kb.sb([128, TB], F32, "sgl")) for _ in range(2)]
        ost = [T(kb.sb([128, TB], BF16, "s5ost")) for _ in range(2)]
        w0r_res = [Res() for _ in range(16)]
        w0i_res = [Res() for _ in range(16)]
        kb.op("dve", [], w0r_res, lambda e: e.memset(w0r[:], 0.0))
        kb.op("dve", [], w0i_res, lambda e: e.memset(w0i[:], 0.0))
        uv = dr["s5u_scr"]
        npp = 0
        for bk in range(NBK):
            ts_ = slice(bk * TB, (bk + 1) * TB)
            um = umm[bk % 2]
            us = usk[bk % 2]
            kb.dma("pool", um[:], uv.rearrange("t (g c) s -> c (t g) s", c=16)[:, :, ts_], [g.scr_res["s5u"]], [um.res])
            kb.dma("sp", us[:], uv.rearrange("t p s -> p t s")[:, :, ts_], [g.scr_res["s5u"]], [us.res])
            def stage1(pr):
                pbr = nps(g)
                pbi = nps(g)
                for (pb_, B_) in ((pbr, BR), (pbi, BI)):
                    for j in range(2):
                        kb.op("pe", [B_.res, um.res], [pb_.res],
                              lambda e: e.matmul(pb_[:, 0:TB], lhsT=B_[:, pr, j, :], rhs=um[:, 2 * pr + j, :],
                                                 start=(j == 0), stop=(j == 1)), inc=(j == 1))
                d0, d1, d2, d3 = dmb[pr % 2]
                for (o_, tab, src) in ((d0, Tc, pbr), (d1, Ts, pbi), (d2, Tc, pbi), (d3, Ts, pbr)):
                    kb.op("dve", [tab.res, src.res], [o_.res],
                          lambda e: e.tensor_tensor(out=o_[:], in0=tab[:, pr, :], in1=src[:, 0:TB], op=ALU.mult))
                wr_in, wi_in = winb[pr % 2]
                kb.op("pool", [d0.res, d1.res], [wr_in.res],
                      lambda e: e.tensor_tensor(out=wr_in[:], in0=d0[:], in1=d1[:], op=ALU.add))
                kb.op("pool", [d2.res, d3.res], [wi_in.res],
                      lambda e: e.tensor_tensor(out=wi_in[:], in0=d2[:], in1=d3[:], op=ALU.subtract))
                wr = wrt[pr % 2]
                wi = wit[pr % 2]
                kb.op("dve", [Tm.res, wr_in.res, w0r_res[pr]], [wr.res],
                      lambda e: e.tensor_tensor_scan(out=wr[:], data0=Tm[:, pr, :], data1=wr_in[:],
                                                     initial=w0r[:, pr:pr + 1], op0=ALU.mult, op1=ALU.add))
                kb.op("dve", [Tm.res, wi_in.res, w0i_res[pr]], [wi.res],
                      lambda e: e.tensor_tensor_scan(out=wi[:], data0=Tm[:, pr, :], data1=wi_in[:],
                                                     initial=w0i[:, pr:pr + 1], op0=ALU.mult, op1=ALU.add))
                c_ = cr[pr % 2]
                kb.op("act", [wr.res, Rc.res], [c_.res],
                      lambda e: e.activation(out=c_[:, 0:1], in_=wr[:, TB - 1:TB], func=AF.Copy, scale=Rc[:, pr:pr + 1]))
                kb.op("act", [wr.res, Rs.res], [c_.res],
                      lambda e: e.activation(out=c_[:, 1:2], in_=wr[:, TB - 1:TB], func=AF.Copy, scale=Rs[:, pr:pr + 1]))
                kb.op("act", [wi.res, nRs.res, c_.res], [w0r_res[pr]],
                      lambda e: e.activation(out=w0r[:, pr:pr + 1], in_=wi[:, TB - 1:TB], func=AF.Identity,
                                             scale=nRs[:, pr:pr + 1], bias=c_[:, 0:1]))
                kb.op("act", [wi.res, Rc.res, c_.res], [w0i_res[pr]],
                      lambda e: e.activation(out=w0i[:, pr:pr + 1], in_=wi[:, TB - 1:TB], func=AF.Identity,
                                             scale=Rc[:, pr:pr + 1], bias=c_[:, 1:2]))
                P = PP[pr % 2]
                for k_, (o_, tab, src) in enumerate(((P[0], Tc, wr), (P[1], Ts, wi), (P[2], Ts, wr), (P[3], Tc, wi))):
                    en = "dve" if k_ == 3 else "pool"
                    kb.op(en, [tab.res, src.res], [o_.res],
                          lambda e: e.tensor_tensor(out=o_[:], in0=tab[:, pr, :], in1=src[:], op=ALU.mult))

            def stage2(pr):
                P = PP[pr % 2]
                t = pr // 4
                py = g.ps[4 + t]
                for k_, (Cm, Pk) in enumerate(((CRE, P[0]), (NCRE, P[1]), (NCIM, P[2]), (NCIM, P[3]))):
                    first = (pr % 4 == 0 and k_ == 0)
                    last = (pr % 4 == 3 and k_ == 3)
                    kb.op("pe", [Cm.res, Pk.res], [py.res],
                          lambda e: e.matmul(py[:, 0:TB], lhsT=Cm[:, pr, :], rhs=Pk[:], start=first, stop=last),
                          inc=(k_ == 3))
                if pr % 4 == 3:
                    y = ysb[t % 2]
                    g1 = gt1[t % 2]
                    kb.op("dve", [us.res, dcol.res, py.res], [y.res],
                          lambda e: e.scalar_tensor_tensor(out=y[:], in0=us[:, t, :], scalar=dcol[:, t:t + 1],
                                                           in1=py[:, 0:TB], op0=ALU.mult, op1=ALU.add))
                    kb.op("act", [y.res], [g1.res], lambda e: e.activation(out=g1[:], in_=y[:], func=AF.Square))
                    kb.op("dve", [g1.res], [g1.res],
                          lambda e: e.tensor_scalar(out=g1[:], in0=g1[:], scalar1=0.0713548162726, scalar2=1.5957691216057308,
                                                    op0=ALU.mult, op1=ALU.add))
                    kb.op("dve", [g1.res, y.res], [g1.res],
                          lambda e: e.tensor_tensor(out=g1[:], in0=g1[:], in1=y[:], op=ALU.mult))
                    kb.op("act", [g1.res], [g1.res], lambda e: e.activation(out=g1[:], in_=g1[:], func=AF.Sigmoid))
                    kb.op("dve", [g1.res, y.res], [zf.res],
                          lambda e: e.tensor_tensor(out=zf[:, t, :], in0=g1[:], in1=y[:], op=ALU.mult))
                    kb.op("act", [zf.res], [zb.res], lambda e: e.activation(out=zb[:, t, :], in_=zf[:, t, :], func=AF.Copy))

            stage1(0)
            for pr in range(16):
                if pr + 1 < 16:
                    stage1(pr + 1)
                stage2(pr)
            for ct in range(4):
                pg = nps(g)
                for kt in range(4):
                    kb.op("pe", [gw.res, zb.res], [pg.res],
                          lambda e: e.matmul(pg[:, 0:TB], lhsT=gw[:, kt, tsl(ct)], rhs=zb[:, kt, :],
                                             start=(kt == 0), stop=(kt == 3)), inc=(kt == 3))
                s_ = sgl[ct % 2]
                kb.op("act", [pg.res, dcol.res], [s_.res],
                      lambda e: e.activation(out=s_[:], in_=pg[:, 0:TB], func=AF.Sigmoid, bias=dcol[:, 4 + ct:5 + ct],
                                             scale=1.0))
                os_ = ost[ct % 2]
                kb.op("dve", [s_.res, zf.res], [os_.res],
                      lambda e: e.tensor_tensor(out=os_[:], in0=zf[:, ct, :], in1=s_[:], op=ALU.mult))
                kb.dma("sp", dr["o_scr"][12 + ct, :, ts_], os_[:], [os_.res], [g.scr_res["o"]])
    g.psn = 8
    barrier(kb)


WEIGHT_SPECS = {
    "ada_w": (D, 6 * D), "ada_b": (6 * D,), "norm_g": (4, D), "w_in": (D, INW), "diff_lambda": (4, 64),
    "ml_conv": (4, 1024), "ml_gate_b": (2, 4), "gla_wa2": (16, 256), "gla_ba": (256,),
    "s5_a_re": (32, 64), "s5_a_im": (32, 64), "s5_log_dt": (32,), "s5_b_re": (32, 64, 16), "s5_b_im": (32, 64, 16),
    "s5_c_re": (32, 16, 64), "s5_c_im": (32, 16, 64), "s5_d": (32, 16), "s5_glu_w": (512, 512), "s5_glu_b": (512,),
    "w_branch": (4, 512, D), "w_gate": (4, D, D), "b_gate": (4, D), "w_out": (D, D),
    "ffn_w_in": (D, 2 * FFH), "ffn_w_out": (FFH, D),
}
CONST_SPECS = {"c_ident": (128, 128), "c_tri": (128, 128), "c_biasT": (4, 5, 128, 512), "c_bias_far": (1, 4),
               "c_half": (128, 2), "c_pm": (128, 128)}


SWAP_LANES = True
BF_WEIGHTS = ("w_in", "w_gate", "w_branch", "w_out", "ffn_w_in")


def emit_weight_cast(kb, g, dr, nc, NL):
    for name in BF_WEIGHTS:
        src = dr[name]
        shp = list(src.shape)
        dst = nc.dram_tensor(name + "_bf", shp, BF16, kind="Internal").ap()
        if len(shp) == 4:
            s2 = src.rearrange("l i r c -> (l i r) c")
            d2 = dst.rearrange("l i r c -> (l i r) c")
        else:
            s2 = src.rearrange("l r c -> (l r) c")
            d2 = dst.rearrange("l r c -> (l r) c")
        rows = s2.shape[0]
        step = 512
        for r0 in range(0, rows, step):
            r1 = min(rows, r0 + step)
            kb.dma("pool", d2[r0:r1, :], s2[r0:r1, :], [], [g.w_res])
        dr[name] = dst
    NH = FFH // 128
    dst = nc.dram_tensor("ffn_w_out_r", [NL, NDT, 128, NH, 128], BF16, kind="Internal").ap()
    for l in range(NL):
        srcv = dr["ffn_w_out"][l].rearrange("(kt p) n -> p kt n", p=128)
        for dt in range(NDT):
            kb.dma("pool", dst[l, dt], srcv[:, :, tsl(dt)], [], [g.w_res])
    dr["ffn_w_out_r"] = dst
    barrier(kb)


def build_program(S, NL, debug=(), phases="ABCD"):
    nc = bass.Bass("TRN2", target_bir_lowering=False)
    kb = KB(nc)
    if SWAP_LANES:
        kb.add_lane("sp", "pool", 4)
        kb.add_lane("w", "sp", 4)
    else:
        kb.add_lane("sp", "sp", 4)
        kb.add_lane("w", "pool", 4)
    kb.add_lane("pool", "pool", 4)
    g = G()
    dr = {}
    dr["x"] = nc.dram_tensor("x", [S, D], F32, kind="ExternalInput").ap()
    dr["c"] = nc.dram_tensor("c", [1, D], F32, kind="ExternalInput").ap()
    for k, shp in WEIGHT_SPECS.items():
        dr[k] = nc.dram_tensor(k, [NL] + list(shp), F32, kind="ExternalInput").ap()
    for k, shp in CONST_SPECS.items():
        dr[k] = nc.dram_tensor(k, list(shp), F32, kind="ExternalInput").ap()
    dr["xres"] = nc.dram_tensor("out", [S, D], F32, kind="ExternalOutput").ap()
    scr = {"mod": ([NL, 6 * D], F32), "hT": ([16, 128, S], BF16), "qk": ([8, 128, S], BF16), "dav": ([S, 512], BF16),
           "mlqk": ([8, 128, S], F32), "mlv": ([S, 512], F32), "mlo": ([S, 512], F32), "mlif": ([S, 8], F32),
           "glqk": ([4, 128, S], F32), "gla": ([16, S], F32), "glv": ([S, 512], F32), "glr": ([S, 512], F32),
           "s5u": ([4, 128, S], F32), "o": ([16, 128, S], BF16)}
    g.scr_res = {}
    for k, (shp, dt) in scr.items():
        kind = "ExternalOutput" if k in debug else "Internal"
        dr[k + "_scr"] = nc.dram_tensor(k + "_scr", shp, dt, kind=kind).ap()
        g.scr_res[k] = Res()
    g.mod_res = g.scr_res["mod"]
    g.x_res = Res()
    alloc_psum(kb, g)
    emit_consts(kb, g, dr)
    nchunk = max(1, S // 1024)
    rows = S // nchunk
    for i in range(nchunk):
        kb.dma("sp", dr["xres"][i * rows:(i + 1) * rows, :], dr["x"][i * rows:(i + 1) * rows, :], [], [g.x_res])
    emit_mods(kb, g, dr, NL)
    g.w_res = Res()
    emit_weight_cast(kb, g, dr, nc, NL)
    for l in range(NL):
        with kb.scope():
            L = emit_layer_consts(kb, g, dr, l)
            if "A" in phases:
                emit_phaseA(kb, g, dr, L, l, S)
            if "B" in phases or "a" in phases:
                emit_attn(kb, g, dr, l, S)
            if "B" in phases or "m" in phases:
                emit_mlstm(kb, g, dr, l, S)
            if "B" in phases or "g" in phases:
                emit_gla(kb, g, dr, l, S)
            if "B" in phases or "s" in phases:
                emit_s5(kb, g, dr, l, S)
            if "C" in phases:
                emit_phaseC1(kb, g, dr, L, l, S)
            if "D" in phases:
                emit_phaseC2(kb, g, dr, L, l, S)
        barrier(kb)
    barrier(kb)
    return nc


def t5_bucket(rel):
    n = np.maximum(-rel, 0)
    exact = N_BUCKETS // 2
    nf = np.maximum(n, 1).astype(np.float32)
    large = exact + (np.log(nf / exact) / math.log(MAX_DISTANCE / exact) * (N_BUCKETS - exact)).astype(np.int32)
    return np.where(n < exact, n, np.minimum(large, N_BUCKETS - 1))


def host_consts(rel_bias):
    rb = np.asarray(rel_bias, np.float32)
    k = np.arange(128)[:, None]
    q = np.arange(128)[None, :]
    biasT = np.empty((4, 5, 128, 512), np.float32)
    qq = np.arange(512)[None, :]
    for di, dmin in enumerate(range(-3, 2)):
        rel = k - (dmin * 128 + qq)
        idx = t5_bucket(rel)
        for h in range(4):
            tab = rb[:, h][idx]
            biasT[h, di] = np.where(rel <= 0, tab, np.float32(-30000.0))
    c = {
        "c_ident": np.eye(128, dtype=np.float32),
        "c_tri": np.triu(np.ones((128, 128), np.float32)),
        "c_biasT": biasT,
        "c_bias_far": np.ascontiguousarray(rb[N_BUCKETS - 1:N_BUCKETS, :]),
        "c_half": np.stack([(np.arange(128) < 64), (np.arange(128) >= 64)], axis=1).astype(np.float32),
    }
    g8 = (np.arange(128) // 16)[:, None]
    j = (np.arange(128) // 64)[None, :]
    c["c_pm"] = ((g8 % 2) == j).astype(np.float32)
    return c


_PROG = {}


def kernel(**inputs):
    x = np.asarray(inputs["x"], np.float32)
    B, S, _ = x.shape
    NL = np.asarray(inputs["ada_w"]).shape[0]
    key = (S, NL)
    if key not in _PROG:
        _PROG[key] = build_program(S, NL)
    nc = _PROG[key]
    consts = host_consts(inputs["rel_bias"])
    shared = {k: np.ascontiguousarray(np.asarray(inputs[k], np.float32)) for k in WEIGHT_SPECS}
    shared.update(consts)
    in_maps = []
    for b in range(B):
        m = dict(shared)
        m["x"] = np.ascontiguousarray(x[b])
        m["c"] = np.ascontiguousarray(np.asarray(inputs["c"], np.float32)[b:b + 1])
        in_maps.append(m)
    res = run_bass_kernel_spmd(nc, in_maps, core_ids=list(range(B)))
    return np.stack([np.asarray(r["out"], np.float32) for r in res.results], axis=0)
```

```python
import math
import contextlib
import numpy as np
import concourse.bass as bass
import concourse.mybir as mybir
from concourse.bass_utils import run_bass_kernel_spmd

F32 = mybir.dt.float32
BF16 = mybir.dt.bfloat16
AF = mybir.ActivationFunctionType
ALU = mybir.AluOpType
AX = mybir.AxisListType

D = 2048
NDT = 16
DEPTH = 4
FFH = 5632
INW = 5656
EPS = 1e-6
N_BUCKETS = 32
MAX_DISTANCE = 128


class Res:
    __slots__ = ("w", "r")

    def __init__(self):
        self.w = None
        self.r = {}


class KB:
    def __init__(self, nc):
        self.nc = nc
        self.eng = {"pe": nc.tensor, "act": nc.scalar, "dve": nc.vector, "pool": nc.gpsimd, "sp": nc.sync}
        self.sems = []
        self.own = {}
        self.cnt = {}
        self.known = {e: {} for e in self.eng}
        for e in ("pe", "act", "dve", "pool"):
            self.own[e] = self._newsem("c_" + e)
            self.cnt[e] = 0
        self.lanes = {}
        self.uid = 0
        self.stack = None

    def _newsem(self, name):
        self.sems.append(self.nc.alloc_semaphore(name))
        return len(self.sems) - 1

    def add_lane(self, name, eng, k):
        self.lanes[name] = {"eng": eng, "sems": [self._newsem(f"l_{name}{i}") for i in range(k)], "n": 0}

    def _deps(self, reads, writes):
        toks = []
        for r in reads:
            if r.w is not None:
                toks.append(r.w)
        for w in writes:
            if w.w is not None:
                toks.append(w.w)
            toks.extend(w.r.items())
        return toks

    def _wait(self, e, toks):
        kn = self.known[e]
        eng = self.eng[e]
        for si, v in toks:
            if e == "pe" and si == self.own.get("pe"):
                continue
            if kn.get(si, 0) < v:
                eng.wait_ge(self.sems[si], v)
                kn[si] = v

    def _mark(self, tok, reads, writes):
        si, v = tok
        for r in reads:
            if r.r.get(si, 0) < v:
                r.r[si] = v
        for w in writes:
            w.w = tok
            w.r = {}

    def op(self, e, reads, writes, fn, inc=True, fuse=None):
        toks = self._deps(reads, writes)
        if fuse is None:
            fuse = (e != "pe")
        last = None
        if fuse:
            kn = self.known[e]
            need = {}
            for si, v in toks:
                if kn.get(si, 0) < v and need.get(si, 0) < v:
                    need[si] = v
            if need:
                items = list(need.items())
                last = items[-1]
                toks = items[:-1]
            else:
                toks = []
        self._wait(e, toks)
        if last is not None:
            n0 = self.nc.n_instructions()
        ins = fn(self.eng[e])
        if last is not None:
            assert self.nc.n_instructions() - n0 == 1, "fused wait on a multi-instruction builder"
            ins._wait_ge(self.sems[last[0]], last[1])
            self.known[e][last[0]] = last[1]
        if inc:
            self.cnt[e] += 1
            ins.then_inc(self.sems[self.own[e]], 1)
            tok = (self.own[e], self.cnt[e])
        else:
            tok = (self.own[e], self.cnt[e] + 1)
        self._mark(tok, reads, writes)

    def dma(self, lane, out, in_, reads, writes, **kw):
        L = self.lanes[lane]
        e = L["eng"]
        n = L["n"]
        k = len(L["sems"])
        toks = self._deps(reads, writes)
        si = L["sems"][n % k]
        if n >= k:
            toks.append((si, 16 * (n // k)))
        self._wait(e, toks)
        self.eng[e].dma_start(out=out, in_=in_, **kw).then_inc(self.sems[si], 16)
        L["n"] = n + 1
        self._mark((si, 16 * (n // k + 1)), reads, writes)

    def final_wait(self, e, ress):
        toks = []
        for r in ress:
            if r.w is not None:
                toks.append(r.w)
        self._wait(e, toks)

    def sb(self, shape, dt, name=None):
        self.uid += 1
        nm = f"{name or 't'}_{self.uid}"
        if self.stack is not None:
            return self.stack.enter_context(self.nc.sbuf_tensor(nm, list(shape), dt))
        return self.nc.alloc_sbuf_tensor(nm, list(shape), dt)

    def scope(self):
        return _Scope(self)

    def dram(self, name, shape, dt, kind="Internal"):
        return self.nc.dram_tensor(name, list(shape), dt, kind=kind).ap()


class _Scope:
    def __init__(self, kb):
        self.kb = kb

    def __enter__(self):
        self.prev = self.kb.stack
        self.kb.stack = contextlib.ExitStack()
        return self

    def __exit__(self, *a):
        self.kb.stack.close()
        self.kb.stack = self.prev
        return False


class T:
    def __init__(self, t):
        self.t = t
        self.res = Res()

    def __getitem__(self, k):
        return self.t[k]


def barrier(kb):
    toks = []
    for e in ("pe", "act", "dve", "pool"):
        if kb.cnt[e] > 0:
            toks.append((kb.own[e], kb.cnt[e]))
    for L in kb.lanes.values():
        n = L["n"]
        k = len(L["sems"])
        for j in range(min(n, k)):
            m = n - 1 - j
            toks.append((L["sems"][m % k], 16 * (m // k + 1)))
    for e in kb.eng:
        kb._wait(e, toks)


class G:
    pass


def alloc_psum(kb, g):
    g.ps = []
    for i in range(8):
        g.ps.append(T(kb.nc.alloc_psum_tensor(f"psb{i}", [128, 512], F32)))
    g.psi = 0
    g.psn = 8


def nps(g):
    p = g.ps[g.psi % g.psn]
    g.psi += 1
    return p


def tsl(i, n=128):
    return slice(i * n, (i + 1) * n)


def emit_consts(kb, g, dr):
    nc = kb.nc
    g.ident = T(kb.sb([128, 128], F32, "ident"))
    g.tri = T(kb.sb([128, 128], F32, "tri"))
    g.ones = T(kb.sb([128, 128], F32, "ones"))
    g.tri_b = T(kb.sb([128, 128], BF16, "trib"))
    kb.dma("sp", g.ident[:], dr["c_ident"], [], [g.ident.res])
    kb.dma("sp", g.tri[:], dr["c_tri"], [], [g.tri.res])
    kb.op("dve", [], [g.ones.res], lambda e: e.memset(g.ones[:], 1.0))
    kb.op("dve", [g.tri.res], [g.tri_b.res], lambda e: e.tensor_copy(out=g.tri_b[:], in_=g.tri[:]))


def emit_mods(kb, g, dr, NL):
    nc = kb.nc
    with kb.scope():
        cs = T(kb.sb([128, NDT], F32, "cs"))
        sg = T(kb.sb([128, NDT], F32, "sg"))
        kb.dma("sp", cs[:], dr["c"].rearrange("o (kt p) -> p (o kt)", p=128), [], [cs.res],
               allow_slow_non_contiguous=True)
        kb.op("act", [cs.res], [sg.res], lambda e: e.activation(out=sg[:], in_=cs[:], func=AF.Sigmoid))
        kb.op("dve", [cs.res, sg.res], [cs.res],
              lambda e: e.tensor_tensor(out=cs[:], in0=cs[:], in1=sg[:], op=ALU.mult))
        wt = [T(kb.sb([128, NDT, 512], F32, "adaw")) for _ in range(2)]
        bt = [T(kb.sb([1, 512], F32, "adab")) for _ in range(2)]
        ot = [T(kb.sb([1, 512], F32, "adao")) for _ in range(2)]
        it = 0
        for l in range(NL):
            wv = dr["ada_w"][l].rearrange("(kt p) n -> p kt n", p=128)
            for cg in range(6 * D // 512):
                w = wt[it % 2]
                b = bt[it % 2]
                o = ot[it % 2]
                kb.dma("sp", w[:], wv[:, :, tsl(cg, 512)], [], [w.res])
                kb.dma("sp", b[:], dr["ada_b"][l:l + 1, tsl(cg, 512)], [], [b.res])
                p = nps(g)
                for kt in range(NDT):
                    kb.op("pe", [cs.res, w.res], [p.res],
                          lambda e, kt=kt: e.matmul(p[0:1, :], lhsT=cs[:, kt:kt + 1], rhs=w[:, kt, :],
                                                    start=(kt == 0), stop=(kt == NDT - 1)),
                          inc=(kt == NDT - 1))
                kb.op("dve", [p.res, b.res], [o.res],
                      lambda e: e.tensor_tensor(out=o[:], in0=p[0:1, :], in1=b[:], op=ALU.add))
                kb.dma("sp", dr["mod_scr"][l:l + 1, tsl(cg, 512)], o[:], [o.res], [g.mod_res])
                it += 1
    barrier(kb)


def emit_layer_consts(kb, g, dr, l):
    L = G()
    modrow = T(kb.sb([96, 128], F32, "modrow"))
    grow = T(kb.sb([64, 128], F32, "grow"))
    L.modT = T(kb.sb([128, 96], F32, "modT"))
    L.gT = T(kb.sb([128, 64], F32, "gT"))
    kb.dma("sp", modrow[:], dr["mod_scr"][l].rearrange("(j p) -> j p", p=128), [g.mod_res], [modrow.res])
    kb.dma("sp", grow[:], dr["norm_g"][l].rearrange("i (j p) -> (i j) p", p=128), [], [grow.res])
    p = nps(g)
    kb.op("pe", [modrow.res, g.ident.res], [p.res],
          lambda e: e.transpose(p[:, 0:96], modrow[:], g.ident[0:96, 0:96]))
    kb.op("dve", [p.res], [L.modT.res], lambda e: e.tensor_copy(out=L.modT[:], in_=p[:, 0:96]))
    p2 = nps(g)
    kb.op("pe", [grow.res, g.ident.res], [p2.res],
          lambda e: e.transpose(p2[:, 0:64], grow[:], g.ident[0:64, 0:64]))
    kb.op("dve", [p2.res], [L.gT.res], lambda e: e.tensor_copy(out=L.gT[:], in_=p2[:, 0:64]))
    L.gs_m = T(kb.sb([128, NDT], F32, "gsm"))
    L.gs_f = T(kb.sb([128, NDT], F32, "gsf"))
    for (dst, gi, sc) in ((L.gs_m, 0, 16), (L.gs_f, 2, 64)):
        kb.op("dve", [L.modT.res, L.gT.res], [dst.res],
              lambda e, dst=dst, gi=gi, sc=sc: e.scalar_tensor_tensor(
                  out=dst[:], in0=L.modT[:, sc:sc + 16], scalar=1.0, in1=L.gT[:, gi * 16:gi * 16 + 16],
                  op0=ALU.add, op1=ALU.mult))
    L.sh_m = T(kb.sb([128, NDT], F32, "shm"))
    L.sh_f = T(kb.sb([128, NDT], F32, "shf"))
    kb.op("dve", [L.modT.res], [L.sh_m.res], lambda e: e.tensor_copy(out=L.sh_m[:], in_=L.modT[:, 0:16]))
    kb.op("dve", [L.modT.res], [L.sh_f.res], lambda e: e.tensor_copy(out=L.sh_f[:], in_=L.modT[:, 48:64]))
    return L


def rstd_from_ss(kb, ss, n, tmp):
    kb.op("dve", [ss.res], [ss.res],
          lambda e: e.tensor_scalar(out=ss[:], in0=ss[:], scalar1=1.0 / n, scalar2=EPS, op0=ALU.mult, op1=ALU.add))
    kb.op("act", [ss.res], [ss.res], lambda e: e.activation(out=ss[:], in_=ss[:], func=AF.Sqrt))
    kb.op("dve", [ss.res], [ss.res], lambda e: e.reciprocal(out=ss[:], in_=ss[:]))


def norm_transpose_block(kb, g, xts, junk, gs, sh, hT, hres, stat):
    for tt in range(4):
        x = xts[tt]
        ss = stat[tt]
        kb.op("act", [x.res], [junk.res, ss.res],
              lambda e: e.activation(out=junk[:], in_=x[:], func=AF.Square, accum_out=ss[:]), fuse=False)
        rstd_from_ss(kb, ss, D, None)
        kb.op("dve", [x.res, ss.res], [x.res],
              lambda e: e.tensor_scalar(out=x[:], in0=x[:], scalar1=ss[:, 0:1], scalar2=None, op0=ALU.mult))
    for dt in range(NDT):
        p = nps(g)
        for tt in range(4):
            kb.op("pe", [xts[tt].res, g.ident.res], [p.res],
                  lambda e: e.transpose(p[:, tsl(tt)], xts[tt][:, tsl(dt)], g.ident[:]), inc=(tt == 3))
        kb.op("act", [p.res, gs.res, sh.res], [hres[dt]],
              lambda e: e.activation(out=hT[:, dt, :], in_=p[:], func=AF.Identity,
                                     scale=gs[:, dt:dt + 1], bias=sh[:, dt:dt + 1]))


PROJ_F = [
    ("qk", 0, 1024), ("mlqk", 1536, 1024), ("glqk", 3592, 512), ("gla", 5128, 16), ("s5u", 5144, 512)]
PROJ_T = [
    ("dav", 1024, 512), ("mlv", 2560, 512), ("mlo", 3072, 512), ("mlif", 3584, 8), ("glv", 4104, 512),
    ("glr", 4616, 512)]


def emit_phaseA(kb, g, dr, L, l, S):
    nc = kb.nc
    NB = S // 512
    xv = dr["xres"]
    with kb.scope():
        xt = [[T(kb.sb([128, D], F32, "xt")) for _ in range(4)] for _ in range(2)]
        junk = T(kb.sb([128, D], F32, "junk"))
        stat = [T(kb.sb([128, 1], F32, "stat")) for _ in range(4)]
        hT = T(kb.sb([128, NDT, 512], BF16, "hT"))
        hres = [Res() for _ in range(NDT)]
        wT = [T(kb.sb([128, NDT, 512], BF16, "wT")) for _ in range(3)]
        stf = [T(kb.sb([128, 512], F32, "stf")) for _ in range(4)]
        stb = [T(kb.sb([128, 512], BF16, "stb")) for _ in range(3)]
        cnt = {"wf": 0, "wt": 0, "sf": 0, "sb": 0, "ev": 0}
        win = dr["w_in"][l].rearrange("(kt p) n -> p kt n", p=128)

        def load_x(b):
            for tt in range(4):
                t = xt[b % 2][tt]
                kb.dma("sp", t[:], xv[b * 512 + tt * 128: b * 512 + (tt + 1) * 128, :], [g.x_res], [t.res])

        def evac(out_ap, in_ap, reads, writes, scale=None):
            cnt["ev"] += 1
            if cnt["ev"] % 2 == 0:
                kb.op("act", reads, writes,
                      lambda e: e.activation(out=out_ap, in_=in_ap, func=AF.Copy,
                                             scale=(1.0 if scale is None else scale)))
            else:
                if scale is None:
                    kb.op("dve", reads, writes, lambda e: e.tensor_copy(out=out_ap, in_=in_ap))
                else:
                    kb.op("dve", reads, writes,
                          lambda e: e.tensor_scalar(out=out_ap, in0=in_ap, scalar1=scale, scalar2=None,
                                                    op0=ALU.mult))

        load_x(0)
        for b in range(NB):
            if b + 1 < NB:
                load_x(b + 1)
            xts = xt[b % 2]
            norm_transpose_block(kb, g, xts, junk, L.gs_m, L.sh_m, hT, hres, stat)
            kb.dma("sp", dr["hT_scr"].rearrange("dt p s -> p dt s")[:, :, tsl(b, 512)], hT[:], hres,
                   [g.scr_res["hT"]])
            ts0 = b * 512
            for (name, c0, ncols) in PROJ_F:
                for gi in range((ncols + 511) // 512):
                    gcols = min(512, ncols - gi * 512)
                    w = wT[cnt["wt"] % 3]
                    cnt["wt"] += 1
                    kb.dma("w", w[:, :, 0:gcols], win[:, :, c0 + gi * 512: c0 + gi * 512 + gcols], [], [w.res])
                    for tq in range((gcols + 127) // 128):
                        ti = gi * 4 + tq
                        nc_ = min(128, gcols - tq * 128)
                        p = nps(g)
                        for kt in range(NDT):
                            kb.op("pe", [w.res, hres[kt]], [p.res],
                                  lambda e: e.matmul(p[0:nc_, :], lhsT=w[:, kt, tq * 128:tq * 128 + nc_], rhs=hT[:, kt, :],
                                                     start=(kt == 0), stop=(kt == NDT - 1)), inc=(kt == NDT - 1))
                        if name == "qk":
                            s = stb[cnt["sb"] % 3]
                            cnt["sb"] += 1
                            sc = 0.125 if ti < 4 else None
                            evac(s[:], p[:], [p.res], [s.res], scale=sc)
                            kb.dma("sp", dr["qk_scr"][ti, :, ts0:ts0 + 512], s[:], [s.res], [g.scr_res["qk"]])
                        else:
                            s = stf[cnt["sf"] % 4]
                            cnt["sf"] += 1
                            sc = 0.125 if (name == "glqk" and ti < 2) else None
                            evac(s[0:nc_, :], p[0:nc_, :], [p.res], [s.res], scale=sc)
                            dst = {"mlqk": dr["mlqk_scr"], "glqk": dr["glqk_scr"], "s5u": dr["s5u_scr"]}.get(name)
                            if name == "gla":
                                kb.dma("sp", dr["gla_scr"][:, ts0:ts0 + 512], s[0:16, :], [s.res], [g.scr_res["gla"]])
                            else:
                                kb.dma("sp", dst[ti, :, ts0:ts0 + 512], s[:], [s.res], [g.scr_res[name]])
            for (name, c0, ncols) in PROJ_T:
                w = wT[cnt["wt"] % 3]
                cnt["wt"] += 1
                kb.dma("w", w[:, :, 0:ncols], win[:, :, c0:c0 + ncols], [], [w.res])
                for tt in range(4):
                    p = nps(g)
                    for kt in range(NDT):
                        kb.op("pe", [w.res, hres[kt]], [p.res],
                              lambda e: e.matmul(p[:, 0:ncols], lhsT=hT[:, kt, tsl(tt)], rhs=w[:, kt, 0:ncols],
                                                 start=(kt == 0), stop=(kt == NDT - 1)), inc=(kt == NDT - 1))
                    r0 = ts0 + tt * 128
                    if name == "dav":
                        s = stb[cnt["sb"] % 3]
                        cnt["sb"] += 1
                    else:
                        s = stf[cnt["sf"] % 4]
                        cnt["sf"] += 1
                    evac(s[:, 0:ncols], p[:, 0:ncols], [p.res], [s.res])
                    kb.dma("sp", dr[name + "_scr"][r0:r0 + 128, :], s[:, 0:ncols], [s.res], [g.scr_res[name]])
    barrier(kb)


def make_gg(kb, g, dr, l, gi, mi, dst, tmp):
    kb.dma("sp", dst[:], dr["norm_g"][l, gi:gi + 1, :].partition_broadcast(128), [], [dst.res])
    kb.dma("sp", tmp[:], dr["mod_scr"][l:l + 1, mi * D:(mi + 1) * D].partition_broadcast(128),
           [g.mod_res], [tmp.res])
    kb.op("dve", [tmp.res], [dst.res],
          lambda e: e.tensor_tensor(out=dst[:], in0=dst[:], in1=tmp[:], op=ALU.mult))


def epilogue(kb, g, dr, yT, yres, gg, b, bufs):
    xe, junk, ss4, ss, tmp = bufs
    for tt in range(4):
        r0 = b * 512 + tt * 128
        x = xe[tt % 2]
        kb.dma("sp", x[:], dr["xres"][r0:r0 + 128, :], [g.x_res], [x.res])
        banks = [nps(g) for _ in range(4)]
        for cg in range(4):
            for dd in range(4):
                kb.op("pe", [yres[cg * 4 + dd], g.ident.res], [banks[cg].res],
                      lambda e: e.transpose(banks[cg][:, tsl(dd)], yT[:, cg * 4 + dd, tsl(tt)], g.ident[:]),
                      inc=(dd == 3))
            kb.op("act", [banks[cg].res], [junk.res, ss4.res],
                  lambda e: e.activation(out=junk[:], in_=banks[cg][:], func=AF.Square,
                                         accum_out=ss4[:, cg:cg + 1]), fuse=False)
        kb.op("dve", [ss4.res], [ss.res], lambda e: e.reduce_sum(out=ss[:], in_=ss4[:], axis=AX.X))
        rstd_from_ss(kb, ss, D, None)
        for cg in range(4):
            t = tmp[cg % 2]
            kb.op("dve", [banks[cg].res, ss.res, gg.res], [t.res],
                  lambda e: e.scalar_tensor_tensor(out=t[:], in0=banks[cg][:], scalar=ss[:, 0:1],
                                                   in1=gg[:, tsl(cg, 512)], op0=ALU.mult, op1=ALU.mult))
            kb.op("pool", [t.res], [x.res],
                  lambda e: e.tensor_tensor(out=x[:, tsl(cg, 512)], in0=x[:, tsl(cg, 512)], in1=t[:], op=ALU.add))
        kb.dma("sp", dr["xres"][r0:r0 + 128, :], x[:], [x.res], [g.x_res])


def emit_phaseC1(kb, g, dr, L, l, S):
    nc = kb.nc
    NB = S // 512
    with kb.scope():
        hT = [T(kb.sb([128, NDT, 512], BF16, "hT"))] * 2
        oT = [T(kb.sb([128, NDT, 512], BF16, "oT"))] * 2
        mg = T(kb.sb([128, NDT, 512], BF16, "mg"))
        mres = [Res() for _ in range(NDT)]
        wg = [T(kb.sb([128, NDT, 512], BF16, "wg")) for _ in range(2)]
        wb = [T(kb.sb([128, 4, 512], BF16, "wb")) for _ in range(2)]
        yT = T(kb.sb([128, NDT, 512], F32, "yT"))
        yres = [Res() for _ in range(NDT)]
        acc = [T(kb.sb([128, 512], F32, "acc")) for _ in range(4)]
        sg = [T(kb.sb([128, 512], F32, "sg")) for _ in range(2)]
        tm = [T(kb.sb([128, 512], F32, "tm")) for _ in range(2)]
        bgrow = T(kb.sb([64, 128], F32, "bgrow"))
        bgT = T(kb.sb([128, 64], F32, "bgT"))
        xe = [T(kb.sb([128, D], F32, "xe")) for _ in range(2)]
        junk = T(kb.sb([128, 512], F32, "junk"))
        ss4 = T(kb.sb([128, 4], F32, "ss4"))
        ss = T(kb.sb([128, 1], F32, "ss"))
        ebufs = (xe, junk, ss4, ss, tm)
        gg_m = T(kb.sb([128, D], F32, "ggm"))
        make_gg(kb, g, dr, l, 1, 2, gg_m, xe[0])
        kb.dma("sp", bgrow[:], dr["b_gate"][l].rearrange("i (j p) -> (i j) p", p=128), [], [bgrow.res])
        p = nps(g)
        kb.op("pe", [bgrow.res, g.ident.res], [p.res],
              lambda e: e.transpose(p[:, 0:64], bgrow[:], g.ident[0:64, 0:64]))
        kb.op("dve", [p.res], [bgT.res], lambda e: e.tensor_copy(out=bgT[:], in_=p[:, 0:64]))
        nw = 0

        def load_blk(b):
            kb.dma("sp", hT[b % 2][:], dr["hT_scr"].rearrange("dt p s -> p dt s")[:, :, tsl(b, 512)],
                   [g.scr_res["hT"]], [hT[b % 2].res])
            kb.dma("sp", oT[b % 2][:], dr["o_scr"].rearrange("dt p s -> p dt s")[:, :, tsl(b, 512)],
                   [g.scr_res["o"]], [oT[b % 2].res])

        for b in range(NB):
            load_blk(b)
            h = hT[b % 2]
            o = oT[b % 2]
            for cg in range(4):
                for i in range(4):
                    w = wg[nw % 2]
                    w2 = wb[nw % 2]
                    nw += 1
                    kb.dma("w", w[:], dr["w_gate"][l, i].rearrange("(kt p) n -> p kt n", p=128)[:, :, tsl(cg, 512)],
                           [], [w.res])
                    kb.dma("w", w2[:], dr["w_branch"][l, i].rearrange("(kt p) n -> p kt n", p=128)[:, :, tsl(cg, 512)],
                           [], [w2.res])
                    for dd in range(4):
                        dt = cg * 4 + dd
                        pg = nps(g)
                        pb = nps(g)
                        for kt in range(NDT):
                            kb.op("pe", [w.res, h.res], [pg.res],
                                  lambda e: e.matmul(pg[:], lhsT=w[:, kt, tsl(dd)], rhs=h[:, kt, :],
                                                     start=(kt == 0), stop=(kt == NDT - 1)), inc=(kt == NDT - 1))
                        for kk in range(4):
                            kb.op("pe", [w2.res, o.res], [pb.res],
                                  lambda e: e.matmul(pb[:], lhsT=w2[:, kk, tsl(dd)], rhs=o[:, i * 4 + kk, :],
                                                     start=(kk == 0), stop=(kk == 3)), inc=(kk == 3))
                        s_ = sg[(i * 4 + dd) % 2]
                        kb.op("act", [pg.res, bgT.res], [s_.res],
                              lambda e: e.activation(out=s_[:], in_=pg[:], func=AF.Sigmoid,
                                                     bias=bgT[:, i * 16 + dt:i * 16 + dt + 1], scale=1.0))
                        a = acc[dd]
                        if i == 0:
                            kb.op("dve", [s_.res, pb.res], [a.res],
                                  lambda e: e.tensor_tensor(out=a[:], in0=s_[:], in1=pb[:], op=ALU.mult))
                        else:
                            kb.op("dve", [s_.res, pb.res], [s_.res],
                                  lambda e: e.tensor_tensor(out=s_[:], in0=s_[:], in1=pb[:], op=ALU.mult))
                            if i < 3:
                                kb.op("pool", [s_.res, a.res], [a.res],
                                      lambda e: e.tensor_tensor(out=a[:], in0=a[:], in1=s_[:], op=ALU.add))
                            else:
                                kb.op("pool", [s_.res, a.res], [mres[dt]],
                                      lambda e: e.tensor_tensor(out=mg[:, dt, :], in0=a[:], in1=s_[:], op=ALU.add))
            for cg in range(4):
                w = wg[nw % 2]
                nw += 1
                kb.dma("w", w[:], dr["w_out"][l].rearrange("(kt p) n -> p kt n", p=128)[:, :, tsl(cg, 512)],
                       [], [w.res])
                for dd in range(4):
                    dt = cg * 4 + dd
                    pm = nps(g)
                    for kt in range(NDT):
                        kb.op("pe", [w.res, mres[kt]], [pm.res],
                              lambda e: e.matmul(pm[:], lhsT=w[:, kt, tsl(dd)], rhs=mg[:, kt, :],
                                                 start=(kt == 0), stop=(kt == NDT - 1)), inc=(kt == NDT - 1))
                    kb.op("act", [pm.res], [yres[dt]],
                          lambda e: e.activation(out=yT[:, dt, :], in_=pm[:], func=AF.Copy))
            epilogue(kb, g, dr, yT, yres, gg_m, b, ebufs)
    barrier(kb)


def emit_phaseC2(kb, g, dr, L, l, S):
    nc = kb.nc
    NB = S // 512
    NH = FFH // 128
    with kb.scope():
        xt = [T(kb.sb([128, D], F32, "xt")) for _ in range(4)]
        junkb = T(kb.sb([128, D], BF16, "junkb"))
        stat = [T(kb.sb([128, 1], F32, "stat")) for _ in range(4)]
        hT = T(kb.sb([128, NDT, 512], BF16, "h2T"))
        hres = [Res() for _ in range(NDT)]
        uT = T(kb.sb([128, NH, 512], BF16, "uT"))
        ures = [Res() for _ in range(NH)]
        wa = [T(kb.sb([128, NDT, 256], BF16, "wa")) for _ in range(2)]
        wgt = [T(kb.sb([128, NDT, 256], BF16, "wgt")) for _ in range(2)]
        wo = [T(kb.sb([128, NH, 128], BF16, "wo")) for _ in range(2)]
        yT = T(kb.sb([128, NDT, 512], F32, "yT"))
        yres = [Res() for _ in range(NDT)]
        sa = [T(kb.sb([128, 512], F32, "sa")) for _ in range(2)]
        tm = [T(kb.sb([128, 512], F32, "tm")) for _ in range(2)]
        junk = T(kb.sb([128, 512], F32, "junk"))
        ss4 = T(kb.sb([128, 4], F32, "ss4"))
        ss = T(kb.sb([128, 1], F32, "ss"))
        ebufs = ([xt[0], xt[1]], junk, ss4, ss, tm)
        gg_f = T(kb.sb([128, D], F32, "ggf"))
        make_gg(kb, g, dr, l, 3, 5, gg_f, xt[0])
        wi = dr["ffn_w_in"][l].rearrange("(kt p) n -> p kt n", p=128)
        wov = dr["ffn_w_out_r"][l]
        nw = 0
        nwo = 0
        for b in range(NB):
            for tt in range(4):
                kb.dma("sp", xt[tt][:], dr["xres"][b * 512 + tt * 128: b * 512 + (tt + 1) * 128, :],
                       [g.x_res], [xt[tt].res])
            norm_transpose_block(kb, g, xt, junkb, L.gs_f, L.sh_f, hT, hres, stat)
            for j in range(NH):
                if j % 2 == 0:
                    w1 = wa[nw % 2]
                    w2 = wgt[nw % 2]
                    nw += 1
                    kb.dma("w", w1[:], wi[:, :, tsl(j // 2, 256)], [], [w1.res])
                    kb.dma("w", w2[:], wi[:, :, FFH + (j // 2) * 256: FFH + (j // 2 + 1) * 256], [], [w2.res])
                jj = tsl(j % 2)
                pa = nps(g)
                pg = nps(g)
                for kt in range(NDT):
                    kb.op("pe", [w1.res, hres[kt]], [pa.res],
                          lambda e: e.matmul(pa[:], lhsT=w1[:, kt, jj], rhs=hT[:, kt, :],
                                             start=(kt == 0), stop=(kt == NDT - 1)), inc=(kt == NDT - 1))
                for kt in range(NDT):
                    kb.op("pe", [w2.res, hres[kt]], [pg.res],
                          lambda e: e.matmul(pg[:], lhsT=w2[:, kt, jj], rhs=hT[:, kt, :],
                                             start=(kt == 0), stop=(kt == NDT - 1)), inc=(kt == NDT - 1))
                s_ = sa[j % 2]
                kb.op("act", [pa.res], [s_.res], lambda e: e.activation(out=s_[:], in_=pa[:], func=AF.Silu))
                kb.op("dve", [s_.res, pg.res], [ures[j]],
                      lambda e: e.tensor_tensor(out=uT[:, j, :], in0=s_[:], in1=pg[:], op=ALU.mult))
            for dt in range(NDT):
                w = wo[nwo % 2]
                nwo += 1
                kb.dma("w", w[:], wov[dt], [], [w.res])
                py = nps(g)
                for kt in range(NH):
                    kb.op("pe", [w.res, ures[kt]], [py.res],
                          lambda e: e.matmul(py[:], lhsT=w[:, kt, :], rhs=uT[:, kt, :],
                                             start=(kt == 0), stop=(kt == NH - 1)), inc=(kt == NH - 1))
                kb.op("act", [py.res], [yres[dt]],
                      lambda e: e.activation(out=yT[:, dt, :], in_=py[:], func=AF.Copy))
            epilogue(kb, g, dr, yT, yres, gg_f, b, ebufs)
    barrier(kb)


def emit_attn(kb, g, dr, l, S):
    nc = kb.nc
    NQB = S // 512
    NKT = S // 128
    lam_init = 0.8 - 0.6 * math.exp(-0.3 * l)
    g.psn = 4
    with kb.scope():
        QT = T(kb.sb([128, S], BF16, "QT"))
        KT = T(kb.sb([128, S], BF16, "KT"))
        V = T(kb.sb([128, NKT, 129], BF16, "V"))
        biasT = T(kb.sb([128, 5, 512], F32, "biasT"))
        cb = T(kb.sb([128, 4], F32, "cb"))
        lp = T(kb.sb([128, 256], F32, "lp"))
        lam = T(kb.sb([128, 4], F32, "lam"))
        pT = [T(kb.sb([128, 512], BF16, "pT")) for _ in range(3)]
        tmpf = [T(kb.sb([128, 512], F32, "tmpf")) for _ in range(2)]
        o0 = [T(kb.sb([128, 128], F32, "o0")) for _ in range(4)]
        o1 = [T(kb.sb([128, 128], F32, "o1")) for _ in range(2)]
        rec = [T(kb.sb([128, 2], F32, "rec")) for _ in range(2)]
        ssq = [T(kb.sb([128, 1], F32, "ssq")) for _ in range(2)]
        junk = T(kb.sb([128, 128], F32, "junk"))
        ost = [T(kb.sb([128, 512], BF16, "ost")) for _ in range(2)]
        kb.dma("sp", lp[:], dr["diff_lambda"][l:l + 1].rearrange("o a d -> o (a d)").partition_broadcast(128),
               [], [lp.res])
        kb.dma("sp", cb[:], dr["c_bias_far"].partition_broadcast(128), [], [cb.res])
        kb.op("dve", [lp.res], [lp.res],
              lambda e: e.tensor_tensor(out=lp[:, 0:64], in0=lp[:, 0:64], in1=lp[:, 64:128], op=ALU.mult))
        kb.op("dve", [lp.res], [lp.res],
              lambda e: e.tensor_tensor(out=lp[:, 128:192], in0=lp[:, 128:192], in1=lp[:, 192:256], op=ALU.mult))
        kb.op("dve", [lp.res], [lam.res], lambda e: e.reduce_sum(out=lam[:, 0:1], in_=lp[:, 0:64], axis=AX.X))
        kb.op("dve", [lp.res], [lam.res], lambda e: e.reduce_sum(out=lam[:, 1:2], in_=lp[:, 128:192], axis=AX.X))
        kb.op("act", [lam.res], [lam.res], lambda e: e.activation(out=lam[:, 0:2], in_=lam[:, 0:2], func=AF.Exp))
        kb.op("dve", [lam.res], [lam.res],
              lambda e: e.tensor_tensor(out=lam[:, 2:3], in0=lam[:, 1:2], in1=lam[:, 0:1], op=ALU.subtract))
        kb.op("dve", [lam.res], [lam.res],
              lambda e: e.tensor_scalar(out=lam[:, 2:3], in0=lam[:, 2:3], scalar1=-lam_init, scalar2=None,
                                        op0=ALU.add))
        kb.op("dve", [], [V.res], lambda e: e.memset(V[:, :, 128:129], 1.0))
        npt = 0
        nev = 0
        for h in range(4):
            kb.dma("sp", QT[:], dr["qk_scr"][h], [g.scr_res["qk"]], [QT.res])
            kb.dma("sp", KT[:], dr["qk_scr"][4 + h], [g.scr_res["qk"]], [KT.res])
            kb.dma("sp", V[:, :, 0:128], dr["dav_scr"][:, tsl(h)].rearrange("(kt p) c -> p kt c", p=128),
                   [g.scr_res["dav"]], [V.res])
            kb.dma("sp", biasT[:], dr["c_biasT"][h].rearrange("a k q -> k a q"), [], [biasT.res])
            for qb in range(NQB):
                for m in range(2):
                    ms = slice(m * 64, (m + 1) * 64)
                    O = [g.ps[4 + qs] for qs in range(4)]
                    nk = 4 * (qb + 1)

                    def qk(kt):
                        sT = nps(g)
                        kb.op("pe", [KT.res, QT.res], [sT.res],
                              lambda e: e.matmul(sT[:], lhsT=KT[ms, tsl(kt)], rhs=QT[ms, tsl(qb, 512)],
                                                 start=True, stop=True))
                        return sT

                    sT_next = qk(0)
                    for kt in range(nk):
                        sT = sT_next
                        if kt + 1 < nk:
                            sT_next = qk(kt + 1)
                        dmin = qb * 4 - kt
                        p_ = pT[npt % 3]
                        npt += 1
                        if dmin >= 2:
                            kb.op("act", [sT.res, cb.res], [p_.res],
                                  lambda e: e.activation(out=p_[:], in_=sT[:], func=AF.Exp,
                                                         bias=cb[:, h:h + 1], scale=1.0))
                        else:
                            q0 = max(0, -dmin) * 128
                            tf = tmpf[kt % 2]
                            kb.op("dve", [sT.res, biasT.res], [tf.res],
                                  lambda e: e.tensor_tensor(out=tf[:, q0:512], in0=sT[:, q0:512],
                                                            in1=biasT[:, dmin + 3, q0:512], op=ALU.add))
                            kb.op("act", [tf.res], [p_.res],
                                  lambda e: e.activation(out=p_[:, q0:512], in_=tf[:, q0:512], func=AF.Exp))
                        for qs in range(4):
                            dl = dmin + qs
                            if dl < 0:
                                continue
                            kb.op("pe", [p_.res, V.res], [O[qs].res],
                                  lambda e: e.matmul(O[qs][:, 0:129], lhsT=p_[:, tsl(qs)], rhs=V[:, kt, :],
                                                     start=(kt == 0), stop=(dl == 0)), inc=(dl == 0))
                    for qs in range(4):
                        r = rec[qs % 2]
                        kb.op("dve", [O[qs].res], [r.res],
                              lambda e: e.reciprocal(out=r[:, 0:1], in_=O[qs][:, 128:129]))
                        if m == 0:
                            kb.op("dve", [O[qs].res, r.res], [o0[qs].res],
                                  lambda e: e.tensor_scalar(out=o0[qs][:], in0=O[qs][:, 0:128], scalar1=r[:, 0:1],
                                                            scalar2=None, op0=ALU.mult))
                        else:
                            oo = o1[qs % 2]
                            sq = ssq[qs % 2]
                            kb.op("dve", [r.res, lam.res], [r.res],
                                  lambda e: e.tensor_tensor(out=r[:, 1:2], in0=r[:, 0:1], in1=lam[:, 2:3], op=ALU.mult))
                            kb.op("dve", [O[qs].res, r.res, o0[qs].res], [oo.res],
                                  lambda e: e.scalar_tensor_tensor(out=oo[:], in0=O[qs][:, 0:128], scalar=r[:, 1:2],
                                                                   in1=o0[qs][:], op0=ALU.mult, op1=ALU.add))
                            kb.op("act", [oo.res], [junk.res, sq.res],
                                  lambda e: e.activation(out=junk[:], in_=oo[:], func=AF.Square, accum_out=sq[:]), fuse=False)
                            rstd_from_ss(kb, sq, 128, None)
                            kb.op("dve", [oo.res, sq.res], [oo.res],
                                  lambda e: e.tensor_scalar(out=oo[:], in0=oo[:], scalar1=sq[:, 0:1],
                                                            scalar2=(1.0 - lam_init), op0=ALU.mult, op1=ALU.mult))
                            pt_ = nps(g)
                            kb.op("pe", [oo.res, g.ident.res], [pt_.res],
                                  lambda e: e.transpose(pt_[:, 0:128], oo[:], g.ident[:]))
                            os_ = ost[qb % 2]
                            kb.op("dve", [pt_.res], [os_.res],
                                  lambda e: e.tensor_copy(out=os_[:, tsl(qs)], in_=pt_[:, 0:128]))
                    if m == 1:
                        os_ = ost[qb % 2]
                        kb.dma("sp", dr["o_scr"][h, :, tsl(qb, 512)], os_[:], [os_.res], [g.scr_res["o"]])
    g.psn = 8
    barrier(kb)


def emit_mlstm(kb, g, dr, l, S):
    nc = kb.nc
    NCH = S // 128
    SEG = min(2048, S)
    KSC = 128 ** -0.5
    with kb.scope():
        class HB:
            pass
        HBs = []
        for _hh in range(2):
            b_ = HB()
            b_.qT = T(kb.sb([128, S], BF16, "qT"))
            b_.kT = T(kb.sb([128, S], BF16, "kT"))
            b_.ktok = T(kb.sb([128, NCH, 128], BF16, "ktok"))
            b_.vpp = T(kb.sb([128, NCH, 129], BF16, "vpp"))
            b_.ost_ = [T(kb.sb([128, 8, 128], F32, "osg")) for _ in range(2)]
            b_.sTm = [T(kb.sb([128, 128], BF16, "sTm")) for _ in range(2)]
            b_.Cf = T(kb.sb([128, 129], F32, "Cf"))
            b_.Cb = [T(kb.sb([128, 129], BF16, "Cb")) for _ in range(2)]
            b_.dd = [T(kb.sb([128, 4], F32, "dd")) for _ in range(2)]
            b_.ho = [T(kb.sb([128, 128], F32, "ho")) for _ in range(2)]
            b_.ost = [T(kb.sb([128, 512], BF16, "ost")) for _ in range(2)]
            HBs.append(b_)
        raw = T(kb.sb([128, SEG + 3], F32, "raw"))
        yc = T(kb.sb([128, SEG], F32, "yc"))
        cw = T(kb.sb([128, 4, 8], F32, "cw"))
        gif = T(kb.sb([128, NCH, 8], F32, "gif"))
        gbb = T(kb.sb([128, 8], F32, "gbb"))
        spt = T(kb.sb([128, NCH, 4], F32, "spt"))
        tot = T(kb.sb([128, NCH, 4], F32, "tot"))
        A = T(kb.sb([128, NCH, 4], F32, "A"))
        R = T(kb.sb([128, NCH, 4], F32, "R"))
        EL = T(kb.sb([128, NCH, 4], F32, "EL"))
        vst = [T(kb.sb([128, 8, 128], F32, "vst")) for _ in range(2)]
        for j in range(4):
            kb.dma("sp", cw[:, j, :], dr["ml_conv"][l, j].rearrange("(t p) -> p t", p=128), [], [cw.res],
                   allow_slow_non_contiguous=True)
        kb.dma("sp", gbb[:], dr["ml_gate_b"][l:l + 1].rearrange("o a h -> o (a h)").partition_broadcast(128),
               [], [gbb.res])
        kb.dma("sp", gif[:], dr["mlif_scr"].rearrange("(c p) j -> p c j", p=128), [g.scr_res["mlif"]], [gif.res])
        for j in range(8):
            kb.op("dve", [gif.res, gbb.res], [gif.res],
                  lambda e: e.tensor_scalar(out=gif[:, :, j], in0=gif[:, :, j], scalar1=gbb[:, j:j + 1],
                                            scalar2=None, op0=ALU.add))
        kb.op("act", [gif.res], [spt.res],
              lambda e: e.activation(out=spt[:], in_=gif[:, :, 4:8], func=AF.Exp, scale=-1.0))
        kb.op("act", [spt.res], [spt.res],
              lambda e: e.activation(out=spt[:], in_=spt[:], func=AF.Ln, bias=1.0, scale=1.0))
        pc = nps(g)
        ptot = nps(g)
        spf = spt[:].rearrange("p c h -> p (c h)")
        kb.op("pe", [spt.res, g.tri.res], [pc.res],
              lambda e: e.matmul(pc[:, 0:NCH * 4], lhsT=g.tri[:], rhs=spf, start=True, stop=True))
        kb.op("pe", [spt.res, g.ones.res], [ptot.res],
              lambda e: e.matmul(ptot[:, 0:NCH * 4], lhsT=g.ones[:], rhs=spf, start=True, stop=True))
        totf = tot[:].rearrange("p c h -> p (c h)")
        Af = A[:].rearrange("p c h -> p (c h)")
        Rf = R[:].rearrange("p c h -> p (c h)")
        ELf = EL[:].rearrange("p c h -> p (c h)")
        kb.op("dve", [ptot.res], [tot.res], lambda e: e.tensor_copy(out=totf, in_=ptot[:, 0:NCH * 4]))
        kb.op("dve", [pc.res, tot.res], [R.res],
              lambda e: e.tensor_tensor(out=Rf, in0=pc[:, 0:NCH * 4], in1=totf, op=ALU.subtract))
        kb.op("dve", [R.res, gif.res], [A.res],
              lambda e: e.tensor_tensor(out=A[:], in0=R[:], in1=gif[:, :, 0:4], op=ALU.add))
        kb.op("act", [A.res], [A.res], lambda e: e.activation(out=Af, in_=Af, func=AF.Exp))
        kb.op("act", [R.res], [R.res], lambda e: e.activation(out=Rf, in_=Rf, func=AF.Exp, scale=-1.0))
        kb.op("act", [tot.res], [EL.res], lambda e: e.activation(out=ELf, in_=totf, func=AF.Exp, scale=-1.0))

        def conv_seg(tile_idx, s0, is_k, B_):
            qT, kT, ktok = B_.qT, B_.kT, B_.ktok
            n = min(SEG, S - s0)
            src = dr["mlqk_scr"][tile_idx]
            if s0 == 0:
                kb.op("dve", [], [raw.res], lambda e: e.memset(raw[:, 0:3], 0.0))
                kb.dma("sp", raw[:, 3:3 + n], src[:, 0:n], [g.scr_res["mlqk"]], [raw.res])
            else:
                kb.dma("sp", raw[:, 0:3 + n], src[:, s0 - 3:s0 + n], [g.scr_res["mlqk"]], [raw.res])
            kb.op("dve", [raw.res, cw.res], [yc.res],
                  lambda e: e.tensor_scalar(out=yc[:, 0:n], in0=raw[:, 3:3 + n], scalar1=cw[:, 3, tile_idx:tile_idx + 1],
                                            scalar2=None, op0=ALU.mult))
            for j in (2, 1, 0):
                kb.op("dve", [raw.res, cw.res, yc.res], [yc.res],
                      lambda e: e.scalar_tensor_tensor(out=yc[:, 0:n], in0=raw[:, j:j + n],
                                                       scalar=cw[:, j, tile_idx:tile_idx + 1], in1=yc[:, 0:n],
                                                       op0=ALU.mult, op1=ALU.add))
            if not is_k:
                kb.op("act", [yc.res], [qT.res],
                      lambda e: e.activation(out=qT[:, s0:s0 + n], in_=yc[:, 0:n], func=AF.Silu))
            else:
                kb.op("act", [yc.res], [yc.res],
                      lambda e: e.activation(out=yc[:, 0:n], in_=yc[:, 0:n], func=AF.Silu))
                kb.op("dve", [yc.res], [kT.res],
                      lambda e: e.tensor_scalar(out=kT[:, s0:s0 + n], in0=yc[:, 0:n], scalar1=KSC, scalar2=None,
                                                op0=ALU.mult))
                for c4 in range(n // 512):
                    p = nps(g)
                    for cc in range(4):
                        kb.op("pe", [yc.res, g.ident.res], [p.res],
                              lambda e: e.transpose(p[:, tsl(cc)], yc[:, c4 * 512 + cc * 128: c4 * 512 + (cc + 1) * 128],
                                                    g.ident[:]), inc=(cc == 3))
                    c0 = s0 // 128 + c4 * 4
                    kb.op("act", [p.res], [ktok.res],
                          lambda e: e.activation(out=ktok[:, c0:c0 + 4, :].rearrange("p c d -> p (c d)"), in_=p[:],
                                                 func=AF.Copy, scale=KSC))

        def prologue(h, B_):
            vpp = B_.vpp
            for s0 in range(0, S, SEG):
                conv_seg(h, s0, False, B_)
            for s0 in range(0, S, SEG):
                conv_seg(4 + h, s0, True, B_)
            for c8 in range(NCH // 8 if NCH >= 8 else 1):
                ncg = min(8, NCH)
                vs = vst[c8 % 2]
                kb.dma("sp", vs[:, 0:ncg, :],
                       dr["mlv_scr"][c8 * 1024: c8 * 1024 + ncg * 128, tsl(h)].rearrange("(c p) d -> p c d", p=128),
                       [g.scr_res["mlv"]], [vs.res])
                for cc in range(ncg):
                    c = c8 * 8 + cc
                    kb.op("dve", [vs.res, A.res], [vpp.res],
                          lambda e: e.tensor_scalar(out=vpp[:, c, 0:128], in0=vs[:, cc, :], scalar1=A[:, c, h:h + 1],
                                                    scalar2=None, op0=ALU.mult))
            kb.op("dve", [A.res], [vpp.res], lambda e: e.tensor_copy(out=vpp[:, :, 128], in_=A[:, :, h]))
        def chunk_step(h, B_, c):
                qT, kT, ktok, vpp = B_.qT, B_.kT, B_.ktok, B_.vpp
                ost_, sTm, Cf, Cb, dd, ho, ost = B_.ost_, B_.sTm, B_.Cf, B_.Cb, B_.dd, B_.ho, B_.ost
                cs_ = tsl(c)
                if c % 8 == 0:
                    ncg = min(8, NCH)
                    og = ost_[(c // 8) % 2]
                    kb.dma("sp", og[:, 0:ncg, :],
                           dr["mlo_scr"][c * 128: (c + ncg) * 128, tsl(h)].rearrange("(c p) d -> p c d", p=128),
                           [g.scr_res["mlo"]], [og.res])
                    kb.op("act", [og.res], [og.res],
                          lambda e: e.activation(out=og[:, 0:ncg, :], in_=og[:, 0:ncg, :], func=AF.Sigmoid))
                og = ost_[(c // 8) % 2]
                ps_s = nps(g)
                kb.op("pe", [kT.res, qT.res], [ps_s.res],
                      lambda e: e.matmul(ps_s[:, 0:128], lhsT=kT[:, cs_], rhs=qT[:, cs_], start=True, stop=True))
                sm = sTm[c % 2]
                kb.op("dve", [ps_s.res, g.tri.res], [sm.res],
                      lambda e: e.tensor_tensor(out=sm[:], in0=ps_s[:, 0:128], in1=g.tri[:], op=ALU.mult))
                cb_ = Cb[c % 2]
                if c > 0:
                    kb.op("dve", [Cf.res, EL.res], [cb_.res],
                          lambda e: e.tensor_scalar(out=cb_[:], in0=Cf[:], scalar1=EL[:, c, h:h + 1], scalar2=None,
                                                    op0=ALU.mult))
                pn = nps(g)
                kb.op("pe", [sm.res, vpp.res], [pn.res],
                      lambda e: e.matmul(pn[:, 0:129], lhsT=sm[:], rhs=vpp[:, c, :], start=True, stop=(c == 0)),
                      inc=(c == 0))
                if c > 0:
                    kb.op("pe", [qT.res, cb_.res], [pn.res],
                          lambda e: e.matmul(pn[:, 0:129], lhsT=qT[:, cs_], rhs=cb_[:], start=False, stop=True))
                pd = nps(g)
                kb.op("pe", [ktok.res, vpp.res], [pd.res],
                      lambda e: e.matmul(pd[:, 0:129], lhsT=ktok[:, c, :], rhs=vpp[:, c, :], start=True, stop=True))
                if c == 0:
                    kb.op("dve", [pd.res], [Cf.res], lambda e: e.tensor_copy(out=Cf[:], in_=pd[:, 0:129]))
                else:
                    kb.op("dve", [pd.res, Cf.res, EL.res], [Cf.res],
                          lambda e: e.scalar_tensor_tensor(out=Cf[:], in0=Cf[:], scalar=EL[:, c, h:h + 1],
                                                           in1=pd[:, 0:129], op0=ALU.mult, op1=ALU.add))
                d_ = dd[c % 2]
                kb.op("dve", [pn.res, R.res], [d_.res],
                      lambda e: e.tensor_tensor(out=d_[:, 0:1], in0=pn[:, 128:129], in1=R[:, c, h:h + 1], op=ALU.mult))
                kb.op("dve", [d_.res], [d_.res],
                      lambda e: e.scalar_tensor_tensor(out=d_[:, 1:2], in0=d_[:, 0:1], scalar=-1.0, in1=d_[:, 0:1],
                                                       op0=ALU.mult, op1=ALU.max))
                kb.op("dve", [d_.res], [d_.res],
                      lambda e: e.tensor_scalar(out=d_[:, 1:2], in0=d_[:, 1:2], scalar1=1.0, scalar2=None,
                                                op0=ALU.max))
                kb.op("dve", [d_.res], [d_.res], lambda e: e.reciprocal(out=d_[:, 2:3], in_=d_[:, 1:2]))
                kb.op("dve", [d_.res, R.res], [d_.res],
                      lambda e: e.tensor_tensor(out=d_[:, 3:4], in0=d_[:, 2:3], in1=R[:, c, h:h + 1], op=ALU.mult))
                ho_ = ho[c % 2]
                kb.op("dve", [pn.res, d_.res, og.res], [ho_.res],
                      lambda e: e.scalar_tensor_tensor(out=ho_[:], in0=pn[:, 0:128], scalar=d_[:, 3:4],
                                                       in1=og[:, c % 8, :], op0=ALU.mult, op1=ALU.mult))
                pt_ = nps(g)
                kb.op("pe", [ho_.res, g.ident.res], [pt_.res],
                      lambda e: e.transpose(pt_[:, 0:128], ho_[:], g.ident[:]))
                os_ = ost[(c // 4) % 2]
                kb.op("act", [pt_.res], [os_.res],
                      lambda e: e.activation(out=os_[:, tsl(c % 4)], in_=pt_[:, 0:128], func=AF.Copy))
                if c % 4 == 3:
                    kb.dma("sp", dr["o_scr"][4 + h, :, tsl(c // 4, 512)], os_[:], [os_.res], [g.scr_res["o"]])

        for h0 in (0, 2):
            for hh in range(2):
                prologue(h0 + hh, HBs[hh])
            for c in range(NCH):
                for hh in range(2):
                    chunk_step(h0 + hh, HBs[hh], c)
    barrier(kb)


def emit_gla(kb, g, dr, l, S):
    nc = kb.nc
    NCH = S // 128
    SEG = min(2048, S)
    with kb.scope():
        qtT = T(kb.sb([128, S], BF16, "qtT"))
        ktT = T(kb.sb([128, S], BF16, "ktT"))
        ktok = T(kb.sb([128, NCH, 128], BF16, "gktok"))
        vb = [T(kb.sb([128, NCH, 128], BF16, "gvb")) for _ in range(2)]
        ELt = T(kb.sb([128, NCH], F32, "ELt"))
        wa2 = T(kb.sb([16, 256], F32, "wa2"))
        nba = T(kb.sb([128, 2], F32, "nba"))
        glaT = T(kb.sb([16, SEG], F32, "glaT"))
        spg = T(kb.sb([128, SEG], F32, "spg"))
        csg = T(kb.sb([128, SEG], F32, "csg"))
        rm = T(kb.sb([128, SEG], F32, "rm"))
        Ee = T(kb.sb([128, SEG], F32, "Ee"))
        rawq = T(kb.sb([128, SEG], F32, "rawq"))
        ktf = T(kb.sb([128, SEG], F32, "ktf"))
        srg = [T(kb.sb([128, 8, 128], F32, "srg")) for _ in range(2)]
        sTm = [T(kb.sb([128, 128], BF16, "gsTm")) for _ in range(2)]
        U = T(kb.sb([128, 128], F32, "U"))
        Sb = [T(kb.sb([128, 128], BF16, "Sb")) for _ in range(2)]
        ssq = [T(kb.sb([128, 1], F32, "gss")) for _ in range(2)]
        junk = T(kb.sb([128, 128], F32, "gjunk"))
        ho = [T(kb.sb([128, 128], F32, "gho")) for _ in range(2)]
        ost = [[T(kb.sb([128, 512], BF16, "gost")) for _ in range(2)] for _ in range(2)]
        kb.dma("sp", wa2[:], dr["gla_wa2"][l], [], [wa2.res])
        kb.dma("sp", nba[:], dr["gla_ba"][l].rearrange("(t p) -> p t", p=128), [], [nba.res],
               allow_slow_non_contiguous=True)
        kb.op("dve", [nba.res], [nba.res],
              lambda e: e.tensor_scalar(out=nba[:], in0=nba[:], scalar1=-1.0, scalar2=None, op0=ALU.mult))
        kb.op("dve", [], [rm.res], lambda e: e.memset(rm[:], 1.0))
        kb.op("dve", [rm.res], [rm.res],
              lambda e: e.memset(rm[:].rearrange("p (c t) -> p c t", t=128)[:, :, 0:1], 0.0))
        for hp in range(2):
            for s0 in range(0, S, SEG):
                n = min(SEG, S - s0)
                kb.dma("sp", glaT[:, 0:n], dr["gla_scr"][:, s0:s0 + n], [g.scr_res["gla"]], [glaT.res])
                kb.dma("sp", rawq[:, 0:n], dr["glqk_scr"][hp, :, s0:s0 + n], [g.scr_res["glqk"]], [rawq.res])
                kb.dma("sp", ktf[:, 0:n], dr["glqk_scr"][2 + hp, :, s0:s0 + n], [g.scr_res["glqk"]], [ktf.res])
                for b5 in range(n // 512):
                    pz = nps(g)
                    kb.op("pe", [wa2.res, glaT.res], [pz.res],
                          lambda e: e.matmul(pz[:], lhsT=wa2[:, tsl(hp)], rhs=glaT[:, tsl(b5, 512)],
                                             start=True, stop=True))
                    kb.op("act", [pz.res, nba.res], [spg.res],
                          lambda e: e.activation(out=spg[:, tsl(b5, 512)], in_=pz[:], func=AF.Exp,
                                                 bias=nba[:, hp:hp + 1], scale=-1.0))
                kb.op("act", [spg.res], [spg.res],
                      lambda e: e.activation(out=spg[:, 0:n], in_=spg[:, 0:n], func=AF.Ln, bias=1.0, scale=1.0))
                kb.op("dve", [spg.res, rm.res], [csg.res],
                      lambda e: e.tensor_tensor_scan(out=csg[:, 0:n], data0=rm[:, 0:n], data1=spg[:, 0:n],
                                                     initial=0.0, op0=ALU.mult, op1=ALU.add))
                kb.op("act", [csg.res], [Ee.res],
                      lambda e: e.activation(out=Ee[:, 0:n], in_=csg[:, 0:n], func=AF.Exp, scale=-1.0 / 16.0))
                kb.op("dve", [rawq.res, Ee.res], [qtT.res],
                      lambda e: e.tensor_tensor(out=qtT[:, s0:s0 + n], in0=rawq[:, 0:n], in1=Ee[:, 0:n], op=ALU.mult))
                c0 = s0 // 128
                kb.op("dve", [Ee.res], [ELt.res],
                      lambda e: e.tensor_copy(out=ELt[:, c0:c0 + n // 128],
                                              in_=Ee[:, 0:n].rearrange("p (c t) -> p c t", t=128)[:, :, 127]))
                kb.op("act", [csg.res], [Ee.res],
                      lambda e: e.activation(out=Ee[:, 0:n], in_=csg[:, 0:n], func=AF.Exp, scale=1.0 / 16.0))
                kb.op("dve", [ktf.res, Ee.res], [ktf.res],
                      lambda e: e.tensor_tensor(out=ktf[:, 0:n], in0=ktf[:, 0:n], in1=Ee[:, 0:n], op=ALU.mult))
                kb.op("act", [ktf.res], [ktT.res],
                      lambda e: e.activation(out=ktT[:, s0:s0 + n], in_=ktf[:, 0:n], func=AF.Copy))
                for c4 in range(n // 512):
                    p = nps(g)
                    for cc in range(4):
                        kb.op("pe", [ktf.res, g.ident.res], [p.res],
                              lambda e: e.transpose(p[:, tsl(cc)], ktf[:, c4 * 512 + cc * 128: c4 * 512 + (cc + 1) * 128],
                                                    g.ident[:]), inc=(cc == 3))
                    cc0 = c0 + c4 * 4
                    kb.op("dve", [p.res], [ktok.res],
                          lambda e: e.tensor_copy(out=ktok[:, cc0:cc0 + 4, :].rearrange("p c d -> p (c d)"), in_=p[:]))
            for hh in range(2):
                hd = hp * 2 + hh
                kb.dma("pool", vb[hh][:], dr["glv_scr"][:, tsl(hd)].rearrange("(c p) d -> p c d", p=128),
                       [g.scr_res["glv"]], [vb[hh].res])
            for c in range(NCH):
                cs_ = tsl(c)
                for hh in range(2):
                    hd = hp * 2 + hh
                    hs = slice(hh * 64, (hh + 1) * 64)
                    if c % 8 == 0:
                        ncg = min(8, NCH)
                        sr = srg[hh]
                        kb.dma("sp", sr[:, 0:ncg, :],
                               dr["glr_scr"][c * 128:(c + ncg) * 128, tsl(hd)].rearrange("(c p) d -> p c d", p=128),
                               [g.scr_res["glr"]], [sr.res])
                        kb.op("act", [sr.res], [sr.res],
                              lambda e: e.activation(out=sr[:, 0:ncg, :], in_=sr[:, 0:ncg, :], func=AF.Silu))
                    sr = srg[hh]
                    ps_s = nps(g)
                    kb.op("pe", [ktT.res, qtT.res], [ps_s.res],
                          lambda e: e.matmul(ps_s[:, 0:128], lhsT=ktT[hs, cs_], rhs=qtT[hs, cs_], start=True, stop=True))
                    sm = sTm[hh]
                    kb.op("dve", [ps_s.res, g.tri.res], [sm.res],
                          lambda e: e.tensor_tensor(out=sm[:], in0=ps_s[:, 0:128], in1=g.tri[:], op=ALU.mult))
                    sb_ = Sb[hh]
                    if c > 0:
                        kb.op("dve", [U.res, ELt.res], [sb_.res],
                              lambda e: e.tensor_scalar(out=sb_[hs, :], in0=U[hs, :], scalar1=ELt[hs, c - 1:c],
                                                        scalar2=None, op0=ALU.mult))
                    po = nps(g)
                    kb.op("pe", [sm.res, vb[hh].res], [po.res],
                          lambda e: e.matmul(po[:, 0:128], lhsT=sm[:], rhs=vb[hh][:, c, :], start=True, stop=(c == 0)),
                          inc=(c == 0))
                    if c > 0:
                        kb.op("pe", [qtT.res, sb_.res], [po.res],
                              lambda e: e.matmul(po[:, 0:128], lhsT=qtT[hs, cs_], rhs=sb_[hs, :], start=False, stop=True))
                    pd = nps(g)
                    kb.op("pe", [ktok.res, vb[hh].res], [pd.res],
                          lambda e: e.matmul(pd[:, 0:128], lhsT=ktok[:, c, :], rhs=vb[hh][:, c, :], start=True, stop=True))
                    if c == 0:
                        kb.op("dve", [pd.res], [U.res], lambda e: e.tensor_copy(out=U[hs, :], in_=pd[hs, 0:128]))
                    else:
                        kb.op("dve", [pd.res, U.res, ELt.res], [U.res],
                              lambda e: e.scalar_tensor_tensor(out=U[hs, :], in0=U[hs, :], scalar=ELt[hs, c - 1:c],
                                                               in1=pd[hs, 0:128], op0=ALU.mult, op1=ALU.add))
                    sq = ssq[hh]
                    kb.op("act", [po.res], [junk.res, sq.res],
                          lambda e: e.activation(out=junk[:], in_=po[:, 0:128], func=AF.Square, accum_out=sq[:]), fuse=False)
                    rstd_from_ss(kb, sq, 128, None)
                    ho_ = ho[hh]
                    kb.op("dve", [po.res, sq.res, sr.res], [ho_.res],
                          lambda e: e.scalar_tensor_tensor(out=ho_[:], in0=po[:, 0:128], scalar=sq[:, 0:1],
                                                           in1=sr[:, c % 8, :], op0=ALU.mult, op1=ALU.mult))
                    pt_ = nps(g)
                    kb.op("pe", [ho_.res, g.ident.res], [pt_.res],
                          lambda e: e.transpose(pt_[:, 0:128], ho_[:], g.ident[:]))
                    os_ = ost[hh][(c // 4) % 2]
                    kb.op("act", [pt_.res], [os_.res],
                          lambda e: e.activation(out=os_[:, tsl(c % 4)], in_=pt_[:, 0:128], func=AF.Copy))
                    if c % 4 == 3:
                        kb.dma("sp", dr["o_scr"][8 + hd, :, tsl(c // 4, 512)], os_[:], [os_.res], [g.scr_res["o"]])
    barrier(kb)


def emit_s5(kb, g, dr, l, S):
    nc = kb.nc
    TB = 256
    NBK = S // TB
    g.psn = 4
    with kb.scope():
        def small(name, shape=(128, 16)):
            return T(kb.sb(list(shape), F32, name))

        rows = [T(kb.sb([16, 128], F32, "s5row")) for _ in range(3)]
        ldt2 = T(kb.sb([16, 2], F32, "ldt2"))
        are, aim, dtt = small("are"), small("aim"), small("dtt")
        mu, Lc, Ls, t1, t2, t3 = small("mu"), small("Lc"), small("Ls"), small("t1"), small("t2"), small("t3")
        Rc, Rs, nRs = small("Rc"), small("Rs"), small("nRs")
        fre, fim, nfim = small("fre"), small("fim"), small("nfim")
        Tc = T(kb.sb([128, 16, TB], F32, "Tc"))
        Ts = T(kb.sb([128, 16, TB], F32, "Ts"))
        Tm = T(kb.sb([128, 16, TB], F32, "Tm"))
        half = T(kb.sb([128, 2], F32, "half"))
        pm = T(kb.sb([128, 128], F32, "pm"))
        bre = T(kb.sb([128, 16, 16], F32, "bre"))
        bim = T(kb.sb([128, 16, 16], F32, "bim"))
        bbr = T(kb.sb([128, 16, 16], F32, "bbr"))
        bbi = T(kb.sb([128, 16, 16], F32, "bbi"))
        btmp = T(kb.sb([128, 16, 16], F32, "btmp"))
        BR = T(kb.sb([16, 16, 2, 128], BF16, "BR"))
        BI = T(kb.sb([16, 16, 2, 128], BF16, "BI"))
        cin = T(kb.sb([128, 128], F32, "cin"))
        CRE = T(kb.sb([128, 16, 128], BF16, "CRE"))
        NCRE = T(kb.sb([128, 16, 128], BF16, "NCRE"))
        NCIM = T(kb.sb([128, 16, 128], BF16, "NCIM"))
        drow = T(kb.sb([8, 128], F32, "drow"))
        dcol = T(kb.sb([128, 8], F32, "dcol"))
        gw = T(kb.sb([128, 4, 512], BF16, "gw"))
        kb.dma("sp", half[:], dr["c_half"], [], [half.res])
        kb.dma("sp", pm[:], dr["c_pm"], [], [pm.res])
        kb.dma("sp", rows[0][:], dr["s5_a_re"][l].rearrange("(pr j) p -> pr (j p)", j=2), [], [rows[0].res])
        kb.dma("sp", rows[1][:], dr["s5_a_im"][l].rearrange("(pr j) p -> pr (j p)", j=2), [], [rows[1].res])
        kb.dma("sp", ldt2[:], dr["s5_log_dt"][l].rearrange("(pr j) -> pr j", j=2), [], [ldt2.res])
        for j in range(2):
            kb.op("dve", [ldt2.res], [rows[2].res],
                  lambda e: e.tensor_copy(out=rows[2][:, j * 64:(j + 1) * 64], in_=ldt2[:, j:j + 1].to_broadcast([16, 64])))
        for src, dst in ((rows[0], are), (rows[1], aim), (rows[2], dtt)):
            p = nps(g)
            kb.op("pe", [src.res, g.ident.res], [p.res],
                  lambda e: e.transpose(p[:, 0:16], src[:], g.ident[0:16, 0:16]))
            kb.op("dve", [p.res], [dst.res], lambda e: e.tensor_copy(out=dst[:], in_=p[:, 0:16]))
        kb.op("act", [dtt.res], [dtt.res], lambda e: e.activation(out=dtt[:], in_=dtt[:], func=AF.Exp))

        def tt(out, a, b, op):
            kb.op("dve", [a.res, b.res], [out.res], lambda e: e.tensor_tensor(out=out[:], in0=a[:], in1=b[:], op=op))

        tt(mu, dtt, are, ALU.mult)
        kb.op("act", [mu.res], [mu.res], lambda e: e.activation(out=mu[:], in_=mu[:], func=AF.Exp))
        tt(t1, dtt, aim, ALU.mult)
        kb.op("act", [t1.res], [Ls.res], lambda e: e.activation(out=Ls[:], in_=t1[:], func=AF.Sin, scale=1.0 / 16.0))
        hpi = small("hpi", (128, 1))
        kb.op("dve", [], [hpi.res], lambda e: e.memset(hpi[:], math.pi / 2))
        kb.op("act", [t1.res, hpi.res], [Lc.res],
              lambda e: e.activation(out=Lc[:], in_=t1[:], func=AF.Sin, scale=1.0 / 16.0, bias=hpi[:, 0:1]))

        def csq(c, s):
            tt(t2, c, s, ALU.mult)
            tt(c, c, c, ALU.mult)
            tt(t3, s, s, ALU.mult)
            tt(c, c, t3, ALU.subtract)
            kb.op("dve", [t2.res], [s.res],
                  lambda e: e.tensor_scalar(out=s[:], in0=t2[:], scalar1=2.0, scalar2=None, op0=ALU.mult))

        for _ in range(4):
            csq(Lc, Ls)
        nr, ni, den = small("nr"), small("ni"), small("den")
        tt(nr, mu, Lc, ALU.mult)
        kb.op("dve", [nr.res], [nr.res],
              lambda e: e.tensor_scalar(out=nr[:], in0=nr[:], scalar1=-1.0, scalar2=None, op0=ALU.add))
        tt(ni, mu, Ls, ALU.mult)
        tt(den, are, are, ALU.mult)
        tt(t2, aim, aim, ALU.mult)
        tt(den, den, t2, ALU.add)
        kb.op("dve", [den.res], [den.res], lambda e: e.reciprocal(out=den[:], in_=den[:]))
        tt(fre, nr, are, ALU.mult)
        tt(t2, ni, aim, ALU.mult)
        tt(fre, fre, t2, ALU.add)
        tt(fre, fre, den, ALU.mult)
        tt(fim, ni, are, ALU.mult)
        tt(t2, nr, aim, ALU.mult)
        tt(fim, fim, t2, ALU.subtract)
        tt(fim, fim, den, ALU.mult)
        kb.op("dve", [], [Tc.res], lambda e: e.memset(Tc[:, :, 0:1], 1.0))
        kb.op("dve", [], [Ts.res], lambda e: e.memset(Ts[:, :, 0:1], 0.0))
        kb.op("dve", [mu.res], [Tm.res],
              lambda e: e.tensor_copy(out=Tm[:], in_=mu[:].unsqueeze(2).to_broadcast([128, 16, TB])))
        tmpa = T(kb.sb([128, 16, TB // 2], F32, "tmpa"))
        n = 1
        while n < TB:
            lcb = Lc[:].unsqueeze(2).to_broadcast([128, 16, n])
            lsb = Ls[:].unsqueeze(2).to_broadcast([128, 16, n])
            kb.op("dve", [Tc.res, Lc.res], [Tc.res],
                  lambda e: e.tensor_tensor(out=Tc[:, :, n:2 * n], in0=Tc[:, :, 0:n], in1=lcb, op=ALU.mult))
            kb.op("dve", [Ts.res, Ls.res], [tmpa.res],
                  lambda e: e.tensor_tensor(out=tmpa[:, :, 0:n], in0=Ts[:, :, 0:n], in1=lsb, op=ALU.mult))
            kb.op("dve", [Tc.res, tmpa.res], [Tc.res],
                  lambda e: e.tensor_tensor(out=Tc[:, :, n:2 * n], in0=Tc[:, :, n:2 * n], in1=tmpa[:, :, 0:n],
                                            op=ALU.subtract))
            kb.op("dve", [Ts.res, Lc.res], [Ts.res],
                  lambda e: e.tensor_tensor(out=Ts[:, :, n:2 * n], in0=Ts[:, :, 0:n], in1=lcb, op=ALU.mult))
            kb.op("dve", [Tc.res, Ls.res], [tmpa.res],
                  lambda e: e.tensor_tensor(out=tmpa[:, :, 0:n], in0=Tc[:, :, 0:n], in1=lsb, op=ALU.mult))
            kb.op("dve", [Ts.res, tmpa.res], [Ts.res],
                  lambda e: e.tensor_tensor(out=Ts[:, :, n:2 * n], in0=Ts[:, :, n:2 * n], in1=tmpa[:, :, 0:n],
                                            op=ALU.add))
            csq(Lc, Ls)
            n *= 2
        kb.op("dve", [Lc.res], [Rc.res], lambda e: e.tensor_copy(out=Rc[:], in_=Lc[:]))
        kb.op("dve", [Ls.res], [Rs.res], lambda e: e.tensor_copy(out=Rs[:], in_=Ls[:]))
        kb.op("dve", [Ls.res], [nRs.res],
              lambda e: e.tensor_scalar(out=nRs[:], in0=Ls[:], scalar1=-1.0, scalar2=None, op0=ALU.mult))
        kb.dma("sp", bre[:], dr["s5_b_re"][l].rearrange("(pr j) p c -> (j p) pr c", j=2), [], [bre.res])
        kb.dma("sp", bim[:], dr["s5_b_im"][l].rearrange("(pr j) p c -> (j p) pr c", j=2), [], [bim.res])
        frb = fre[:].unsqueeze(2).to_broadcast([128, 16, 16])
        fib = fim[:].unsqueeze(2).to_broadcast([128, 16, 16])

        def t3op(out, a, b_ap, breads, op):
            kb.op("dve", [a.res] + breads, [out.res], lambda e: e.tensor_tensor(out=out[:], in0=a[:], in1=b_ap, op=op))

        t3op(bbr, bre, frb, [fre.res], ALU.mult)
        t3op(btmp, bim, fib, [fim.res], ALU.mult)
        t3op(bbr, bbr, btmp[:], [btmp.res], ALU.subtract)
        t3op(bbi, bim, frb, [fre.res], ALU.mult)
        t3op(btmp, bre, fib, [fim.res], ALU.mult)
        t3op(bbi, bbi, btmp[:], [btmp.res], ALU.add)
        for (srcb, dstB) in ((bbr, BR), (bbi, BI)):
            for j in range(2):
                kb.op("dve", [srcb.res, half.res], [btmp.res],
                      lambda e: e.tensor_scalar(out=btmp[:], in0=srcb[:], scalar1=half[:, j:j + 1], scalar2=None,
                                                op0=ALU.mult))
                for p4 in range(4):
                    p = nps(g)
                    for q in range(4):
                        kb.op("pe", [btmp.res, g.ident.res], [p.res],
                              lambda e: e.transpose(p[0:16, tsl(q)], btmp[:, p4 * 4 + q, :], g.ident[:]), inc=(q == 3))
                    kb.op("act", [p.res], [dstB.res],
                          lambda e: e.activation(out=dstB[:, p4 * 4:p4 * 4 + 4, j, :],
                                                 in_=p[0:16, :].rearrange("c (q m) -> c q m", q=4), func=AF.Copy))
        for (t_, z_) in ((CRE, 0), (NCRE, 0), (NCIM, 0)):
            kb.op("pool", [], [t_.res], lambda e: e.memset(t_[:], 0.0))
        for (srcname, outs) in (("s5_c_re", ((CRE, 1.0), (NCRE, -1.0))), ("s5_c_im", ((NCIM, -1.0),))):
            cv = dr[srcname][l].rearrange("g c p -> (g c) p")
            for gt in range(4):
                kb.dma("sp", cin[:, 0:64], cv[tsl(gt), :], [], [cin.res])
                kb.dma("sp", cin[:, 64:128], cv[tsl(gt), :], [], [cin.res])
                kb.op("dve", [cin.res, pm.res], [cin.res],
                      lambda e: e.tensor_tensor(out=cin[:], in0=cin[:], in1=pm[:], op=ALU.mult))
                p = nps(g)
                kb.op("pe", [cin.res, g.ident.res], [p.res], lambda e: e.transpose(p[:, 0:128], cin[:], g.ident[:]))
                for (dstC, sgn) in outs:
                    for q in range(4):
                        kb.op("act", [p.res], [dstC.res],
                              lambda e: e.activation(out=dstC[:, gt * 4 + q, q * 32:(q + 1) * 32],
                                                     in_=p[:, q * 32:(q + 1) * 32], func=AF.Copy, scale=sgn))
        kb.dma("sp", drow[0:4, :], dr["s5_d"][l].rearrange("(t g) c -> t (g c)", t=4), [], [drow.res])
        kb.dma("sp", drow[4:8, :], dr["s5_glu_b"][l].rearrange("(t p) -> t p", p=128), [], [drow.res])
        p = nps(g)
        kb.op("pe", [drow.res, g.ident.res], [p.res], lambda e: e.transpose(p[:, 0:8], drow[:], g.ident[0:8, 0:8]))
        kb.op("dve", [p.res], [dcol.res], lambda e: e.tensor_copy(out=dcol[:], in_=p[:, 0:8]))
        kb.dma("pool", gw[:], dr["s5_glu_w"][l].rearrange("(kt p) n -> p kt n", p=128), [], [gw.res])

        umm = [T(kb.sb([16, 32, TB], BF16, "umm")) for _ in range(2)]
        usk = [T(kb.sb([128, 4, TB], F32, "usk")) for _ in range(2)]
        dmb = [[T(kb.sb([128, TB], F32, "dm")) for _ in range(4)] for _ in range(2)]
        winb = [[T(kb.sb([128, TB], F32, "win")) for _ in range(2)] for _ in range(2)]
        wrt = [T(kb.sb([128, TB], F32, "wrt")) for _ in range(2)]
        wit = [T(kb.sb([128, TB], F32, "wit")) for _ in range(2)]
        PP = [[T(kb.sb([128, TB], BF16, "PP")) for _ in range(4)] for _ in range(2)]
        w0r = T(kb.sb([128, 16], F32, "w0r"))
        w0i = T(kb.sb([128, 16], F32, "w0i"))
        cr = [T(kb.sb([128, 2], F32, "cr")) for _ in range(2)]
        ysb = [T(kb.sb([128, TB], F32, "ysb")) for _ in range(2)]
        gt1 = [T(kb.sb([128, TB], F32, "gt1")) for _ in range(2)]
        zf = T(kb.sb([128, 4, TB], F32, "zf"))
        zb = T(kb.sb([128, 4, TB], BF16, "zb"))
        sgl = [T(kb.sb([128, TB], F32, "sgl")) for _ in range(2)]
        ost = [T(kb.sb([128, TB], BF16, "s5ost")) for _ in range(2)]
        w0r_res = [Res() for _ in range(16)]
        w0i_res = [Res() for _ in range(16)]
        kb.op("dve", [], w0r_res, lambda e: e.memset(w0r[:], 0.0))
        kb.op("dve", [], w0i_res, lambda e: e.memset(w0i[:], 0.0))
        uv = dr["s5u_scr"]
        npp = 0
        for bk in range(NBK):
            ts_ = slice(bk * TB, (bk + 1) * TB)
            um = umm[bk % 2]
            us = usk[bk % 2]
            kb.dma("pool", um[:], uv.rearrange("t (g c) s -> c (t g) s", c=16)[:, :, ts_], [g.scr_res["s5u"]], [um.res])
            kb.dma("sp", us[:], uv.rearrange("t p s -> p t s")[:, :, ts_], [g.scr_res["s5u"]], [us.res])
            def stage1(pr):
                pbr = nps(g)
                pbi = nps(g)
                for (pb_, B_) in ((pbr, BR), (pbi, BI)):
                    for j in range(2):
                        kb.op("pe", [B_.res, um.res], [pb_.res],
                              lambda e: e.matmul(pb_[:, 0:TB], lhsT=B_[:, pr, j, :], rhs=um[:, 2 * pr + j, :],
                                                 start=(j == 0), stop=(j == 1)), inc=(j == 1))
                d0, d1, d2, d3 = dmb[pr % 2]
                for (o_, tab, src) in ((d0, Tc, pbr), (d1, Ts, pbi), (d2, Tc, pbi), (d3, Ts, pbr)):
                    kb.op("dve", [tab.res, src.res], [o_.res],
                          lambda e: e.tensor_tensor(out=o_[:], in0=tab[:, pr, :], in1=src[:, 0:TB], op=ALU.mult))
                wr_in, wi_in = winb[pr % 2]
                kb.op("pool", [d0.res, d1.res], [wr_in.res],
                      lambda e: e.tensor_tensor(out=wr_in[:], in0=d0[:], in1=d1[:], op=ALU.add))
                kb.op("pool", [d2.res, d3.res], [wi_in.res],
                      lambda e: e.tensor_tensor(out=wi_in[:], in0=d2[:], in1=d3[:], op=ALU.subtract))
                wr = wrt[pr % 2]
                wi = wit[pr % 2]
                kb.op("dve", [Tm.res, wr_in.res, w0r_res[pr]], [wr.res],
                      lambda e: e.tensor_tensor_scan(out=wr[:], data0=Tm[:, pr, :], data1=wr_in[:],
                                                     initial=w0r[:, pr:pr + 1], op0=ALU.mult, op1=ALU.add))
                kb.op("dve", [Tm.res, wi_in.res, w0i_res[pr]], [wi.res],
                      lambda e: e.tensor_tensor_scan(out=wi[:], data0=Tm[:, pr, :], data1=wi_in[:],
                                                     initial=w0i[:, pr:pr + 1], op0=ALU.mult, op1=ALU.add))
                c_ = cr[pr % 2]
                kb.op("act", [wr.res, Rc.res], [c_.res],
                      lambda e: e.activation(out=c_[:, 0:1], in_=wr[:, TB - 1:TB], func=AF.Copy, scale=Rc[:, pr:pr + 1]))
                kb.op("act", [wr.res, Rs.res], [c_.res],
                      lambda e: e.activation(out=c_[:, 1:2], in_=wr[:, TB - 1:TB], func=AF.Copy, scale=Rs[:, pr:pr + 1]))
                kb.op("act", [wi.res, nRs.res, c_.res], [w0r_res[pr]],
                      lambda e: e.activation(out=w0r[:, pr:pr + 1], in_=wi[:, TB - 1:TB], func=AF.Identity,
                                             scale=nRs[:, pr:pr + 1], bias=c_[:, 0:1]))
                kb.op("act", [wi.res, Rc.res, c_.res], [w0i_res[pr]],
                      lambda e: e.activation(out=w0i[:, pr:pr + 1], in_=wi[:, TB - 1:TB], func=AF.Identity,
                                             scale=Rc[:, pr:pr + 1], bias=c_[:, 1:2]))
                P = PP[pr % 2]
                for k_, (o_, tab, src) in enumerate(((P[0], Tc, wr), (P[1], Ts, wi), (P[2], Ts, wr), (P[3], Tc, wi))):
                    en = "dve" if k_ == 3 else "pool"
                    kb.op(en, [tab.res, src.res], [o_.res],
                          lambda e: e.tensor_tensor(out=o_[:], in0=tab[:, pr, :], in1=src[:], op=ALU.mult))

            def stage2(pr):
                P = PP[pr % 2]
                t = pr // 4
                py = g.ps[4 + t]
                for k_, (Cm, Pk) in enumerate(((CRE, P[0]), (NCRE, P[1]), (NCIM, P[2]), (NCIM, P[3]))):
                    first = (pr % 4 == 0 and k_ == 0)
                    last = (pr % 4 == 3 and k_ == 3)
                    kb.op("pe", [Cm.res, Pk.res], [py.res],
                          lambda e: e.matmul(py[:, 0:TB], lhsT=Cm[:, pr, :], rhs=Pk[:], start=first, stop=last),
                          inc=(k_ == 3))
                if pr % 4 == 3:
                    y = ysb[t % 2]
                    g1 = gt1[t % 2]
                    kb.op("dve", [us.res, dcol.res, py.res], [y.res],
                          lambda e: e.scalar_tensor_tensor(out=y[:], in0=us[:, t, :], scalar=dcol[:, t:t + 1],
                                                           in1=py[:, 0:TB], op0=ALU.mult, op1=ALU.add))
                    kb.op("act", [y.res], [g1.res], lambda e: e.activation(out=g1[:], in_=y[:], func=AF.Square))
                    kb.op("dve", [g1.res], [g1.res],
                          lambda e: e.tensor_scalar(out=g1[:], in0=g1[:], scalar1=0.0713548162726, scalar2=1.5957691216057308,
                                                    op0=ALU.mult, op1=ALU.add))
                    kb.op("dve", [g1.res, y.res], [g1.res],
                          lambda e: e.tensor_tensor(out=g1[:], in0=g1[:], in1=y[:], op=ALU.mult))
                    kb.op("act", [g1.res], [g1.res], lambda e: e.activation(out=g1[:], in_=g1[:], func=AF.Sigmoid))
                    kb.op("dve", [g1.res, y.res], [zf.res],
                          lambda e: e.tensor_tensor(out=zf[:, t, :], in0=g1[:], in1=y[:], op=ALU.mult))
                    kb.op("act", [zf.res], [zb.res], lambda e: e.activation(out=zb[:, t, :], in_=zf[:, t, :], func=AF.Copy))

            stage1(0)
            for pr in range(16):
                if pr + 1 < 16:
                    stage1(pr + 1)
                stage2(pr)
            for ct in range(4):
                pg = nps(g)
                for kt in range(4):
                    kb.op("pe", [gw.res, zb.res], [pg.res],
                          lambda e: e.matmul(pg[:, 0:TB], lhsT=gw[:, kt, tsl(ct)], rhs=zb[:, kt, :],
                                             start=(kt == 0), stop=(kt == 3)), inc=(kt == 3))
                s_ = sgl[ct % 2]
                kb.op("act", [pg.res, dcol.res], [s_.res],
                      lambda e: e.activation(out=s_[:], in_=pg[:, 0:TB], func=AF.Sigmoid, bias=dcol[:, 4 + ct:5 + ct],
                                             scale=1.0))
                os_ = ost[ct % 2]
                kb.op("dve", [s_.res, zf.res], [os_.res],
                      lambda e: e.tensor_tensor(out=os_[:], in0=zf[:, ct, :], in1=s_[:], op=ALU.mult))
                kb.dma("sp", dr["o_scr"][12 + ct, :, ts_], os_[:], [os_.res], [g.scr_res["o"]])
    g.psn = 8
    barrier(kb)


WEIGHT_SPECS = {
    "ada_w": (D, 6 * D), "ada_b": (6 * D,), "norm_g": (4, D), "w_in": (D, INW), "diff_lambda": (4, 64),
    "ml_conv": (4, 1024), "ml_gate_b": (2, 4), "gla_wa2": (16, 256), "gla_ba": (256,),
    "s5_a_re": (32, 64), "s5_a_im": (32, 64), "s5_log_dt": (32,), "s5_b_re": (32, 64, 16), "s5_b_im": (32, 64, 16),
    "s5_c_re": (32, 16, 64), "s5_c_im": (32, 16, 64), "s5_d": (32, 16), "s5_glu_w": (512, 512), "s5_glu_b": (512,),
    "w_branch": (4, 512, D), "w_gate": (4, D, D), "b_gate": (4, D), "w_out": (D, D),
    "ffn_w_in": (D, 2 * FFH), "ffn_w_out": (FFH, D),
}
CONST_SPECS = {"c_ident": (128, 128), "c_tri": (128, 128), "c_biasT": (4, 5, 128, 512), "c_bias_far": (1, 4),
               "c_half": (128, 2), "c_pm": (128, 128)}


SWAP_LANES = True
BF_WEIGHTS = ("w_in", "w_gate", "w_branch", "w_out", "ffn_w_in")


def emit_weight_cast(kb, g, dr, nc, NL):
    for name in BF_WEIGHTS:
        src = dr[name]
        shp = list(src.shape)
        dst = nc.dram_tensor(name + "_bf", shp, BF16, kind="Internal").ap()
        if len(shp) == 4:
            s2 = src.rearrange("l i r c -> (l i r) c")
            d2 = dst.rearrange("l i r c -> (l i r) c")
        else:
            s2 = src.rearrange("l r c -> (l r) c")
            d2 = dst.rearrange("l r c -> (l r) c")
        rows = s2.shape[0]
        step = 512
        for r0 in range(0, rows, step):
            r1 = min(rows, r0 + step)
            kb.dma("pool", d2[r0:r1, :], s2[r0:r1, :], [], [g.w_res])
        dr[name] = dst
    NH = FFH // 128
    dst = nc.dram_tensor("ffn_w_out_r", [NL, NDT, 128, NH, 128], BF16, kind="Internal").ap()
    for l in range(NL):
        srcv = dr["ffn_w_out"][l].rearrange("(kt p) n -> p kt n", p=128)
        for dt in range(NDT):
            kb.dma("pool", dst[l, dt], srcv[:, :, tsl(dt)], [], [g.w_res])
    dr["ffn_w_out_r"] = dst
    barrier(kb)


def build_program(S, NL, debug=(), phases="ABCD"):
    nc = bass.Bass("TRN2", target_bir_lowering=False)
    kb = KB(nc)
    if SWAP_LANES:
        kb.add_lane("sp", "pool", 4)
        kb.add_lane("w", "sp", 4)
    else:
        kb.add_lane("sp", "sp", 4)
        kb.add_lane("w", "pool", 4)
    kb.add_lane("pool", "pool", 4)
    g = G()
    dr = {}
    dr["x"] = nc.dram_tensor("x", [S, D], F32, kind="ExternalInput").ap()
    dr["c"] = nc.dram_tensor("c", [1, D], F32, kind="ExternalInput").ap()
    for k, shp in WEIGHT_SPECS.items():
        dr[k] = nc.dram_tensor(k, [NL] + list(shp), F32, kind="ExternalInput").ap()
    for k, shp in CONST_SPECS.items():
        dr[k] = nc.dram_tensor(k, list(shp), F32, kind="ExternalInput").ap()
    dr["xres"] = nc.dram_tensor("out", [S, D], F32, kind="ExternalOutput").ap()
    scr = {"mod": ([NL, 6 * D], F32), "hT": ([16, 128, S], BF16), "qk": ([8, 128, S], BF16), "dav": ([S, 512], BF16),
           "mlqk": ([8, 128, S], F32), "mlv": ([S, 512], F32), "mlo": ([S, 512], F32), "mlif": ([S, 8], F32),
           "glqk": ([4, 128, S], F32), "gla": ([16, S], F32), "glv": ([S, 512], F32), "glr": ([S, 512], F32),
           "s5u": ([4, 128, S], F32), "o": ([16, 128, S], BF16)}
    g.scr_res = {}
    for k, (shp, dt) in scr.items():
        kind = "ExternalOutput" if k in debug else "Internal"
        dr[k + "_scr"] = nc.dram_tensor(k + "_scr", shp, dt, kind=kind).ap()
        g.scr_res[k] = Res()
    g.mod_res = g.scr_res["mod"]
    g.x_res = Res()
    alloc_psum(kb, g)
    emit_consts(kb, g, dr)
    nchunk = max(1, S // 1024)
    rows = S // nchunk
    for i in range(nchunk):
        kb.dma("sp", dr["xres"][i * rows:(i + 1) * rows, :], dr["x"][i * rows:(i + 1) * rows, :], [], [g.x_res])
    emit_mods(kb, g, dr, NL)
    g.w_res = Res()
    emit_weight_cast(kb, g, dr, nc, NL)
    for l in range(NL):
        with kb.scope():
            L = emit_layer_consts(kb, g, dr, l)
            if "A" in phases:
                emit_phaseA(kb, g, dr, L, l, S)
            if "B" in phases or "a" in phases:
                emit_attn(kb, g, dr, l, S)
            if "B" in phases or "m" in phases:
                emit_mlstm(kb, g, dr, l, S)
            if "B" in phases or "g" in phases:
                emit_gla(kb, g, dr, l, S)
            if "B" in phases or "s" in phases:
                emit_s5(kb, g, dr, l, S)
            if "C" in phases:
                emit_phaseC1(kb, g, dr, L, l, S)
            if "D" in phases:
                emit_phaseC2(kb, g, dr, L, l, S)
        barrier(kb)
    barrier(kb)
    return nc


def t5_bucket(rel):
    n = np.maximum(-rel, 0)
    exact = N_BUCKETS // 2
    nf = np.maximum(n, 1).astype(np.float32)
    large = exact + (np.log(nf / exact) / math.log(MAX_DISTANCE / exact) * (N_BUCKETS - exact)).astype(np.int32)
    return np.where(n < exact, n, np.minimum(large, N_BUCKETS - 1))


def host_consts(rel_bias):
    rb = np.asarray(rel_bias, np.float32)
    k = np.arange(128)[:, None]
    q = np.arange(128)[None, :]
    biasT = np.empty((4, 5, 128, 512), np.float32)
    qq = np.arange(512)[None, :]
    for di, dmin in enumerate(range(-3, 2)):
        rel = k - (dmin * 128 + qq)
        idx = t5_bucket(rel)
        for h in range(4):
            tab = rb[:, h][idx]
            biasT[h, di] = np.where(rel <= 0, tab, np.float32(-30000.0))
    c = {
        "c_ident": np.eye(128, dtype=np.float32),
        "c_tri": np.triu(np.ones((128, 128), np.float32)),
        "c_biasT": biasT,
        "c_bias_far": np.ascontiguousarray(rb[N_BUCKETS - 1:N_BUCKETS, :]),
        "c_half": np.stack([(np.arange(128) < 64), (np.arange(128) >= 64)], axis=1).astype(np.float32),
    }
    g8 = (np.arange(128) // 16)[:, None]
    j = (np.arange(128) // 64)[None, :]
    c["c_pm"] = ((g8 % 2) == j).astype(np.float32)
    return c


_PROG = {}


def kernel(**inputs):
    x = np.asarray(inputs["x"], np.float32)
    B, S, _ = x.shape
    NL = np.asarray(inputs["ada_w"]).shape[0]
    key = (S, NL)
    if key not in _PROG:
        _PROG[key] = build_program(S, NL)
    nc = _PROG[key]
    consts = host_consts(inputs["rel_bias"])
    shared = {k: np.ascontiguousarray(np.asarray(inputs[k], np.float32)) for k in WEIGHT_SPECS}
    shared.update(consts)
    in_maps = []
    for b in range(B):
        m = dict(shared)
        m["x"] = np.ascontiguousarray(x[b])
        m["c"] = np.ascontiguousarray(np.asarray(inputs["c"], np.float32)[b:b + 1])
        in_maps.append(m)
    res = run_bass_kernel_spmd(nc, in_maps, core_ids=list(range(B)))
    return np.stack([np.asarray(r["out"], np.float32) for r in res.results], axis=0)
```
